# Optimizing a Trainium2 kernel written in Bass

```python
import math
import jax, jax.numpy as jnp
from jax import lax
import numpy as np

D_MODEL = 1024
BATCH = 8
SEQ = 4096
DEPTH = 2

D_FF = ((8 * D_MODEL // 3 + 127) // 128) * 128
FFN_RES_WEIGHT = 0.5
ALPHA = (2 * DEPTH) ** 0.25
BETA = (8 * DEPTH) ** -0.25
LN_EPS = 1e-5
BLK = 128
CONV_CH = D_MODEL // 4
CONV_WIDTH = 3
DSWA_HEAD_DIM = 64
DSWA_W = D_MODEL - CONV_CH
DSWA_HEADS = DSWA_W // DSWA_HEAD_DIM
DILATIONS = ((128, 1), (512, 4), (2048, 16))
EVEN_PROJ = 3 * CONV_CH + 3 * DSWA_W
MLSTM_HEADS = 4
MLSTM_W = D_MODEL // 2
MLSTM_HEAD_DIM = MLSTM_W // MLSTM_HEADS
MLSTM_CHUNK = 128
SB_HEAD_DIM = 64
SB_W = D_MODEL - MLSTM_W
SB_HEADS = SB_W // SB_HEAD_DIM
ODD_PROJ = 4 * MLSTM_W + 2 * MLSTM_HEADS + 3 * SB_W

kernel_name = "hybrid_shortconv_dilated_mlstm_stickbreaking"


def _split(t, sizes):
    cuts = [int(c) for c in np.cumsum(sizes)[:-1]]
    return jnp.split(t, cuts, axis=-1)


def _post_norm(x, y, g, b):
    z = (ALPHA * x + y).astype(jnp.float32)
    mu = z.mean(-1, keepdims=True)
    var = jnp.square(z - mu).mean(-1, keepdims=True)
    return ((z - mu) * lax.rsqrt(var + LN_EPS) * g + b).astype(x.dtype)


def _swiglu(x, w_in, w_out):
    gate, up = jnp.split(x @ w_in, 2, axis=-1)
    return (jax.nn.silu(gate) * up) @ w_out


def _short_conv(bg, cg, xh, conv_w):
    u = cg * xh
    y = lax.conv_general_dilated(
        u, conv_w[:, None, :], window_strides=(1,),
        padding=((CONV_WIDTH - 1, 0),),
        dimension_numbers=("NWC", "WIO", "NWC"),
        feature_group_count=CONV_CH)
    return bg * y


def _dilated_branch(q, k, v, window, dilation):
    b, s_pad, h, dh = q.shape
    win = window // dilation
    n_sub = s_pad // dilation
    nb = n_sub // BLK

    def to_blocks(t):
        t = t.reshape(b, n_sub, dilation, h, dh).transpose(0, 2, 3, 1, 4)
        return t.reshape(b, dilation, h, nb, BLK, dh)

    def with_prev(t):
        prev = jnp.pad(t[:, :, :, :-1], ((0, 0), (0, 0), (0, 0), (1, 0), (0, 0), (0, 0)))
        return jnp.concatenate([prev, t], axis=-2)

    def to_seq(t):
        t = t.reshape((b, dilation, h, n_sub) + t.shape[5:])
        t = jnp.moveaxis(t, 3, 1)
        return t.reshape((b, s_pad, h) + t.shape[4:])

    qb = to_blocks(q)
    kb = with_prev(to_blocks(k))
    vb = with_prev(to_blocks(v))
    scores = jnp.einsum("bdhnqe,bdhnke->bdhnqk", qb, kb).astype(jnp.float32) / math.sqrt(dh)
    qi = jnp.arange(BLK)[:, None]
    kj = jnp.arange(2 * BLK)[None, :]
    rel = qi + BLK - kj
    band = (rel >= 0) & (rel <= win)
    exists = (jnp.arange(nb)[:, None, None] > 0) | (kj >= BLK)[None]
    scores = jnp.where(band[None] & exists, scores, -jnp.inf)
    m = scores.max(-1)
    p = jnp.exp(scores - m[..., None])
    den = p.sum(-1)
    out = jnp.einsum("bdhnqk,bdhnke->bdhnqe", p, vb.astype(jnp.float32)) / den[..., None]
    return to_seq(out), to_seq(m), to_seq(den)


def _dilated_attention(q, k, v):
    s = q.shape[1]
    unit = max(d for _, d in DILATIONS) * BLK
    s_pad = -(-s // unit) * unit
    pad = ((0, 0), (0, s_pad - s), (0, 0), (0, 0))
    q, k, v = (jnp.pad(t, pad) for t in (q, k, v))
    outs, maxes, dens = zip(*[_dilated_branch(q, k, v, w, d) for w, d in DILATIONS])
    m = jnp.stack(maxes)
    wgt = jnp.stack(dens) * jnp.exp(m - m.max(0))
    out = jnp.einsum("gbsh,gbshe->bshe", wgt, jnp.stack(outs)) / wgt.sum(0)[..., None]
    return out[:, :s]


def _mlstm(q, k, v, i_pre, f_pre):
    b, s, h, dh = q.shape
    L = MLSTM_CHUNK
    nc = s // L
    f32 = jnp.float32

    def chunks(t):
        t = t.astype(f32).reshape((b, nc, L, h) + t.shape[3:])
        return jnp.moveaxis(jnp.moveaxis(t, 1, 0), 3, 2)

    xs = (chunks(q), chunks(k) / math.sqrt(dh), chunks(v),
          chunks(i_pre), chunks(jax.nn.log_sigmoid(f_pre.astype(f32))))
    causal = jnp.tril(jnp.ones((L, L), bool))

    def step(carry, inp):
        C, n, m = carry
        qt, kt, vt, it, lf = inp
        bcum = jnp.cumsum(lf, axis=-1)
        d_intra = jnp.where(causal, bcum[..., :, None] - bcum[..., None, :] + it[..., None, :], -jnp.inf)
        d_inter = bcum + m[..., None]
        m_t = jnp.maximum(d_inter, d_intra.max(-1))
        w_inter = jnp.exp(d_inter - m_t)
        qk = jnp.einsum("bhld,bhsd->bhls", qt, kt) * jnp.exp(d_intra - m_t[..., None])
        num = w_inter[..., None] * jnp.einsum("bhld,bhde->bhle", qt, C) + jnp.einsum("bhls,bhse->bhle", qk, vt)
        den = w_inter * jnp.einsum("bhld,bhd->bhl", qt, n) + qk.sum(-1)
        h_out = num / jnp.maximum(jnp.abs(den), jnp.exp(-m_t))[..., None]
        btot = bcum[..., -1]
        d_state = btot[..., None] - bcum + it
        m_new = jnp.maximum(btot + m, d_state.max(-1))
        w_s = jnp.exp(d_state - m_new[..., None])
        decay = jnp.exp(btot + m - m_new)
        C_new = decay[..., None, None] * C + jnp.einsum("bhs,bhsd,bhse->bhde", w_s, kt, vt)
        n_new = decay[..., None] * n + jnp.einsum("bhs,bhsd->bhd", w_s, kt)
        return (C_new, n_new, m_new), h_out

    init = (jnp.zeros((b, h, dh, dh), f32), jnp.zeros((b, h, dh), f32), jnp.zeros((b, h), f32))
    _, hs = lax.scan(step, init, xs)
    return hs.transpose(1, 0, 3, 2, 4).reshape(b, s, h, dh)


def _head_norm(h, g):
    b, s = h.shape[:2]
    mu = h.mean(-1, keepdims=True)
    var = jnp.square(h - mu).mean(-1, keepdims=True)
    return ((h - mu) * lax.rsqrt(var + LN_EPS)).reshape(b, s, -1) * g


def _stick_breaking(q, k, v):
    b, s, h, dh = q.shape
    nb = s // BLK
    qb = q.reshape(b, nb, BLK, h, dh).transpose(1, 0, 3, 2, 4)
    kt = k.transpose(0, 2, 1, 3)
    vt = v.transpose(0, 2, 1, 3).astype(jnp.float32)
    key_pos = jnp.arange(s)

    def block(args):
        qblk, start = args
        z = jnp.einsum("bhqd,bhkd->bhqk", qblk, kt).astype(jnp.float32) / math.sqrt(dh)
        before = key_pos[None, :] < (start + jnp.arange(BLK))[:, None]
        log_1m = jnp.where(before, jax.nn.log_sigmoid(-z), 0.0)
        suffix = lax.cumsum(log_1m, axis=log_1m.ndim - 1, reverse=True) - log_1m
        a = jnp.where(before, jnp.exp(jax.nn.log_sigmoid(z) + suffix), 0.0)
        return jnp.einsum("bhqk,bhkd->bhqd", a, vt)

    out = lax.map(block, (qb, jnp.arange(nb) * BLK))
    return out.transpose(1, 0, 3, 2, 4).reshape(b, s, h, dh)


def _even_mixer(x, w_in, conv_w, w_out):
    b, s, _ = x.shape
    bg, cg, xh, q, k, v = _split(x @ w_in, [CONV_CH] * 3 + [DSWA_W] * 3)
    y_conv = _short_conv(bg, cg, xh, conv_w)
    heads = lambda t: t.reshape(b, s, DSWA_HEADS, DSWA_HEAD_DIM)
    y_att = _dilated_attention(heads(q), heads(k), heads(v)).reshape(b, s, DSWA_W)
    return jnp.concatenate([y_conv, y_att.astype(x.dtype)], axis=-1) @ w_out


def _odd_mixer(x, w_in, b_i, b_f, norm_g, w_out):
    b, s, _ = x.shape
    q_m, k_m, v_m, o_m, i_g, f_g, q_s, k_s, v_s = _split(
        x @ w_in, [MLSTM_W] * 4 + [MLSTM_HEADS] * 2 + [SB_W] * 3)
    mh = lambda t: t.reshape(b, s, MLSTM_HEADS, MLSTM_HEAD_DIM)
    sh = lambda t: t.reshape(b, s, SB_HEADS, SB_HEAD_DIM)
    h_tilde = _mlstm(mh(q_m), mh(k_m), mh(v_m), i_g + b_i, f_g + b_f)
    h_cell = jax.nn.sigmoid(mh(o_m).astype(jnp.float32)) * h_tilde
    y_m = _head_norm(h_cell, norm_g)
    y_s = _stick_breaking(sh(q_s), sh(k_s), sh(v_s)).reshape(b, s, SB_W)
    return jnp.concatenate([y_m.astype(x.dtype), y_s.astype(x.dtype)], axis=-1) @ w_out


def setup_inputs(seed: int = 0) -> dict:
    key = jax.random.key(seed)
    ks = iter(jax.random.split(key, 40))
    nrm = lambda shape, scale: scale * jax.random.normal(next(ks), shape, jnp.float32)
    gain = lambda: 1.0 + nrm((D_MODEL,), 0.02)
    bias = lambda: nrm((D_MODEL,), 0.02)
    ffn_in = lambda: nrm((D_MODEL, 2 * D_FF), D_MODEL ** -0.5)
    ffn_out = lambda: nrm((D_FF, D_MODEL), BETA * D_FF ** -0.5)
    mix_out = lambda: nrm((D_MODEL, D_MODEL), BETA * D_MODEL ** -0.5)
    return {
        "x": nrm((BATCH, SEQ, D_MODEL), 1.0),
        "l0_ffn1_w_in": ffn_in(), "l0_ffn1_w_out": ffn_out(),
        "l0_ln1_g": gain(), "l0_ln1_b": bias(),
        "l0_mix_w_in": nrm((D_MODEL, EVEN_PROJ), D_MODEL ** -0.5),
        "l0_conv_w": nrm((CONV_WIDTH, CONV_CH), CONV_WIDTH ** -0.5),
        "l0_mix_w_out": mix_out(),
        "l0_ln2_g": gain(), "l0_ln2_b": bias(),
        "l0_ffn2_w_in": ffn_in(), "l0_ffn2_w_out": ffn_out(),
        "l0_ln3_g": gain(), "l0_ln3_b": bias(),
        "l1_ffn1_w_in": ffn_in(), "l1_ffn1_w_out": ffn_out(),
        "l1_ln1_g": gain(), "l1_ln1_b": bias(),
        "l1_mix_w_in": nrm((D_MODEL, ODD_PROJ), D_MODEL ** -0.5),
        "l1_mlstm_b_i": nrm((MLSTM_HEADS,), 0.1),
        "l1_mlstm_b_f": jnp.linspace(3.0, 6.0, MLSTM_HEADS, dtype=jnp.float32) + nrm((MLSTM_HEADS,), 0.1),
        "l1_mlstm_norm_g": 1.0 + nrm((MLSTM_W,), 0.02),
        "l1_mix_w_out": mix_out(),
        "l1_ln2_g": gain(), "l1_ln2_b": bias(),
        "l1_ffn2_w_in": ffn_in(), "l1_ffn2_w_out": ffn_out(),
        "l1_ln3_g": gain(), "l1_ln3_b": bias(),
    }


def reference(x,
              l0_ffn1_w_in, l0_ffn1_w_out, l0_ln1_g, l0_ln1_b,
              l0_mix_w_in, l0_conv_w, l0_mix_w_out, l0_ln2_g, l0_ln2_b,
              l0_ffn2_w_in, l0_ffn2_w_out, l0_ln3_g, l0_ln3_b,
              l1_ffn1_w_in, l1_ffn1_w_out, l1_ln1_g, l1_ln1_b,
              l1_mix_w_in, l1_mlstm_b_i, l1_mlstm_b_f, l1_mlstm_norm_g, l1_mix_w_out,
              l1_ln2_g, l1_ln2_b,
              l1_ffn2_w_in, l1_ffn2_w_out, l1_ln3_g, l1_ln3_b):
    ffn1 = ((l0_ffn1_w_in, l0_ffn1_w_out), (l1_ffn1_w_in, l1_ffn1_w_out))
    ffn2 = ((l0_ffn2_w_in, l0_ffn2_w_out), (l1_ffn2_w_in, l1_ffn2_w_out))
    ln1 = ((l0_ln1_g, l0_ln1_b), (l1_ln1_g, l1_ln1_b))
    ln2 = ((l0_ln2_g, l0_ln2_b), (l1_ln2_g, l1_ln2_b))
    ln3 = ((l0_ln3_g, l0_ln3_b), (l1_ln3_g, l1_ln3_b))
    for layer in range(DEPTH):
        x = _post_norm(x, FFN_RES_WEIGHT * _swiglu(x, *ffn1[layer]), *ln1[layer])
        if layer % 2 == 0:
            mix = _even_mixer(x, l0_mix_w_in, l0_conv_w, l0_mix_w_out)
        else:
            mix = _odd_mixer(x, l1_mix_w_in, l1_mlstm_b_i, l1_mlstm_b_f, l1_mlstm_norm_g, l1_mix_w_out)
        x = _post_norm(x, mix, *ln2[layer])
        x = _post_norm(x, FFN_RES_WEIGHT * _swiglu(x, *ffn2[layer]), *ln3[layer])
    return x
```

```python
import numpy as np
from contextlib import ExitStack
import concourse.bass as bass
import concourse.mybir as mybir
from concourse.bass_utils import run_bass_kernel_spmd

F32 = mybir.dt.float32
F32R = mybir.dt.float32r
AF = mybir.ActivationFunctionType
ALU = mybir.AluOpType
AX = mybir.AxisListType

D = 1024
S = 4096
DFF = 2816
NCH = DFF // 128
ALPHA = 4 ** 0.25
LN_EPS = 1e-5
NCORES = 8
DEBUG_OUT = True


class Buf:
    __slots__ = ("name", "w", "r", "dsem")

    def __init__(self, name, dsem=None):
        self.name = name
        self.w = {}
        self.r = {}
        self.dsem = dsem


class Eng:
    def __init__(self, name, sem):
        self.name = name
        self.sem = sem
        self.waited = {}
        self.insts = []


class SemC:
    def __init__(self, handle, is_dma):
        self.h = handle
        self.count = 0
        self.is_dma = is_dma


class Prog:
    def __init__(self, nc, stack):
        self.nc = nc
        self.stack = stack
        self.nsem = 0
        self.dma_sems = []
        self.free_sems = []
        self.phase_sems = []
        self.eng = {}
        for n in ("pe", "act", "dve", "pool", "sp"):
            self.eng[n] = Eng(n, self.new_sem(n, False))

    def new_sem(self, name, is_dma):
        self.nsem += 1
        h = self.stack.enter_context(self.nc.semaphore("s_%s_%d" % (name, self.nsem)))
        sc = SemC(h, is_dma)
        if is_dma:
            self.dma_sems.append(sc)
        return sc

    def sbuf(self, name, shape, dtype=F32):
        return self.stack.enter_context(self.nc.sbuf_tensor("sb_" + name, shape, dtype))

    def psum(self, name, shape, dtype=F32):
        return self.stack.enter_context(self.nc.psum_tensor("pt_" + name, shape, dtype))

    def _deps(self, e, reads, writes):
        need = {}

        def add(d):
            for s, v in d.items():
                if s.is_dma:
                    v = s.count
                if need.get(s, 0) < v:
                    need[s] = v
        for b in reads:
            add(b.w)
        for b in writes:
            add(b.w)
            add(b.r)
        waits = []
        for s, v in need.items():
            if s is e.sem and e.name == "pe":
                continue
            if e.waited.get(s, 0) >= v:
                continue
            e.waited[s] = v
            waits.append((s.h, v))
        return waits

    def op(self, en, reads, writes, meth, *args, **kwargs):
        e = self.eng[en]
        fn = (lambda h, meth=meth, args=args, kwargs=kwargs: getattr(h, meth)(*args, **kwargs))
        waits = self._deps(e, reads, writes)
        e.sem.count += 1
        v = e.sem.count
        e.insts.append((waits, fn, e.sem.h, 1))
        for b in reads:
            b.r[e.sem] = v
        for b in writes:
            b.w[e.sem] = v

    def dma(self, en, out, in_, reads, writes):
        e = self.eng[en]
        assert len(writes) == 1
        dst = writes[0]
        if dst.dsem is None:
            if self.free_sems:
                dst.dsem = self.free_sems.pop()
            else:
                dst.dsem = self.new_sem("d_" + dst.name, True)
            self.phase_sems.append(dst.dsem)
        waits = self._deps(e, reads, writes)
        dst.dsem.count += 16
        v = dst.dsem.count
        e.insts.append((waits, lambda h: h.dma_start(out=out, in_=in_), dst.dsem.h, 16))
        for b in reads:
            b.r[dst.dsem] = v
        dst.w[dst.dsem] = v

    def barrier(self):
        allsems = [e.sem for e in self.eng.values()] + self.dma_sems
        for e in self.eng.values():
            waits = []
            for sc in allsems:
                if sc is e.sem or sc.count == 0:
                    continue
                if e.waited.get(sc, 0) >= sc.count:
                    continue
                e.waited[sc] = sc.count
                waits.append((sc.h, sc.count))
            if waits:
                e.insts.append((waits, None, None, 0))
        self.free_sems.extend(self.phase_sems)
        self.phase_sems = []

    def final_wait(self, en, bufs):
        e = self.eng[en]
        waits = self._deps(e, bufs, ())
        e.insts.append((waits, None, None, 0))

    def emit(self):
        nc = self.nc
        with nc.Block() as block:
            def mk(e):
                def body(h):
                    for waits, fn, sem, inc in e.insts:
                        for sh, v in waits:
                            h.wait_ge(sh, v)
                        if fn is not None:
                            fn(h).then_inc(sem, inc)
                return body
            block.tensor(mk(self.eng["pe"]))
            block.scalar(mk(self.eng["act"]))
            block.vector(mk(self.eng["dve"]))
            block.gpsimd(mk(self.eng["pool"]))
            block.sync(mk(self.eng["sp"]))


def r32(ap):
    return ap.bitcast(F32R)


class Ctx:
    pass


class Arena:
    def __init__(self, P, nelem, name="arena"):
        self.t = P.sbuf(name, [128, nelem])
        self.n = nelem
        self.off = 0

    def reset(self):
        self.off = 0

    def alloc(self, n, parts=128):
        n += n % 2
        assert self.off + n <= self.n, (self.off, n, self.n)
        ap = self.t[0:parts, self.off:self.off + n]
        self.off += n
        return ap

    def alloc3(self, k, n, parts=128):
        return self.alloc(k * n, parts).rearrange("p (k n) -> p k n", k=k)


def setup_common(P, nc, consts):
    C = Ctx()
    C.P = P
    C.nc = nc
    C.psall = P.psum("psall", [128, 4096])
    C.ps = [C.psall[:, i * 512:(i + 1) * 512] for i in range(8)]
    C.psb = [Buf("ps%d" % i) for i in range(8)]
    C.ident = P.sbuf("ident", [128, 128])
    C.identb = Buf("ident")
    P.dma("sp", C.ident[:, :], consts["ident"], [], [C.identb])
    C.mhalf = P.sbuf("mhalf", [128, 1])
    C.mhalfb = Buf("mhalf")
    P.op("dve", [], [C.mhalfb], "memset", C.mhalf[:, :], -0.5)
    C.one = P.sbuf("one", [128, 64])
    C.oneb = Buf("one")
    P.op("dve", [], [C.oneb], "memset", C.one[:, :], 1.0)
    C.A = Arena(P, 30720, "arenaR")
    C.N = Arena(P, 15600, "arenaN")
    C.consts = consts
    return C


def load_gb(C, g_d, b_d, gt, bt, gb_b):
    P = C.P
    P.dma("sp", gt, g_d.partition_broadcast(128), [], [gb_b[0]])
    P.dma("sp", bt, b_d.partition_broadcast(128), [], [gb_b[1]])


class Work:
    pass


def alloc_norm(C, W):
    A = C.N
    W.z = [A.alloc(D) for i in range(4)]
    W.zb = [Buf("z%d" % i) for i in range(4)]
    W.st = [A.alloc(16) for i in range(4)]
    W.stb = [Buf("st%d" % i) for i in range(4)]
    W.gt = A.alloc(D)
    W.bt = A.alloc(D)
    W.gbb = [Buf("gt"), Buf("bt")]
    W.zcnt = 0


def post_norm_evac(C, W, y_banks, xs, xsb):
    P = C.P
    zi = W.zcnt
    W.zcnt += 1
    z = W.z[zi % 4]
    zb = W.zb[zi % 4]
    for hh in range(2):
        yb = y_banks[hh]
        P.op("dve", [xsb, C.psb[yb]], [zb], "scalar_tensor_tensor",
             out=z[:, hh * 512:(hh + 1) * 512], in0=xs[:, hh * 512:(hh + 1) * 512], scalar=ALPHA,
             in1=C.ps[yb][:, :], op0=ALU.mult, op1=ALU.add)
    return zi


def post_norm_finish(C, W, zi, x_out_rows, x_out_buf):
    P = C.P
    z = W.z[zi % 4]
    zb = W.zb[zi % 4]
    st = W.st[zi % 4]
    stb = W.stb[zi % 4]
    gt, bt, gb_b = W.gt, W.bt, W.gbb
    for hh in range(2):
        P.op("dve", [zb], [stb], "bn_stats", out=st[:, hh * 6:(hh + 1) * 6], in_=z[:, hh * 512:(hh + 1) * 512])
    P.op("dve", [stb], [stb], "bn_aggr", out=st[:, 12:14], in_=st[:, 0:12])
    P.op("dve", [stb], [stb], "tensor_scalar", out=st[:, 14:15], in0=st[:, 13:14], scalar1=LN_EPS, scalar2=None,
         op0=ALU.add)
    P.op("pool", [stb, C.mhalfb], [stb], "tensor_tensor", out=st[:, 15:16], in0=st[:, 14:15], in1=C.mhalf[:, 0:1],
         op=ALU.pow)
    P.op("dve", [zb, stb], [zb], "tensor_scalar", out=z, in0=z, scalar1=st[:, 12:13],
         scalar2=st[:, 15:16], op0=ALU.subtract, op1=ALU.mult)
    P.op("pool", [zb, gb_b[0]], [zb], "tensor_tensor", out=z, in0=z, in1=gt, op=ALU.mult)
    P.op("pool", [zb, gb_b[1]], [zb], "tensor_tensor", out=z, in0=z, in1=bt, op=ALU.add)
    P.dma("pool", x_out_rows, z, [zb], [x_out_buf])


def post_norm_tile(C, W, y_banks, xs, xsb, x_out_rows, x_out_buf):
    zi = post_norm_evac(C, W, y_banks, xs, xsb)
    post_norm_finish(C, W, zi, x_out_rows, x_out_buf)


def alloc_xT(C, W, nbuf=2):
    A = C.A
    W.xs = [C.N.alloc(D) for i in range(4)]
    W.xsb = [Buf("xs%d" % i) for i in range(4)]
    W.nxT = nbuf
    W.xT = [A.alloc3(8, 512) for i in range(nbuf)]
    W.xTb = [[Buf("xT%d_%d" % (i, k)) for k in range(8)] for i in range(nbuf)]
    W.xscnt = 0
    W.xTcnt = 0


def load_xT(C, W, x_in, x_in_bufs, t):
    P = C.P
    xTi = W.xTcnt % W.nxT
    W.xTcnt += 1
    xT = W.xT[xTi]
    xTb = W.xTb[xTi]
    xsl = []
    for s in range(4):
        i = W.xscnt % 4
        W.xscnt += 1
        r0 = t * 512 + s * 128
        P.dma("sp", W.xs[i], x_in[r0:r0 + 128, :], [x_in_bufs[t * 4 + s]], [W.xsb[i]])
        xsl.append(i)
    for kc in range(8):
        bank = kc % 2
        for s in range(4):
            i = xsl[s]
            P.op("pe", [W.xsb[i], C.identb], [C.psb[bank]], "transpose",
                 out=C.ps[bank][:, s * 128:(s + 1) * 128], in_=W.xs[i][:, kc * 128:(kc + 1) * 128],
                 identity=C.ident[:, :])
        if kc % 2 == 0:
            P.op("act", [C.psb[bank]], [xTb[kc]], "copy", out=r32(xT[:, kc, :]), in_=C.ps[bank][:, :])
        else:
            P.op("dve", [C.psb[bank]], [xTb[kc]], "tensor_copy", out=r32(xT[:, kc, :]), in_=C.ps[bank][:, :])
    return xT, xTb


def reload_xs(C, W, x_in, x_in_bufs, row_tile):
    P = C.P
    i = W.xscnt % 4
    W.xscnt += 1
    r0 = row_tile * 128
    P.dma("sp", W.xs[i], x_in[r0:r0 + 128, :], [x_in_bufs[row_tile]], [W.xsb[i]])
    return W.xs[i], W.xsb[i]


def ffn_sublayer(C, x_in, x_in_bufs, x_out, x_out_bufs, w_in, w_out, g_d, b_d):
    P = C.P
    A = C.A
    P.barrier()
    A.reset()
    C.N.reset()
    W = Work()
    alloc_norm(C, W)
    xT = A.alloc3(8, 512)
    xTb = [Buf("xT_%d" % k) for k in range(8)]
    xss = [[C.N.alloc(D) for i in range(4)] for j in range(2)]
    xssb = [[Buf("xs%d_%d" % (j, i)) for i in range(4)] for j in range(2)]

    def issue_loads(t):
        for s_ in range(4):
            r0 = t * 512 + s_ * 128
            P.dma("sp", xss[t % 2][s_], x_in[r0:r0 + 128, :], [x_in_bufs[t * 4 + s_]], [xssb[t % 2][s_]])
    aT = A.alloc3(NCH, 512)
    aTb = [Buf("aT%d" % j) for j in range(NCH)]
    wins = [A.alloc(8 * 2 * 256).rearrange("p (k h n) -> p k h n", k=8, h=2) for i in range(3)]
    winbs = [Buf("win%d" % i) for i in range(3)]
    wouts = [A.alloc(D) for i in range(3)]
    woutbs = [Buf("wout%d" % i) for i in range(3)]
    sgs = [C.N.alloc(512) for i in range(2)]
    sgbs = [Buf("sg%d" % i) for i in range(2)]
    cnt = {"win": 0, "wout": 0, "sg": 0}
    load_gb(C, g_d, b_d, W.gt, W.bt, W.gbb)
    w_in_v = w_in.rearrange("(kc p) n -> p kc n", p=128)
    NT = S // 512
    issue_loads(0)
    for t in range(NT):
        for kc in range(8):
            bank = kc % 2
            for s_ in range(4):
                P.op("pe", [xssb[t % 2][s_], C.identb], [C.psb[bank]], "transpose",
                     out=C.ps[bank][:, s_ * 128:(s_ + 1) * 128], in_=xss[t % 2][s_][:, kc * 128:(kc + 1) * 128],
                     identity=C.ident[:, :])
            if kc % 2 == 0:
                P.op("act", [C.psb[bank]], [xTb[kc]], "copy", out=r32(xT[:, kc, :]), in_=C.ps[bank][:, :])
            else:
                P.op("dve", [C.psb[bank]], [xTb[kc]], "tensor_copy", out=r32(xT[:, kc, :]), in_=C.ps[bank][:, :])
        for grp in range(NCH // 2):
            wi = cnt["win"] % 3
            cnt["win"] += 1
            win = wins[wi]
            winb = winbs[wi]
            P.dma("sp", r32(win[:, :, 0, :]), r32(w_in_v[:, :, grp * 256:(grp + 1) * 256]), [], [winb])
            P.dma("sp", r32(win[:, :, 1, :]), r32(w_in_v[:, :, DFF + grp * 256:DFF + (grp + 1) * 256]), [], [winb])
            for jj in range(2):
                j = grp * 2 + jj
                gb_ = 2 + 2 * (j % 2)
                ub_ = 3 + 2 * (j % 2)
                for half, bank in ((0, gb_), (1, ub_)):
                    for kc in range(8):
                        P.op("pe", [winb, xTb[kc]], [C.psb[bank]], "matmul",
                             C.ps[bank][:, :], lhsT=r32(win[:, kc, half, jj * 128:(jj + 1) * 128]),
                             rhs=r32(xT[:, kc, :]), start=(kc == 0), stop=(kc == 7))
                si = cnt["sg"] % 2
                cnt["sg"] += 1
                sg = sgs[si]
                P.op("act", [C.psb[gb_]], [sgbs[si]], "activation", out=sg, in_=C.ps[gb_][:, :], func=AF.Silu)
                P.op("dve", [sgbs[si], C.psb[ub_]], [aTb[j]], "scalar_tensor_tensor",
                     out=r32(aT[:, j, :]), in0=sg, scalar=0.5, in1=C.ps[ub_][:, :], op0=ALU.mult, op1=ALU.mult)
        if t + 1 < NT:
            issue_loads(t + 1)
        for j in range(NCH):
            wi = cnt["wout"] % 3
            cnt["wout"] += 1
            wout = wouts[wi]
            woutb = woutbs[wi]
            P.dma("sp", r32(wout), r32(w_out[j * 128:(j + 1) * 128, :]), [], [woutb])
            for s in range(4):
                for hh in range(2):
                    bank = 2 * s + hh
                    P.op("pe", [aTb[j], woutb], [C.psb[bank]], "matmul",
                         C.ps[bank][:, :], lhsT=r32(aT[:, j, s * 128:(s + 1) * 128]),
                         rhs=r32(wout[:, hh * 512:(hh + 1) * 512]), start=(j == 0), stop=(j == NCH - 1))
        zis = []
        for s in range(4):
            rt = t * 4 + s
            zis.append(post_norm_evac(C, W, (2 * s, 2 * s + 1), xss[t % 2][s], xssb[t % 2][s]))
        for s in range(4):
            rt = t * 4 + s
            post_norm_finish(C, W, zis[s], x_out[rt * 128:(rt + 1) * 128, :], x_out_bufs[rt])


def out_proj_norm(C, yT_d, yT_bufs, w_out, g_d, b_d, x_in, x_in_bufs, x_out, x_out_bufs):
    P = C.P
    A = C.A
    P.barrier()
    A.reset()
    C.N.reset()
    W = Work()
    alloc_norm(C, W)
    W.xs = [C.N.alloc(D) for i in range(4)]
    W.xsb = [Buf("xs%d" % i) for i in range(4)]
    W.xscnt = 0
    wo = A.alloc3(8, D)
    wob = Buf("wo")
    P.dma("sp", r32(wo), r32(w_out.rearrange("(kc p) n -> p kc n", p=128)), [], [wob])
    yts = [A.alloc3(8, 512) for i in range(2)]
    ytbs = [Buf("yt%d" % i) for i in range(2)]
    load_gb(C, g_d, b_d, W.gt, W.bt, W.gbb)
    yT_v = yT_d.rearrange("(kc p) n -> p kc n", p=128)
    for t in range(S // 512):
        yt = yts[t % 2]
        ytb = ytbs[t % 2]
        P.dma("sp", r32(yt), r32(yT_v[:, :, t * 512:(t + 1) * 512]), yT_bufs, [ytb])
        for s in range(4):
            rt = t * 4 + s
            banks = (2 * (rt % 4), 2 * (rt % 4) + 1)
            for hh in range(2):
                for kc in range(8):
                    P.op("pe", [ytb, wob], [C.psb[banks[hh]]], "matmul", C.ps[banks[hh]][:, :],
                         lhsT=r32(yt[:, kc, s * 128:(s + 1) * 128]), rhs=r32(wo[:, kc, hh * 512:(hh + 1) * 512]),
                         start=(kc == 0), stop=(kc == 7))
            xs, xsb = reload_xs(C, W, x_in, x_in_bufs, rt)
            post_norm_tile(C, W, banks, xs, xsb, x_out[rt * 128:(rt + 1) * 128, :], x_out_bufs[rt])


def proj_phase(C, x_in, x_in_bufs, w_in, ccols, small_ci, projT, projb):
    P = C.P
    A = C.A
    N = C.N
    P.barrier()
    A.reset()
    N.reset()
    xs = [N.alloc(D) for i in range(4)]
    xsb = [Buf("xs%d" % i) for i in range(4)]
    evs = [N.alloc(512) for i in range(4)]
    evbs = [Buf("ev%d" % i) for i in range(4)]
    HT = 2048
    xT = A.alloc3(8, HT)
    xTb = [[Buf("xTb%d_%d" % (tt, kc)) for kc in range(8)] for tt in range(4)]
    wcs = [A.alloc3(8, 128) for i in range(3)]
    wcbs = [Buf("wc%d" % i) for i in range(3)]
    w_in_v = w_in.rearrange("(kc p) n -> p kc n", p=128)
    xc = 0
    k = 0
    wk = 0
    for half in range(2):
        for tt in range(4):
            t = half * 4 + tt
            xsl = []
            for s_ in range(4):
                i = xc % 4
                xc += 1
                r0 = t * 512 + s_ * 128
                P.dma("sp", xs[i], x_in[r0:r0 + 128, :], [x_in_bufs[t * 4 + s_]], [xsb[i]])
                xsl.append(i)
            for kc in range(8):
                bank = kc % 2
                for s_ in range(4):
                    i = xsl[s_]
                    P.op("pe", [xsb[i], C.identb], [C.psb[bank]], "transpose",
                         out=C.ps[bank][:, s_ * 128:(s_ + 1) * 128], in_=xs[i][:, kc * 128:(kc + 1) * 128],
                         identity=C.ident[:, :])
                dst = r32(xT[:, kc, tt * 512:(tt + 1) * 512])
                if kc % 2 == 0:
                    P.op("act", [C.psb[bank]], [xTb[tt][kc]], "copy", out=dst, in_=C.ps[bank][:, :])
                else:
                    P.op("dve", [C.psb[bank]], [xTb[tt][kc]], "tensor_copy", out=dst, in_=C.ps[bank][:, :])
        for ci, c0 in enumerate(ccols):
            nrow = 8 if ci == small_ci else 128
            wc = wcs[wk % 3]
            wcb = wcbs[wk % 3]
            wk += 1
            P.dma("sp", r32(wc), r32(w_in_v[:, :, c0:c0 + 128]), [], [wcb])
            for tt in range(4):
                t = half * 4 + tt
                ev = evs[k % 4]
                evb = evbs[k % 4]
                bank = 2 + (k % 4)
                for kc in range(8):
                    P.op("pe", [wcb, xTb[tt][kc]], [C.psb[bank]], "matmul", C.ps[bank][:, :],
                         lhsT=r32(wc[:, kc, :]), rhs=r32(xT[:, kc, tt * 512:(tt + 1) * 512]),
                         start=(kc == 0), stop=(kc == 7))
                if k % 2 == 0:
                    P.op("act", [C.psb[bank]], [evb], "copy", out=ev, in_=C.ps[bank][:, :])
                else:
                    P.op("dve", [C.psb[bank]], [evb], "tensor_copy", out=ev, in_=C.ps[bank][:, :])
                P.dma("pool", projT[c0:c0 + nrow, t * 512:(t + 1) * 512], ev[0:nrow, :], [evb], [projb[ci][t]])
                k += 1


NEGM = -240000.0


def even_mixer(C, x_in, x_in_bufs, x_out, x_out_bufs, prm, scr):
    P = C.P
    A = C.A
    w_in = prm["l0_mix_w_in"]
    projT, projb = scr["projT"], scr["projb"]
    yT, yTb = scr["yT"], scr["yTb"]
    proj_phase(C, x_in, x_in_bufs, w_in, [c * 128 for c in range(24)], -1, projT, projb)
    P.barrier()
    A.reset()
    C.N.reset()
    N = C.N
    HB = S // 2
    bg = N.alloc(HB + 2)
    cg = N.alloc(HB + 2)
    xh = N.alloc(HB + 2)
    acc = N.alloc(HB + 2)
    cw = N.alloc(4)
    bgb, cgb, xhb, accb, cwb = Buf("bg"), Buf("cg"), Buf("xh"), Buf("acc"), Buf("cw")
    for c in range(2):
        P.dma("sp", cw[:, 0:3], C.consts["conv_wT"][c * 128:(c + 1) * 128, :], [bgb, cgb, accb], [cwb])
        for hf in range(2):
            lo = max(0, hf * HB - 2)
            hi = hf * HB + HB
            n = hi - lo
            halo = hf * HB - lo
            P.dma("sp", bg[:, 0:n], projT[c * 128:(c + 1) * 128, lo:hi], projb[c], [bgb])
            P.dma("sp", cg[:, 0:n], projT[(2 + c) * 128:(3 + c) * 128, lo:hi], projb[2 + c], [cgb])
            P.dma("sp", xh[:, 0:n], projT[(4 + c) * 128:(5 + c) * 128, lo:hi], projb[4 + c], [xhb])
            P.op("dve", [cgb, xhb], [cgb], "tensor_tensor", out=cg[:, 0:n], in0=cg[:, 0:n], in1=xh[:, 0:n], op=ALU.mult)
            P.op("dve", [cgb, cwb], [accb], "tensor_scalar", out=acc[:, 0:n], in0=cg[:, 0:n], scalar1=cw[:, 2:3],
                 scalar2=None, op0=ALU.mult)
            P.op("dve", [cgb, cwb, accb], [accb], "scalar_tensor_tensor", out=acc[:, 1:n], in0=cg[:, 0:n - 1],
                 scalar=cw[:, 1:2], in1=acc[:, 1:n], op0=ALU.mult, op1=ALU.add)
            P.op("dve", [cgb, cwb, accb], [accb], "scalar_tensor_tensor", out=acc[:, 2:n], in0=cg[:, 0:n - 2],
                 scalar=cw[:, 0:1], in1=acc[:, 2:n], op0=ALU.mult, op1=ALU.add)
            P.op("dve", [bgb, accb], [accb], "tensor_tensor", out=acc[:, 0:n], in0=acc[:, 0:n], in1=bg[:, 0:n], op=ALU.mult)
            P.dma("pool", yT[c * 128:c * 128 + 64, hf * HB:hi], acc[0:64, halo:n], [accb], [yTb[2 * c]])
            P.dma("pool", yT[c * 128 + 64:c * 128 + 128, hf * HB:hi], acc[64:128, halo:n], [accb], [yTb[2 * c + 1]])
    P.barrier()
    A.reset()
    C.N.reset()
    qt = [A.alloc(S, 64) for i in range(2)]
    kt = [A.alloc(S, 64) for i in range(2)]
    N = C.N
    vt1 = N.alloc(S, 64)
    vt = [vt1, vt1]
    vtb = Buf("v")
    qkvb = [[Buf("q%d" % i), Buf("k%d" % i), vtb] for i in range(2)]
    acc = N.alloc(S, 65)
    accb = Buf("acc")
    mask = N.alloc(256)
    maskb = Buf("mask")
    sel = N.alloc(64, 65)
    selb = Buf("sel")
    P.dma("sp", mask, C.consts["maskT"], [], [maskb])
    P.dma("sp", sel, C.consts["sel"], [], [selb])
    sms = [N.alloc(256) for i in range(3)]
    smbs = [Buf("sm%d" % i) for i in range(3)]
    pts = [A.alloc(256) for i in range(3)]
    ptbs = [Buf("pt%d" % i) for i in range(3)]
    vxs = [A.alloc(65) for i in range(3)]
    vxbs = [Buf("vx%d" % i) for i in range(3)]
    for i in range(3):
        P.op("dve", [C.oneb], [vxbs[i]], "tensor_copy", out=r32(vxs[i][:, 64:65]), in_=C.one[:, 0:1])
    rds = [N.alloc(512, 64) for i in range(2)]
    rdbs = [Buf("rd%d" % i) for i in range(2)]
    yos = [N.alloc(512, 64) for i in range(2)]
    yobs = [Buf("yo%d" % i) for i in range(2)]
    st = {"blk": 0, "vxc": 0, "grp": 0}
    nrm = 0
    for hd in range(12):
        hi = hd % 2
        q, kk, v = qt[hi], kt[hi], vt[hi]
        qb, kb, vb = qkvb[hi]
        cq, rq = divmod(768 + hd * 64, 128)
        ck, rk = divmod(1536 + hd * 64, 128)
        cv, rv = divmod(2304 + hd * 64, 128)
        P.dma("sp", r32(q), r32(projT[768 + hd * 64:768 + (hd + 1) * 64, :]), projb[cq], [qb])
        P.dma("sp", r32(kk), r32(projT[1536 + hd * 64:1536 + (hd + 1) * 64, :]), projb[ck], [kb])
        P.dma("sp", v, projT[2304 + hd * 64:2304 + (hd + 1) * 64, :], projb[cv], [vb])
        blocks = []
        for bi, d in enumerate((1, 4, 16)):
            nb = 32 // d
            for r in range(d):
                for n in range(nb):
                    blocks.append({"bi": bi, "d": d, "r": r, "n": n, "nb": nb, "G": min(4, nb)})

        def stage_a(B):
            d, r, n = B["d"], B["r"], B["n"]
            st0 = r + d * 128 * n
            cur = slice(st0, min(st0 + d * 128, S), d)
            blk = st["blk"]
            st["blk"] += 1
            sbank = blk % 3
            sm, smb = sms[blk % 3], smbs[blk % 3]
            pt, ptb = pts[blk % 3], ptbs[blk % 3]
            ncol = 256 if n > 0 else 128
            P.op("pe", [kb, qb], [C.psb[sbank]], "matmul", C.ps[sbank][:, 0:128],
                 lhsT=r32(kk[:, cur]), rhs=r32(q[:, cur]), start=True, stop=True)
            if n > 0:
                sp0 = r + d * 128 * (n - 1)
                prv = slice(sp0, min(sp0 + d * 128, S), d)
                P.op("pe", [kb, qb], [C.psb[sbank]], "matmul", C.ps[sbank][:, 128:256],
                     lhsT=r32(kk[:, prv]), rhs=r32(q[:, cur]), start=True, stop=True)
            P.op("dve", [C.psb[sbank], maskb], [smb], "tensor_tensor", out=sm[:, 0:ncol],
                 in0=C.ps[sbank][:, 0:ncol], in1=mask[:, 0:ncol], op=ALU.add)
            P.op("act", [smb], [ptb], "activation", out=r32(pt[:, 0:ncol]), in_=sm[:, 0:ncol],
                 func=AF.Exp, scale=0.125)
            vxc = st["vxc"]
            st["vxc"] += 1
            vx, vxb = vxs[vxc % 3], vxbs[vxc % 3]
            tb = 3 + (vxc % 2)
            P.op("pe", [vb, C.identb], [C.psb[tb]], "transpose", out=C.ps[tb][:, 0:64],
                 in_=v[:, cur], identity=C.ident[0:64, 0:64])
            P.op("dve", [C.psb[tb]], [vxb], "tensor_copy", out=r32(vx[:, 0:64]), in_=C.ps[tb][:, 0:64])
            B.update(pt=pt, ptb=ptb, vx=vx, vxb=vxb, st0=st0)

        def stage_b(B, Bprev):
            d, n, G, bi = B["d"], B["n"], B["G"], B["bi"]
            gi = n % G
            if gi == 0:
                st["gbank"] = 5 + (st["grp"] % 2)
                st["grp"] += 1
                st["g_start"] = B["st0"]
            gbank = st["gbank"]
            osl = C.ps[gbank][0:65, gi * 128:(gi + 1) * 128]
            P.op("pe", [B["vxb"], B["ptb"]], [C.psb[gbank]], "matmul", osl, lhsT=r32(B["vx"][:, 0:65]),
                 rhs=r32(B["pt"][:, 0:128]), start=True, stop=(n == 0))
            if n > 0:
                P.op("pe", [Bprev["vxb"], B["ptb"]], [C.psb[gbank]], "matmul", osl, lhsT=r32(Bprev["vx"][:, 0:65]),
                     rhs=r32(B["pt"][:, 128:256]), start=False, stop=True)
            if gi == G - 1:
                g_start = st["g_start"]
                gs = slice(g_start, min(g_start + d * 128 * G, S), d)
                if bi == 0:
                    P.op("act", [C.psb[gbank]], [accb], "copy", out=acc[:, gs], in_=C.ps[gbank][0:65, 0:128 * G])
                else:
                    P.op("dve", [C.psb[gbank], accb], [accb], "tensor_tensor", out=acc[:, gs], in0=acc[:, gs],
                         in1=C.ps[gbank][0:65, 0:128 * G], op=ALU.add)

        for i in range(len(blocks) + 1):
            if i < len(blocks):
                stage_a(blocks[i])
            if i >= 1:
                stage_b(blocks[i - 1], blocks[i - 2] if i >= 2 else None)
        for cb in range(8):
            cs = slice(cb * 512, (cb + 1) * 512)
            rd, rdb = rds[nrm % 2], rdbs[nrm % 2]
            yo, yob = yos[nrm % 2], yobs[nrm % 2]
            nrm += 1
            P.op("pe", [accb, selb], [C.psb[7]], "matmul", C.ps[7][0:64, :], lhsT=sel, rhs=acc[:, cs],
                 start=True, stop=True)
            P.op("dve", [C.psb[7]], [rdb], "reciprocal", out=rd, in_=C.ps[7][0:64, :])
            P.op("dve", [rdb, accb], [yob], "tensor_tensor", out=yo, in0=acc[0:64, cs], in1=rd, op=ALU.mult)
            P.dma("pool", yT[256 + hd * 64:256 + (hd + 1) * 64, cs], yo, [yob], [yTb[4 + hd]])
    out_proj_norm(C, yT, yTb, prm["l0_mix_w_out"], prm["l0_ln2_g"], prm["l0_ln2_b"], x_in, x_in_bufs, x_out, x_out_bufs)


def odd_mixer(C, x_in, x_in_bufs, x_out, x_out_bufs, prm, scr):
    P = C.P
    A = C.A
    N = C.N
    w_in = prm["l1_mix_w_in"]
    projT, projb = scr["projT"], scr["projb"]
    yT, yTb = scr["yT"], scr["yTb"]
    ccols = [i * 128 for i in range(16)] + [2048] + [2056 + i * 128 for i in range(12)]
    proj_phase(C, x_in, x_in_bufs, w_in, ccols, 16, projT, projb)
    P.barrier()
    A.reset()
    N.reset()
    triu = N.alloc(128)
    ones = N.alloc(128)
    zer = N.alloc(130)
    cb_ = Buf("mconst")
    P.dma("sp", triu, C.consts["triu"], [], [cb_])
    P.op("dve", [], [cb_], "memset", ones, 1.0)
    P.op("dve", [], [cb_], "memset", zer, 0.0)
    ng = N.alloc(512)
    ngb = Buf("ng")
    P.dma("sp", ng, prm["l1_mlstm_norm_g"].partition_broadcast(128), [], [ngb])
    biasb = N.alloc(8)
    biasbb = Buf("biasb")
    P.dma("sp", biasb[:, 0:4], prm["l1_mlstm_b_i"].partition_broadcast(128), [], [biasbb])
    P.dma("sp", biasb[:, 4:8], prm["l1_mlstm_b_f"].partition_broadcast(128), [], [biasbb])
    grow = N.alloc(S, 8)
    growb = Buf("grow")
    P.dma("sp", grow, projT[2048:2056, :], projb[16], [growb])
    gcol = N.alloc(256)
    nlf = N.alloc(256)
    tmp = N.alloc(128)
    egs = N.alloc(128)
    ea = N.alloc(128)
    eb = N.alloc(128)
    gb_ = Buf("gcol")
    for b in range(32):
        P.op("pe", [growb, C.identb], [C.psb[0]], "transpose", out=C.ps[0][:, b * 8:(b + 1) * 8],
             in_=grow[:, b * 128:(b + 1) * 128], identity=C.ident[0:8, 0:8])
    for b in range(32):
        P.op("dve", [C.psb[0], biasbb], [gb_], "tensor_tensor", out=gcol[:, b * 8:(b + 1) * 8],
             in0=C.ps[0][:, b * 8:(b + 1) * 8], in1=biasb, op=ALU.add)
    P.op("act", [gb_], [gb_], "activation", out=nlf, in_=gcol, func=AF.Exp, scale=-1.0)
    P.op("act", [gb_], [gb_], "activation", out=nlf, in_=nlf, func=AF.Ln, bias=1.0)
    P.op("pe", [gb_, cb_], [C.psb[1]], "matmul", C.ps[1][:, 0:256], lhsT=triu, rhs=nlf, start=True, stop=True)
    P.op("pe", [gb_, cb_], [C.psb[2]], "matmul", C.ps[2][:, 0:256], lhsT=ones, rhs=nlf, start=True, stop=True)
    g3 = gcol.rearrange("p (b j) -> p b j", j=8)
    cs3 = C.ps[1][:, 0:256].rearrange("p (b j) -> p b j", j=8)
    tot3 = C.ps[2][:, 0:256].rearrange("p (b j) -> p b j", j=8)
    v3 = lambda ap: ap.rearrange("p (b j) -> p b j", j=4)
    P.op("dve", [gb_, C.psb[1]], [gb_], "tensor_tensor", out=v3(tmp), in0=g3[:, :, 0:4], in1=cs3[:, :, 4:8], op=ALU.add)
    P.op("act", [gb_], [gb_], "activation", out=tmp, in_=tmp, func=AF.Exp)
    P.op("dve", [gb_], [gb_], "tensor_scalar", out=egs, in0=tmp, scalar1=float(128 ** -0.5), scalar2=None, op0=ALU.mult)
    P.op("act", [C.psb[1]], [gb_], "activation", out=v3(ea), in_=cs3[:, :, 4:8], func=AF.Exp, scale=-1.0)
    P.op("act", [C.psb[2]], [gb_], "activation", out=v3(eb), in_=tot3[:, :, 4:8], func=AF.Exp, scale=-1.0)
    cext = [A.alloc(130) for h in range(4)]
    cextb = [Buf("cext%d" % h) for h in range(4)]
    for h in range(4):
        P.op("dve", [cb_], [cextb[h]], "tensor_copy", out=r32(cext[h]), in_=zer)
    vexts = [A.alloc(130) for i in range(2)]
    vextbs = [Buf("vext%d" % i) for i in range(2)]
    for i in range(2):
        P.op("dve", [C.oneb], [vextbs[i]], "tensor_copy", out=r32(vexts[i][:, 128:130]), in_=C.one[:, 0:2])
    kgs = [A.alloc(128) for i in range(2)]
    kgbs = [Buf("kg%d" % i) for i in range(2)]
    wts = [A.alloc(128) for i in range(2)]
    wtbs = [Buf("wt%d" % i) for i in range(2)]
    q4s = [A.alloc3(4, 512) for i in range(2)]
    k4s = [A.alloc3(4, 512) for i in range(2)]
    qkbs = [[Buf("q4%d" % i), Buf("k4%d" % i)] for i in range(2)]
    v4 = N.alloc3(4, 512)
    o4 = N.alloc3(4, 512)
    v4b, o4b = Buf("v4"), Buf("o4")
    ymT = N.alloc3(4, 512)
    ymTb = Buf("ymT")
    scs = [N.alloc(16) for i in range(2)]
    scbs = [Buf("sc%d" % i) for i in range(2)]
    hts = [N.alloc(128) for i in range(2)]
    htbs = [Buf("ht%d" % i) for i in range(2)]
    sgos = [N.alloc(128) for i in range(2)]
    sgobs = [Buf("sgo%d" % i) for i in range(2)]
    hv = lambda r0: projT[r0:r0 + 512, :].rearrange("(h p) n -> p h n", p=128)
    pj = lambda c0: [b for ci in range(c0, c0 + 4) for b in projb[ci]]
    for t in range(8):
        ts_ = slice(t * 512, (t + 1) * 512)
        q4, k4 = q4s[t % 2], k4s[t % 2]
        q4b, k4b = qkbs[t % 2]
        P.dma("sp", r32(q4), r32(hv(0)[:, :, ts_]), pj(0), [q4b])
        P.dma("sp", r32(k4), r32(hv(512)[:, :, ts_]), pj(4), [k4b])
        P.dma("sp", v4, hv(1024)[:, :, ts_], pj(8), [v4b])
        P.dma("sp", o4, hv(1536)[:, :, ts_], pj(12), [o4b])
        for cc in range(4):
            c = t * 4 + cc
            cl = slice(cc * 128, (cc + 1) * 128)
            for h in range(4):
                i2 = h % 2
                ch = c * 4 + h
                vext, vextb = vexts[i2], vextbs[i2]
                kg, kgb = kgs[i2], kgbs[i2]
                wt, wtb = wts[i2], wtbs[i2]
                sc, scb = scs[i2], scbs[i2]
                ht, htb = hts[i2], htbs[i2]
                sgo, sgob = sgos[i2], sgobs[i2]
                egs_c = egs[:, ch:ch + 1]
                ea_c = ea[:, ch:ch + 1]
                eb_c = eb[:, ch:ch + 1]
                P.op("pe", [v4b, C.identb], [C.psb[0]], "transpose", out=C.ps[0][:, 0:128], in_=v4[:, h, cl],
                     identity=C.ident[:, :])
                P.op("act", [C.psb[0]], [vextb], "copy", out=r32(vext[:, 0:128]), in_=C.ps[0][:, 0:128])
                P.op("pe", [k4b, C.identb], [C.psb[1]], "transpose", out=C.ps[1][:, 0:128], in_=k4[:, h, cl],
                     identity=C.ident[:, :])
                P.op("dve", [C.psb[1], gb_], [kgb], "tensor_scalar", out=r32(kg), in0=C.ps[1][:, 0:128], scalar1=egs_c,
                     scalar2=None, op0=ALU.mult)
                P.op("pe", [k4b, q4b], [C.psb[2]], "matmul", C.ps[2][:, 0:128], lhsT=r32(k4[:, h, cl]),
                     rhs=r32(q4[:, h, cl]), start=True, stop=True)
                P.op("dve", [C.psb[2], gb_, cb_], [wtb], "scalar_tensor_tensor", out=r32(wt), in0=C.ps[2][:, 0:128],
                     scalar=egs_c, in1=triu, op0=ALU.mult, op1=ALU.mult)
                P.op("pe", [q4b, cextb[h]], [C.psb[3]], "matmul", C.ps[3][:, 0:130], lhsT=r32(q4[:, h, cl]),
                     rhs=r32(cext[h][:, 0:130]), start=True, stop=False)
                P.op("pe", [wtb, vextb], [C.psb[3]], "matmul", C.ps[3][:, 0:130], lhsT=r32(wt), rhs=r32(vext),
                     start=False, stop=True)
                P.op("dve", [C.psb[3], gb_], [scb], "tensor_scalar", out=sc[:, 0:1], in0=C.ps[3][:, 128:129],
                     scalar1=ea_c, scalar2=None, op0=ALU.mult)
                P.op("dve", [scb], [scb], "tensor_scalar", out=sc[:, 3:4], in0=sc[:, 0:1], scalar1=-1.0, scalar2=1.0,
                     op0=ALU.mult, op1=ALU.max)
                P.op("dve", [scb], [scb], "tensor_scalar", out=sc[:, 0:1], in0=sc[:, 0:1], scalar1=1.0, scalar2=None,
                     op0=ALU.max)
                P.op("dve", [scb], [scb], "tensor_tensor", out=sc[:, 0:1], in0=sc[:, 0:1], in1=sc[:, 3:4], op=ALU.max)
                P.op("dve", [scb], [scb], "reciprocal", out=sc[:, 1:2], in_=sc[:, 0:1])
                P.op("dve", [scb, gb_], [scb], "tensor_tensor", out=sc[:, 2:3], in0=sc[:, 1:2], in1=ea_c, op=ALU.mult)
                P.op("dve", [C.psb[3], scb], [htb], "tensor_scalar", out=ht, in0=C.ps[3][:, 0:128], scalar1=sc[:, 2:3],
                     scalar2=None, op0=ALU.mult)
                P.op("pe", [o4b, C.identb], [C.psb[4]], "transpose", out=C.ps[4][:, 0:128], in_=o4[:, h, cl],
                     identity=C.ident[:, :])
                P.op("act", [C.psb[4]], [sgob], "activation", out=sgo, in_=C.ps[4][:, 0:128], func=AF.Sigmoid)
                P.op("dve", [htb, sgob], [htb], "tensor_tensor", out=ht, in0=ht, in1=sgo, op=ALU.mult)
                P.op("dve", [htb], [scb], "bn_stats", out=sc[:, 4:10], in_=ht)
                P.op("dve", [scb], [scb], "bn_aggr", out=sc[:, 10:12], in_=sc[:, 4:10])
                P.op("dve", [scb], [scb], "tensor_scalar", out=sc[:, 12:13], in0=sc[:, 11:12], scalar1=LN_EPS,
                     scalar2=None, op0=ALU.add)
                P.op("pool", [scb, C.mhalfb], [scb], "tensor_tensor", out=sc[:, 13:14], in0=sc[:, 12:13],
                     in1=C.mhalf[:, 0:1], op=ALU.pow)
                P.op("dve", [htb, scb], [htb], "tensor_scalar", out=ht, in0=ht, scalar1=sc[:, 10:11],
                     scalar2=sc[:, 13:14], op0=ALU.subtract, op1=ALU.mult)
                P.op("dve", [htb, ngb], [htb], "tensor_tensor", out=ht, in0=ht, in1=ng[:, h * 128:(h + 1) * 128],
                     op=ALU.mult)
                P.op("pe", [htb, C.identb], [C.psb[5]], "transpose", out=C.ps[5][:, 0:128], in_=ht,
                     identity=C.ident[:, :])
                P.op("act", [C.psb[5]], [ymTb], "copy", out=ymT[:, h, cl], in_=C.ps[5][:, 0:128])
                P.op("pe", [kgb, vextb], [C.psb[6]], "matmul", C.ps[6][:, 0:130], lhsT=r32(kg), rhs=r32(vext),
                     start=True, stop=True)
                P.op("dve", [cextb[h], gb_], [cextb[h]], "tensor_scalar", out=r32(cext[h]), in0=cext[h], scalar1=eb_c,
                     scalar2=None, op0=ALU.mult)
                P.op("dve", [C.psb[6], cextb[h], gb_], [cextb[h]], "scalar_tensor_tensor", out=r32(cext[h]),
                     in0=C.ps[6][:, 0:130], scalar=eb_c, in1=cext[h], op0=ALU.mult, op1=ALU.add)
        for h in range(4):
            for hh in range(2):
                P.dma("pool", yT[h * 128 + hh * 64:h * 128 + (hh + 1) * 64, ts_], ymT[hh * 64:(hh + 1) * 64, h, :],
                      [ymTb], [yTb[2 * h + hh]])
    P.barrier()
    A.reset()
    N.reset()
    ustr = A.alloc(128)
    onesr = A.alloc(128)
    identr = A.alloc(128)
    scb_ = Buf("sconst")
    P.dma("sp", r32(ustr), r32(C.consts["ustr"]), [], [scb_])
    P.dma("sp", r32(onesr), r32(C.consts["ones128"]), [], [scb_])
    P.dma("sp", r32(identr), r32(C.consts["ident"]), [], [scb_])
    maskd = N.alloc3(4, 512)
    maskdb = Buf("maskd")
    P.dma("sp", maskd, C.consts["maskd"].rearrange("p (i q) -> p i q", i=4), [], [maskdb])
    zer = N.alloc(128)
    zerb = Buf("zer")
    P.op("dve", [], [zerb], "memset", zer, 0.0)
    qTs = [A.alloc(S, 64) for i in range(2)]
    kTs = [A.alloc(S, 64) for i in range(2)]
    vTs = [N.alloc(S, 64) for i in range(2)]
    hbufs = [[Buf("sq%d" % i), Buf("sk%d" % i), Buf("sv%d" % i)] for i in range(2)]
    vtoks = [A.alloc3(32, 128) for i in range(2)]
    vtokbs = [Buf("vtok%d" % i) for i in range(2)]
    for j in range(2):
        for kb in range(32):
            P.op("dve", [zerb], [vtokbs[j]], "tensor_copy", out=r32(vtoks[j][:, kb, 64:128]), in_=zer[:, 0:64])
    e_ = N.alloc(1024)
    eb_ = Buf("e")
    tts = [N.alloc(1024) for i in range(2)]
    ttbs = [Buf("tt%d" % i) for i in range(2)]
    nLs = [A.alloc(1024) for i in range(2)]
    nLbs = [Buf("nL%d" % i) for i in range(2)]
    Ats = [A.alloc(1024) for i in range(2)]
    Atbs = [Buf("At%d" % i) for i in range(2)]
    Rs = [A.alloc(512) for i in range(2)]
    Rbs = [Buf("R%d" % i) for i in range(2)]
    yos = [N.alloc(512, 64) for i in range(2)]
    yobs = [Buf("syo%d" % i) for i in range(2)]
    z2 = [C.psall[:, 0:1024], C.psall[:, 1024:2048]]
    z2b = [[C.psb[0], C.psb[1]], [C.psb[2], C.psb[3]]]
    nc2 = C.psall[:, 2048:3072]
    nc2b = [C.psb[4], C.psb[5]]
    steps = []
    for h in range(8):
        for Q in range(8):
            for G2 in range(2 * Q + 1, -1, -1):
                steps.append({"h": h, "Q": Q, "G2": G2, "first": G2 == 2 * Q + 1, "last": G2 == 0})
    cnt = {"r": 0, "y": 0}

    def prologue(h):
        hi = h % 2
        qT, kT, vT = qTs[hi], kTs[hi], vTs[hi]
        qTb, kTb, vTb = hbufs[hi]
        vtok, vtokb = vtoks[hi], vtokbs[hi]
        rq, rk, rv = 2056 + h * 64, 2568 + h * 64, 3080 + h * 64
        cidx = lambda r0: projb[17 + (r0 - 2056) // 128]
        P.dma("sp", r32(qT), r32(projT[rq:rq + 64, :]), cidx(rq), [qTb])
        P.dma("sp", r32(kT), r32(projT[rk:rk + 64, :]), cidx(rk), [kTb])
        P.dma("sp", vT, projT[rv:rv + 64, :], cidx(rv), [vTb])
        for rd in range(4):
            for i in range(8):
                kb = rd * 8 + i
                P.op("pe", [vTb, C.identb], [C.psb[6]], "transpose", out=C.ps[6][:, i * 64:(i + 1) * 64],
                     in_=vT[:, kb * 128:(kb + 1) * 128], identity=C.ident[0:64, 0:64])
            P.op("dve", [C.psb[6]], [vtokb], "tensor_copy", out=r32(vtok[:, rd * 8:(rd + 1) * 8, 0:64]),
                 in_=C.ps[6][:, :].rearrange("p (i e) -> p i e", e=64))

    def stage_a(k):
        St = steps[k]
        h, Q, G2 = St["h"], St["Q"], St["G2"]
        if Q == 0 and G2 == 1:
            prologue(h)
        hi = h % 2
        qT, kT = qTs[hi], kTs[hi]
        qTb, kTb, _ = hbufs[hi]
        qs = qT[:, Q * 512:(Q + 1) * 512]
        kbs = [2 * G2 + 1, 2 * G2]
        zi = k % 2
        z, zb = z2[zi], z2b[zi]
        nL, nLb = nLs[zi], nLbs[zi]
        for i, kb in enumerate(kbs):
            P.op("pe", [kTb, qTb], [zb[i]], "matmul", z[:, i * 512:(i + 1) * 512],
                 lhsT=r32(kT[:, kb * 128:(kb + 1) * 128]), rhs=r32(qs), start=True, stop=True)
        P.op("act", zb, [eb_], "activation", out=e_, in_=z, func=AF.Exp, scale=0.125)
        P.op("act", [eb_], [nLb], "activation", out=r32(nL), in_=e_, func=AF.Ln, bias=1.0)
        if G2 >= 2 * Q:
            for i, kb in enumerate(kbs):
                ib = kb - 4 * Q
                P.op("dve", [nLb, maskdb], [nLb], "tensor_tensor", out=r32(nL[:, i * 512:(i + 1) * 512]),
                     in0=nL[:, i * 512:(i + 1) * 512], in1=maskd[:, ib, :], op=ALU.mult)

    def stage_b(k):
        St = steps[k]
        h, Q, G2, first, last = St["h"], St["Q"], St["G2"], St["first"], St["last"]
        kbs = [2 * G2 + 1, 2 * G2]
        zi = k % 2
        z, zb = z2[zi], z2b[zi]
        nL, nLb = nLs[zi], nLbs[zi]
        At, Atb = Ats[zi], Atbs[zi]
        tt, ttb = tts[zi], ttbs[zi]
        if first:
            Rcur, Rcurb = None, None
        else:
            Rcur, Rcurb = steps[k - 1]["Rn"]
        if not last:
            P.op("pe", [nLb, scb_], [C.psb[6]], "matmul", C.ps[6][:, :], lhsT=r32(onesr), rhs=r32(nL[:, 0:512]),
                 start=True, stop=False)
            P.op("pe", [nLb, scb_], [C.psb[6]], "matmul", C.ps[6][:, :], lhsT=r32(onesr),
                 rhs=r32(nL[:, 512:1024]), start=False, stop=first)
            if not first:
                P.op("pe", [Rcurb, scb_], [C.psb[6]], "matmul", C.ps[6][:, :], lhsT=r32(identr), rhs=r32(Rcur),
                     start=False, stop=True)
            Rn, Rnb = Rs[cnt["r"] % 2], Rbs[cnt["r"] % 2]
            cnt["r"] += 1
            P.op("act", [C.psb[6]], [Rnb], "copy", out=r32(Rn), in_=C.ps[6][:, :])
            St["Rn"] = (Rn, Rnb)
        P.op("pe", [nLb, scb_], [nc2b[0]], "matmul", nc2[:, 0:512], lhsT=r32(ustr), rhs=r32(nL[:, 0:512]),
             start=True, stop=first)
        if not first:
            P.op("pe", [Rcurb, scb_], [nc2b[0]], "matmul", nc2[:, 0:512], lhsT=r32(identr), rhs=r32(Rcur),
                 start=False, stop=True)
        P.op("pe", [nLb, scb_], [nc2b[1]], "matmul", nc2[:, 512:1024], lhsT=r32(ustr),
             rhs=r32(nL[:, 512:1024]), start=True, stop=False)
        P.op("pe", [nLb, scb_], [nc2b[1]], "matmul", nc2[:, 512:1024], lhsT=r32(onesr),
             rhs=r32(nL[:, 0:512]), start=False, stop=first)
        if not first:
            P.op("pe", [Rcurb, scb_], [nc2b[1]], "matmul", nc2[:, 512:1024], lhsT=r32(identr), rhs=r32(Rcur),
                 start=False, stop=True)
        P.op("dve", zb + [nLb], [ttb], "scalar_tensor_tensor", out=tt, in0=z, scalar=0.125, in1=nL,
             op0=ALU.mult, op1=ALU.subtract)
        P.op("dve", [ttb] + nc2b, [ttb], "tensor_tensor", out=tt, in0=tt, in1=nc2, op=ALU.subtract)
        P.op("act", [ttb], [Atb], "activation", out=r32(At), in_=tt, func=AF.Exp)
        if G2 >= 2 * Q:
            for i, kb in enumerate(kbs):
                ib = kb - 4 * Q
                P.op("dve", [Atb, maskdb], [Atb], "tensor_tensor", out=r32(At[:, i * 512:(i + 1) * 512]),
                     in0=At[:, i * 512:(i + 1) * 512], in1=maskd[:, ib, :], op=ALU.mult)

    def stage_c(k):
        St = steps[k]
        h, Q, G2, first, last = St["h"], St["Q"], St["G2"], St["first"], St["last"]
        kbs = [2 * G2 + 1, 2 * G2]
        zi = k % 2
        At, Atb = Ats[zi], Atbs[zi]
        vtok, vtokb = vtoks[h % 2], vtokbs[h % 2]
        for i, kb in enumerate(kbs):
            P.op("pe", [vtokb, Atb], [C.psb[7]], "matmul", C.ps[7][:, :], lhsT=r32(vtok[:, kb, :]),
                 rhs=r32(At[:, i * 512:(i + 1) * 512]), start=(first and i == 0), stop=(last and i == 1))
        if last:
            yo, yob = yos[cnt["y"] % 2], yobs[cnt["y"] % 2]
            cnt["y"] += 1
            P.op("act", [C.psb[7]], [yob], "copy", out=yo, in_=C.ps[7][0:64, :])
            P.dma("pool", yT[512 + h * 64:512 + (h + 1) * 64, Q * 512:(Q + 1) * 512], yo, [yob], [yTb[8 + h]])

    ns = len(steps)
    for it in range(ns + 2):
        if it < ns:
            stage_a(it)
        if 1 <= it <= ns:
            stage_b(it - 1)
        if it >= 2:
            stage_c(it - 2)
    out_proj_norm(C, yT, yTb, prm["l1_mix_w_out"], prm["l1_ln2_g"], prm["l1_ln2_b"], x_in, x_in_bufs, x_out, x_out_bufs)


PARAM_NAMES = [
    "l0_ffn1_w_in", "l0_ffn1_w_out", "l0_ln1_g", "l0_ln1_b",
    "l0_mix_w_in", "l0_conv_w", "l0_mix_w_out", "l0_ln2_g", "l0_ln2_b",
    "l0_ffn2_w_in", "l0_ffn2_w_out", "l0_ln3_g", "l0_ln3_b",
    "l1_ffn1_w_in", "l1_ffn1_w_out", "l1_ln1_g", "l1_ln1_b",
    "l1_mix_w_in", "l1_mlstm_b_i", "l1_mlstm_b_f", "l1_mlstm_norm_g", "l1_mix_w_out",
    "l1_ln2_g", "l1_ln2_b",
    "l1_ffn2_w_in", "l1_ffn2_w_out", "l1_ln3_g", "l1_ln3_b",
]
PARAM_SHAPES = {
    "ffn1_w_in": [D, 2 * DFF], "ffn2_w_in": [D, 2 * DFF], "ffn1_w_out": [DFF, D], "ffn2_w_out": [DFF, D],
    "l0_mix_w_in": [D, 3072], "l1_mix_w_in": [D, 3592], "mix_w_out": [D, D], "l0_conv_w": [3, 256],
    "l1_mlstm_b_i": [4], "l1_mlstm_b_f": [4], "l1_mlstm_norm_g": [512],
}


def pshape(n):
    if n in PARAM_SHAPES:
        return PARAM_SHAPES[n]
    k = n[3:]
    if k in PARAM_SHAPES:
        return PARAM_SHAPES[k]
    return [D]


def build(nsub=6):
    nc = bass.Bass("TRN2", target_bir_lowering=False)
    nc.dge_precook = False
    x_d = nc.dram_tensor("x", [S, D], F32, kind="ExternalInput").ap()
    prm = {n: nc.dram_tensor(n, pshape(n), F32, kind="ExternalInput").ap() for n in PARAM_NAMES}
    consts = {n: nc.dram_tensor(n, list(a.shape), F32, kind="ExternalInput").ap() for n, a in CONSTS.items()}
    consts["conv_wT"] = nc.dram_tensor("conv_wT", [256, 3], F32, kind="ExternalInput").ap()
    out_d = nc.dram_tensor("out", [S, D], F32, kind="ExternalOutput").ap()
    skind = "ExternalOutput" if DEBUG_OUT else "Internal"
    xa = nc.dram_tensor("xa", [S, D], F32, kind=skind).ap()
    xb = nc.dram_tensor("xb", [S, D], F32, kind=skind).ap()
    projT = nc.dram_tensor("projT", [3712, S], F32, kind="Internal").ap()
    yT = nc.dram_tensor("yT", [D, S], F32, kind=skind).ap()
    with ExitStack() as stack:
        P = Prog(nc, stack)
        C = setup_common(P, nc, consts)

        def mkbufs(name, n):
            sem = P.new_sem("d_" + name, True)
            return [Buf("%s%d" % (name, i), sem) for i in range(n)]
        bufs = {"x": mkbufs("x", 32), "xa": mkbufs("xa", 32), "xb": mkbufs("xb", 32), "out": mkbufs("out", 32)}
        aps = {"x": x_d, "xa": xa, "xb": xb, "out": out_d}
        pb = mkbufs("projT", 29 * 8)
        scr = {"projT": projT, "projb": [[pb[c * 8 + t] for t in range(8)] for c in range(29)],
               "yT": yT, "yTb": mkbufs("yT", 16)}
        plan = ["ffn1", "mix", "ffn2"] * 2
        cur = "x"
        for si in range(nsub):
            layer = si // 3
            kind = plan[si]
            nxt = "out" if si == nsub - 1 else ("xa" if cur != "xa" else "xb")
            p = "l%d_" % layer
            if kind in ("ffn1", "ffn2"):
                lnn = "ln1" if kind == "ffn1" else "ln3"
                ffn_sublayer(C, aps[cur], bufs[cur], aps[nxt], bufs[nxt],
                             prm[p + kind + "_w_in"], prm[p + kind + "_w_out"], prm[p + lnn + "_g"], prm[p + lnn + "_b"])
            elif layer == 0:
                even_mixer(C, aps[cur], bufs[cur], aps[nxt], bufs[nxt], prm, scr)
            else:
                odd_mixer(C, aps[cur], bufs[cur], aps[nxt], bufs[nxt], prm, scr)
            cur = nxt
        P.final_wait("pool", bufs["out"])
        P.emit()
    return nc


def _mk_consts():
    c = {}
    c["ident"] = np.eye(128, dtype=np.float32)
    k = np.arange(128)[:, None]
    q = np.arange(128)[None, :]
    m = np.zeros((128, 256), np.float32)
    m[:, 0:128] = np.where(k <= q, 0.0, NEGM)
    m[:, 128:256] = np.where(k >= q, 0.0, NEGM)
    c["maskT"] = m
    sel = np.zeros((65, 64), np.float32)
    sel[64, :] = 1.0
    c["sel"] = sel
    a = np.arange(128)
    c["triu"] = (a[:, None] <= a[None, :]).astype(np.float32)
    c["ustr"] = (a[:, None] > a[None, :]).astype(np.float32)
    c["ones128"] = np.ones((128, 128), np.float32)
    kk = (np.arange(4)[None, :, None] * 128 + a[:, None, None])
    qq = np.arange(512)[None, None, :]
    c["maskd"] = (kk < qq).astype(np.float32).reshape(128, 2048)
    return c


CONSTS = _mk_consts()


def run(inputs, nsub=6, trace=False):
    nc = build(nsub)
    x = np.ascontiguousarray(inputs["x"], dtype=np.float32)
    in_maps = []
    for c in range(NCORES):
        m = {"x": x[c]}
        m.update(CONSTS)
        m["conv_wT"] = np.ascontiguousarray(np.asarray(inputs["l0_conv_w"], dtype=np.float32).T)
        for n in PARAM_NAMES:
            m[n] = np.ascontiguousarray(inputs[n], dtype=np.float32)
        in_maps.append(m)
    res = run_bass_kernel_spmd(nc, in_maps, core_ids=list(range(NCORES)), trace=trace)
    out = np.stack([res.results[c]["out"] for c in range(NCORES)], axis=0)
    if DEBUG_OUT:
        global LAST_DEBUG
        LAST_DEBUG = {n: res.results[0][n] for n in ("xa", "xb", "yT") if n in res.results[0]}
    return out, res


def kernel(**inputs):
    out, _ = run(inputs, 6)
    return out.astype(np.float32)
```

```python
import numpy as np
from contextlib import ExitStack
import concourse.bass as bass
import concourse.mybir as mybir
from concourse.bass_utils import run_bass_kernel_spmd

F32 = mybir.dt.float32
F32R = mybir.dt.float32r
AF = mybir.ActivationFunctionType
ALU = mybir.AluOpType
AX = mybir.AxisListType

D = 1024
S = 4096
DFF = 2816
NCH = DFF // 128
ALPHA = 4 ** 0.25
LN_EPS = 1e-5
NCORES = 8
DEBUG_OUT = False


class Buf:
    __slots__ = ("name", "w", "r", "dsem")

    def __init__(self, name, dsem=None):
        self.name = name
        self.w = {}
        self.r = {}
        self.dsem = dsem


class Eng:
    def __init__(self, name, sem):
        self.name = name
        self.sem = sem
        self.waited = {}
        self.insts = []


class SemC:
    def __init__(self, handle, is_dma):
        self.h = handle
        self.count = 0
        self.is_dma = is_dma


class Prog:
    def __init__(self, nc, stack):
        self.nc = nc
        self.stack = stack
        self.nsem = 0
        self.dma_sems = []
        self.free_sems = []
        self.phase_sems = []
        self.eng = {}
        for n in ("pe", "act", "dve", "pool", "sp"):
            self.eng[n] = Eng(n, self.new_sem(n, False))

    def new_sem(self, name, is_dma):
        self.nsem += 1
        h = self.stack.enter_context(self.nc.semaphore("s_%s_%d" % (name, self.nsem)))
        sc = SemC(h, is_dma)
        if is_dma:
            self.dma_sems.append(sc)
        return sc

    def sbuf(self, name, shape, dtype=F32):
        return self.stack.enter_context(self.nc.sbuf_tensor("sb_" + name, shape, dtype))

    def psum(self, name, shape, dtype=F32):
        return self.stack.enter_context(self.nc.psum_tensor("pt_" + name, shape, dtype))

    def _deps(self, e, reads, writes):
        need = {}

        def add(d):
            for s, v in d.items():
                if s.is_dma:
                    v = s.count
                if need.get(s, 0) < v:
                    need[s] = v
        for b in reads:
            add(b.w)
        for b in writes:
            add(b.w)
            add(b.r)
        waits = []
        for s, v in need.items():
            if s is e.sem and e.name == "pe":
                continue
            if e.waited.get(s, 0) >= v:
                continue
            e.waited[s] = v
            waits.append((s.h, v))
        return waits

    def op(self, en, reads, writes, meth, *args, **kwargs):
        e = self.eng[en]
        fn = (lambda h, meth=meth, args=args, kwargs=kwargs: getattr(h, meth)(*args, **kwargs))
        waits = self._deps(e, reads, writes)
        e.sem.count += 1
        v = e.sem.count
        e.insts.append((waits, fn, e.sem.h, 1))
        for b in reads:
            b.r[e.sem] = v
        for b in writes:
            b.w[e.sem] = v

    def dma(self, en, out, in_, reads, writes):
        e = self.eng[en]
        assert len(writes) == 1
        dst = writes[0]
        if dst.dsem is None:
            if self.free_sems:
                dst.dsem = self.free_sems.pop()
            else:
                dst.dsem = self.new_sem("d_" + dst.name, True)
            self.phase_sems.append(dst.dsem)
        waits = self._deps(e, reads, writes)
        dst.dsem.count += 16
        v = dst.dsem.count
        e.insts.append((waits, lambda h: h.dma_start(out=out, in_=in_), dst.dsem.h, 16))
        for b in reads:
            b.r[dst.dsem] = v
        dst.w[dst.dsem] = v

    def barrier(self):
        allsems = [e.sem for e in self.eng.values()] + self.dma_sems
        for e in self.eng.values():
            waits = []
            for sc in allsems:
                if sc is e.sem or sc.count == 0:
                    continue
                if e.waited.get(sc, 0) >= sc.count:
                    continue
                e.waited[sc] = sc.count
                waits.append((sc.h, sc.count))
            if waits:
                e.insts.append((waits, None, None, 0))
        self.free_sems.extend(self.phase_sems)
        self.phase_sems = []

    def final_wait(self, en, bufs):
        e = self.eng[en]
        waits = self._deps(e, bufs, ())
        e.insts.append((waits, None, None, 0))

    def emit(self):
        nc = self.nc
        with nc.Block() as block:
            def mk(e):
                def body(h):
                    for waits, fn, sem, inc in e.insts:
                        for sh, v in waits:
                            h.wait_ge(sh, v)
                        if fn is not None:
                            fn(h).then_inc(sem, inc)
                return body
            block.tensor(mk(self.eng["pe"]))
            block.scalar(mk(self.eng["act"]))
            block.vector(mk(self.eng["dve"]))
            block.gpsimd(mk(self.eng["pool"]))
            block.sync(mk(self.eng["sp"]))


def r32(ap):
    return ap.bitcast(F32R)


class Ctx:
    pass


class Arena:
    def __init__(self, P, nelem, name="arena"):
        self.t = P.sbuf(name, [128, nelem])
        self.n = nelem
        self.off = 0

    def reset(self):
        self.off = 0

    def alloc(self, n, parts=128):
        n += n % 2
        assert self.off + n <= self.n, (self.off, n, self.n)
        ap = self.t[0:parts, self.off:self.off + n]
        self.off += n
        return ap

    def alloc3(self, k, n, parts=128):
        return self.alloc(k * n, parts).rearrange("p (k n) -> p k n", k=k)


def setup_common(P, nc, consts):
    C = Ctx()
    C.P = P
    C.nc = nc
    C.psall = P.psum("psall", [128, 4096])
    C.ps = [C.psall[:, i * 512:(i + 1) * 512] for i in range(8)]
    C.psb = [Buf("ps%d" % i) for i in range(8)]
    C.ident = P.sbuf("ident", [128, 128])
    C.identb = Buf("ident")
    P.dma("sp", C.ident[:, :], consts["ident"], [], [C.identb])
    C.mhalf = P.sbuf("mhalf", [128, 1])
    C.mhalfb = Buf("mhalf")
    P.op("dve", [], [C.mhalfb], "memset", C.mhalf[:, :], -0.5)
    C.one = P.sbuf("one", [128, 64])
    C.oneb = Buf("one")
    P.op("dve", [], [C.oneb], "memset", C.one[:, :], 1.0)
    C.A = Arena(P, 30720, "arenaR")
    C.N = Arena(P, 15600, "arenaN")
    C.consts = consts
    return C


def load_gb(C, g_d, b_d, gt, bt, gb_b):
    P = C.P
    P.dma("sp", gt, g_d.partition_broadcast(128), [], [gb_b[0]])
    P.dma("sp", bt, b_d.partition_broadcast(128), [], [gb_b[1]])


class Work:
    pass


def alloc_norm(C, W):
    A = C.N
    W.z = [A.alloc(D) for i in range(4)]
    W.zb = [Buf("z%d" % i) for i in range(4)]
    W.st = [A.alloc(16) for i in range(4)]
    W.stb = [Buf("st%d" % i) for i in range(4)]
    W.gt = A.alloc(D)
    W.bt = A.alloc(D)
    W.gbb = [Buf("gt"), Buf("bt")]
    W.zcnt = 0


def post_norm_evac(C, W, y_banks, xs, xsb):
    P = C.P
    zi = W.zcnt
    W.zcnt += 1
    z = W.z[zi % 4]
    zb = W.zb[zi % 4]
    for hh in range(2):
        yb = y_banks[hh]
        P.op("dve", [xsb, C.psb[yb]], [zb], "scalar_tensor_tensor",
             out=z[:, hh * 512:(hh + 1) * 512], in0=xs[:, hh * 512:(hh + 1) * 512], scalar=ALPHA,
             in1=C.ps[yb][:, :], op0=ALU.mult, op1=ALU.add)
    return zi


def post_norm_finish(C, W, zi, x_out_rows, x_out_buf):
    P = C.P
    z = W.z[zi % 4]
    zb = W.zb[zi % 4]
    st = W.st[zi % 4]
    stb = W.stb[zi % 4]
    gt, bt, gb_b = W.gt, W.bt, W.gbb
    for hh in range(2):
        P.op("dve", [zb], [stb], "bn_stats", out=st[:, hh * 6:(hh + 1) * 6], in_=z[:, hh * 512:(hh + 1) * 512])
    P.op("dve", [stb], [stb], "bn_aggr", out=st[:, 12:14], in_=st[:, 0:12])
    P.op("dve", [stb], [stb], "tensor_scalar", out=st[:, 14:15], in0=st[:, 13:14], scalar1=LN_EPS, scalar2=None,
         op0=ALU.add)
    P.op("pool", [stb, C.mhalfb], [stb], "tensor_tensor", out=st[:, 15:16], in0=st[:, 14:15], in1=C.mhalf[:, 0:1],
         op=ALU.pow)
    P.op("dve", [zb, stb], [zb], "tensor_scalar", out=z, in0=z, scalar1=st[:, 12:13],
         scalar2=st[:, 15:16], op0=ALU.subtract, op1=ALU.mult)
    P.op("dve", [zb, gb_b[0]], [zb], "tensor_tensor", out=z, in0=z, in1=gt, op=ALU.mult)
    P.op("dve", [zb, gb_b[1]], [zb], "tensor_tensor", out=z, in0=z, in1=bt, op=ALU.add)
    P.dma("pool", x_out_rows, z, [zb], [x_out_buf])


def post_norm_tile(C, W, y_banks, xs, xsb, x_out_rows, x_out_buf):
    zi = post_norm_evac(C, W, y_banks, xs, xsb)
    post_norm_finish(C, W, zi, x_out_rows, x_out_buf)


def alloc_xT(C, W, nbuf=2):
    A = C.A
    W.xs = [C.N.alloc(D) for i in range(4)]
    W.xsb = [Buf("xs%d" % i) for i in range(4)]
    W.nxT = nbuf
    W.xT = [A.alloc3(8, 512) for i in range(nbuf)]
    W.xTb = [[Buf("xT%d_%d" % (i, k)) for k in range(8)] for i in range(nbuf)]
    W.xscnt = 0
    W.xTcnt = 0


def load_xT(C, W, x_in, x_in_bufs, t):
    P = C.P
    xTi = W.xTcnt % W.nxT
    W.xTcnt += 1
    xT = W.xT[xTi]
    xTb = W.xTb[xTi]
    xsl = []
    for s in range(4):
        i = W.xscnt % 4
        W.xscnt += 1
        r0 = t * 512 + s * 128
        P.dma("sp", W.xs[i], x_in[r0:r0 + 128, :], [x_in_bufs[t * 4 + s]], [W.xsb[i]])
        xsl.append(i)
    for kc in range(8):
        bank = kc % 2
        for s in range(4):
            i = xsl[s]
            P.op("pe", [W.xsb[i], C.identb], [C.psb[bank]], "transpose",
                 out=C.ps[bank][:, s * 128:(s + 1) * 128], in_=W.xs[i][:, kc * 128:(kc + 1) * 128],
                 identity=C.ident[:, :])
        if kc % 2 == 0:
            P.op("act", [C.psb[bank]], [xTb[kc]], "copy", out=r32(xT[:, kc, :]), in_=C.ps[bank][:, :])
        else:
            P.op("dve", [C.psb[bank]], [xTb[kc]], "tensor_copy", out=r32(xT[:, kc, :]), in_=C.ps[bank][:, :])
    return xT, xTb


def reload_xs(C, W, x_in, x_in_bufs, row_tile):
    P = C.P
    i = W.xscnt % 4
    W.xscnt += 1
    r0 = row_tile * 128
    P.dma("sp", W.xs[i], x_in[r0:r0 + 128, :], [x_in_bufs[row_tile]], [W.xsb[i]])
    return W.xs[i], W.xsb[i]


def ffn_sublayer(C, x_in, x_in_bufs, x_out, x_out_bufs, w_in, w_out, g_d, b_d):
    P = C.P
    A = C.A
    P.barrier()
    A.reset()
    C.N.reset()
    W = Work()
    alloc_norm(C, W)
    xT = A.alloc3(8, 512)
    xTb = [Buf("xT_%d" % k) for k in range(8)]
    xss = [[C.N.alloc(D) for i in range(4)] for j in range(2)]
    xssb = [[Buf("xs%d_%d" % (j, i)) for i in range(4)] for j in range(2)]

    def issue_loads(t):
        for s_ in range(4):
            r0 = t * 512 + s_ * 128
            P.dma("sp", xss[t % 2][s_], x_in[r0:r0 + 128, :], [x_in_bufs[t * 4 + s_]], [xssb[t % 2][s_]])
    aT = A.alloc3(NCH, 512)
    aTb = [Buf("aT%d" % j) for j in range(NCH)]
    wins = [A.alloc(8 * 2 * 256).rearrange("p (k h n) -> p k h n", k=8, h=2) for i in range(3)]
    winbs = [Buf("win%d" % i) for i in range(3)]
    wouts = [A.alloc(D) for i in range(3)]
    woutbs = [Buf("wout%d" % i) for i in range(3)]
    sgs = [C.N.alloc(512) for i in range(2)]
    sgbs = [Buf("sg%d" % i) for i in range(2)]
    cnt = {"win": 0, "wout": 0, "sg": 0}
    load_gb(C, g_d, b_d, W.gt, W.bt, W.gbb)
    w_in_v = w_in.rearrange("(kc p) n -> p kc n", p=128)
    NT = S // 512
    issue_loads(0)
    for t in range(NT):
        for kc in range(8):
            bank = kc % 2
            for s_ in range(4):
                P.op("pe", [xssb[t % 2][s_], C.identb], [C.psb[bank]], "transpose",
                     out=C.ps[bank][:, s_ * 128:(s_ + 1) * 128], in_=xss[t % 2][s_][:, kc * 128:(kc + 1) * 128],
                     identity=C.ident[:, :])
            P.op("act", [C.psb[bank]], [xTb[kc]], "copy", out=r32(xT[:, kc, :]), in_=C.ps[bank][:, :])
        for grp in range(NCH // 2):
            wi = cnt["win"] % 3
            cnt["win"] += 1
            win = wins[wi]
            winb = winbs[wi]
            P.dma("sp", r32(win[:, :, 0, :]), r32(w_in_v[:, :, grp * 256:(grp + 1) * 256]), [], [winb])
            P.dma("sp", r32(win[:, :, 1, :]), r32(w_in_v[:, :, DFF + grp * 256:DFF + (grp + 1) * 256]), [], [winb])
            for jj in range(2):
                j = grp * 2 + jj
                gb_ = 2 + 2 * (j % 2)
                ub_ = 3 + 2 * (j % 2)
                for half, bank in ((0, gb_), (1, ub_)):
                    for kc in range(8):
                        P.op("pe", [winb, xTb[kc]], [C.psb[bank]], "matmul",
                             C.ps[bank][:, :], lhsT=r32(win[:, kc, half, jj * 128:(jj + 1) * 128]),
                             rhs=r32(xT[:, kc, :]), start=(kc == 0), stop=(kc == 7))
                si = cnt["sg"] % 2
                cnt["sg"] += 1
                sg = sgs[si]
                P.op("act", [C.psb[gb_]], [sgbs[si]], "activation", out=sg, in_=C.ps[gb_][:, :], func=AF.Silu)
                P.op("dve", [sgbs[si], C.psb[ub_]], [aTb[j]], "scalar_tensor_tensor",
                     out=r32(aT[:, j, :]), in0=sg, scalar=0.5, in1=C.ps[ub_][:, :], op0=ALU.mult, op1=ALU.mult)
        if t + 1 < NT:
            issue_loads(t + 1)
        for j in range(NCH):
            wi = cnt["wout"] % 3
            cnt["wout"] += 1
            wout = wouts[wi]
            woutb = woutbs[wi]
            P.dma("sp", r32(wout), r32(w_out[j * 128:(j + 1) * 128, :]), [], [woutb])
            for s in range(4):
                for hh in range(2):
                    bank = 2 * s + hh
                    P.op("pe", [aTb[j], woutb], [C.psb[bank]], "matmul",
                         C.ps[bank][:, :], lhsT=r32(aT[:, j, s * 128:(s + 1) * 128]),
                         rhs=r32(wout[:, hh * 512:(hh + 1) * 512]), start=(j == 0), stop=(j == NCH - 1))
        zis = []
        for s in range(4):
            rt = t * 4 + s
            zis.append(post_norm_evac(C, W, (2 * s, 2 * s + 1), xss[t % 2][s], xssb[t % 2][s]))
        for s in range(4):
            rt = t * 4 + s
            post_norm_finish(C, W, zis[s], x_out[rt * 128:(rt + 1) * 128, :], x_out_bufs[rt])


def out_proj_norm(C, yT_d, yT_bufs, w_out, g_d, b_d, x_in, x_in_bufs, x_out, x_out_bufs):
    P = C.P
    A = C.A
    P.barrier()
    A.reset()
    C.N.reset()
    W = Work()
    alloc_norm(C, W)
    W.xs = [C.N.alloc(D) for i in range(4)]
    W.xsb = [Buf("xs%d" % i) for i in range(4)]
    W.xscnt = 0
    wo = A.alloc3(8, D)
    wob = Buf("wo")
    P.dma("sp", r32(wo), r32(w_out.rearrange("(kc p) n -> p kc n", p=128)), [], [wob])
    yts = [A.alloc3(8, 512) for i in range(2)]
    ytbs = [Buf("yt%d" % i) for i in range(2)]
    load_gb(C, g_d, b_d, W.gt, W.bt, W.gbb)
    yT_v = yT_d.rearrange("(kc p) n -> p kc n", p=128)
    for t in range(S // 512):
        yt = yts[t % 2]
        ytb = ytbs[t % 2]
        P.dma("sp", r32(yt), r32(yT_v[:, :, t * 512:(t + 1) * 512]), yT_bufs, [ytb])
        for s in range(4):
            rt = t * 4 + s
            banks = (2 * (rt % 4), 2 * (rt % 4) + 1)
            for hh in range(2):
                for kc in range(8):
                    P.op("pe", [ytb, wob], [C.psb[banks[hh]]], "matmul", C.ps[banks[hh]][:, :],
                         lhsT=r32(yt[:, kc, s * 128:(s + 1) * 128]), rhs=r32(wo[:, kc, hh * 512:(hh + 1) * 512]),
                         start=(kc == 0), stop=(kc == 7))
            xs, xsb = reload_xs(C, W, x_in, x_in_bufs, rt)
            post_norm_tile(C, W, banks, xs, xsb, x_out[rt * 128:(rt + 1) * 128, :], x_out_bufs[rt])


def proj_phase(C, x_in, x_in_bufs, w_in, ccols, small_ci, projT, projb):
    P = C.P
    A = C.A
    N = C.N
    P.barrier()
    A.reset()
    N.reset()
    xs = [N.alloc(D) for i in range(4)]
    xsb = [Buf("xs%d" % i) for i in range(4)]
    evs = [N.alloc(512) for i in range(4)]
    evbs = [Buf("ev%d" % i) for i in range(4)]
    HT = 2048
    xT = A.alloc3(8, HT)
    xTb = [[Buf("xTb%d_%d" % (tt, kc)) for kc in range(8)] for tt in range(4)]
    wcs = [A.alloc3(8, 128) for i in range(3)]
    wcbs = [Buf("wc%d" % i) for i in range(3)]
    w_in_v = w_in.rearrange("(kc p) n -> p kc n", p=128)
    xc = 0
    k = 0
    wk = 0
    for half in range(2):
        for tt in range(4):
            t = half * 4 + tt
            xsl = []
            for s_ in range(4):
                i = xc % 4
                xc += 1
                r0 = t * 512 + s_ * 128
                P.dma("sp", xs[i], x_in[r0:r0 + 128, :], [x_in_bufs[t * 4 + s_]], [xsb[i]])
                xsl.append(i)
            for kc in range(8):
                bank = kc % 2
                for s_ in range(4):
                    i = xsl[s_]
                    P.op("pe", [xsb[i], C.identb], [C.psb[bank]], "transpose",
                         out=C.ps[bank][:, s_ * 128:(s_ + 1) * 128], in_=xs[i][:, kc * 128:(kc + 1) * 128],
                         identity=C.ident[:, :])
                dst = r32(xT[:, kc, tt * 512:(tt + 1) * 512])
                if kc % 2 == 0:
                    P.op("act", [C.psb[bank]], [xTb[tt][kc]], "copy", out=dst, in_=C.ps[bank][:, :])
                else:
                    P.op("dve", [C.psb[bank]], [xTb[tt][kc]], "tensor_copy", out=dst, in_=C.ps[bank][:, :])
        for ci, c0 in enumerate(ccols):
            nrow = 8 if ci == small_ci else 128
            wc = wcs[wk % 3]
            wcb = wcbs[wk % 3]
            wk += 1
            P.dma("sp", r32(wc), r32(w_in_v[:, :, c0:c0 + 128]), [], [wcb])
            for tt in range(4):
                t = half * 4 + tt
                ev = evs[k % 4]
                evb = evbs[k % 4]
                bank = 2 + (k % 4)
                for kc in range(8):
                    P.op("pe", [wcb, xTb[tt][kc]], [C.psb[bank]], "matmul", C.ps[bank][:, :],
                         lhsT=r32(wc[:, kc, :]), rhs=r32(xT[:, kc, tt * 512:(tt + 1) * 512]),
                         start=(kc == 0), stop=(kc == 7))
                if k % 2 == 0:
                    P.op("act", [C.psb[bank]], [evb], "copy", out=ev, in_=C.ps[bank][:, :])
                else:
                    P.op("dve", [C.psb[bank]], [evb], "tensor_copy", out=ev, in_=C.ps[bank][:, :])
                P.dma("pool", projT[c0:c0 + nrow, t * 512:(t + 1) * 512], ev[0:nrow, :], [evb], [projb[ci][t]])
                k += 1


NEGM = -240000.0


def even_mixer(C, x_in, x_in_bufs, x_out, x_out_bufs, prm, scr):
    P = C.P
    A = C.A
    w_in = prm["l0_mix_w_in"]
    projT, projb = scr["projT"], scr["projb"]
    yT, yTb = scr["yT"], scr["yTb"]
    proj_phase(C, x_in, x_in_bufs, w_in, [c * 128 for c in range(24)], -1, projT, projb)
    P.barrier()
    A.reset()
    C.N.reset()
    N = C.N
    HB = S // 2
    bg = N.alloc(HB + 2)
    cg = N.alloc(HB + 2)
    xh = N.alloc(HB + 2)
    acc = N.alloc(HB + 2)
    cw = N.alloc(4)
    bgb, cgb, xhb, accb, cwb = Buf("bg"), Buf("cg"), Buf("xh"), Buf("acc"), Buf("cw")
    for c in range(2):
        P.dma("sp", cw[:, 0:3], C.consts["conv_wT"][c * 128:(c + 1) * 128, :], [bgb, cgb, accb], [cwb])
        for hf in range(2):
            lo = max(0, hf * HB - 2)
            hi = hf * HB + HB
            n = hi - lo
            halo = hf * HB - lo
            P.dma("sp", bg[:, 0:n], projT[c * 128:(c + 1) * 128, lo:hi], projb[c], [bgb])
            P.dma("sp", cg[:, 0:n], projT[(2 + c) * 128:(3 + c) * 128, lo:hi], projb[2 + c], [cgb])
            P.dma("sp", xh[:, 0:n], projT[(4 + c) * 128:(5 + c) * 128, lo:hi], projb[4 + c], [xhb])
            P.op("dve", [cgb, xhb], [cgb], "tensor_tensor", out=cg[:, 0:n], in0=cg[:, 0:n], in1=xh[:, 0:n], op=ALU.mult)
            P.op("dve", [cgb, cwb], [accb], "tensor_scalar", out=acc[:, 0:n], in0=cg[:, 0:n], scalar1=cw[:, 2:3],
                 scalar2=None, op0=ALU.mult)
            P.op("dve", [cgb, cwb, accb], [accb], "scalar_tensor_tensor", out=acc[:, 1:n], in0=cg[:, 0:n - 1],
                 scalar=cw[:, 1:2], in1=acc[:, 1:n], op0=ALU.mult, op1=ALU.add)
            P.op("dve", [cgb, cwb, accb], [accb], "scalar_tensor_tensor", out=acc[:, 2:n], in0=cg[:, 0:n - 2],
                 scalar=cw[:, 0:1], in1=acc[:, 2:n], op0=ALU.mult, op1=ALU.add)
            P.op("dve", [bgb, accb], [accb], "tensor_tensor", out=acc[:, 0:n], in0=acc[:, 0:n], in1=bg[:, 0:n], op=ALU.mult)
            P.dma("pool", yT[c * 128:c * 128 + 64, hf * HB:hi], acc[0:64, halo:n], [accb], [yTb[2 * c]])
            P.dma("pool", yT[c * 128 + 64:c * 128 + 128, hf * HB:hi], acc[64:128, halo:n], [accb], [yTb[2 * c + 1]])
    P.barrier()
    A.reset()
    C.N.reset()
    qt = [A.alloc(S, 64) for i in range(2)]
    kt = [A.alloc(S, 64) for i in range(2)]
    N = C.N
    vt1 = N.alloc(S, 64)
    vt = [vt1, vt1]
    vtb = Buf("v")
    qkvb = [[Buf("q%d" % i), Buf("k%d" % i), vtb] for i in range(2)]
    acc = N.alloc(S, 65)
    accb = Buf("acc")
    mask = N.alloc(256)
    maskb = Buf("mask")
    sel = N.alloc(64, 65)
    selb = Buf("sel")
    P.dma("sp", mask, C.consts["maskT"], [], [maskb])
    P.dma("sp", sel, C.consts["sel"], [], [selb])
    sms = [N.alloc(256) for i in range(3)]
    smbs = [Buf("sm%d" % i) for i in range(3)]
    pts = [A.alloc(256) for i in range(3)]
    ptbs = [Buf("pt%d" % i) for i in range(3)]
    vxs = [A.alloc(65) for i in range(3)]
    vxbs = [Buf("vx%d" % i) for i in range(3)]
    for i in range(3):
        P.op("dve", [C.oneb], [vxbs[i]], "tensor_copy", out=r32(vxs[i][:, 64:65]), in_=C.one[:, 0:1])
    rds = [N.alloc(512, 64) for i in range(2)]
    rdbs = [Buf("rd%d" % i) for i in range(2)]
    yos = [N.alloc(512, 64) for i in range(2)]
    yobs = [Buf("yo%d" % i) for i in range(2)]
    st = {"blk": 0, "vxc": 0, "grp": 0}
    nrm = 0
    for hd in range(12):
        hi = hd % 2
        q, kk, v = qt[hi], kt[hi], vt[hi]
        qb, kb, vb = qkvb[hi]
        cq, rq = divmod(768 + hd * 64, 128)
        ck, rk = divmod(1536 + hd * 64, 128)
        cv, rv = divmod(2304 + hd * 64, 128)
        P.dma("sp", r32(q), r32(projT[768 + hd * 64:768 + (hd + 1) * 64, :]), projb[cq], [qb])
        P.dma("sp", r32(kk), r32(projT[1536 + hd * 64:1536 + (hd + 1) * 64, :]), projb[ck], [kb])
        P.dma("sp", v, projT[2304 + hd * 64:2304 + (hd + 1) * 64, :], projb[cv], [vb])
        blocks = []
        for bi, d in enumerate((1, 4, 16)):
            nb = 32 // d
            for r in range(d):
                for n in range(nb):
                    blocks.append({"bi": bi, "d": d, "r": r, "n": n, "nb": nb, "G": min(4, nb)})

        def stage_a(B):
            d, r, n = B["d"], B["r"], B["n"]
            st0 = r + d * 128 * n
            cur = slice(st0, min(st0 + d * 128, S), d)
            blk = st["blk"]
            st["blk"] += 1
            sbank = blk % 3
            sm, smb = sms[blk % 3], smbs[blk % 3]
            pt, ptb = pts[blk % 3], ptbs[blk % 3]
            ncol = 256 if n > 0 else 128
            P.op("pe", [kb, qb], [C.psb[sbank]], "matmul", C.ps[sbank][:, 0:128],
                 lhsT=r32(kk[:, cur]), rhs=r32(q[:, cur]), start=True, stop=True)
            if n > 0:
                sp0 = r + d * 128 * (n - 1)
                prv = slice(sp0, min(sp0 + d * 128, S), d)
                P.op("pe", [kb, qb], [C.psb[sbank]], "matmul", C.ps[sbank][:, 128:256],
                     lhsT=r32(kk[:, prv]), rhs=r32(q[:, cur]), start=True, stop=True)
            P.op("dve", [C.psb[sbank], maskb], [smb], "tensor_tensor", out=sm[:, 0:ncol],
                 in0=C.ps[sbank][:, 0:ncol], in1=mask[:, 0:ncol], op=ALU.add)
            P.op("act", [smb], [ptb], "activation", out=r32(pt[:, 0:ncol]), in_=sm[:, 0:ncol],
                 func=AF.Exp, scale=0.125)
            vxc = st["vxc"]
            st["vxc"] += 1
            vx, vxb = vxs[vxc % 3], vxbs[vxc % 3]
            tb = 3 + (vxc % 2)
            P.op("pe", [vb, C.identb], [C.psb[tb]], "transpose", out=C.ps[tb][:, 0:64],
                 in_=v[:, cur], identity=C.ident[0:64, 0:64])
            P.op("dve", [C.psb[tb]], [vxb], "tensor_copy", out=r32(vx[:, 0:64]), in_=C.ps[tb][:, 0:64])
            B.update(pt=pt, ptb=ptb, vx=vx, vxb=vxb, st0=st0)

        def stage_b(B, Bprev):
            d, n, G, bi = B["d"], B["n"], B["G"], B["bi"]
            gi = n % G
            if gi == 0:
                st["gbank"] = 5 + (st["grp"] % 2)
                st["grp"] += 1
                st["g_start"] = B["st0"]
            gbank = st["gbank"]
            osl = C.ps[gbank][0:65, gi * 128:(gi + 1) * 128]
            P.op("pe", [B["vxb"], B["ptb"]], [C.psb[gbank]], "matmul", osl, lhsT=r32(B["vx"][:, 0:65]),
                 rhs=r32(B["pt"][:, 0:128]), start=True, stop=(n == 0))
            if n > 0:
                P.op("pe", [Bprev["vxb"], B["ptb"]], [C.psb[gbank]], "matmul", osl, lhsT=r32(Bprev["vx"][:, 0:65]),
                     rhs=r32(B["pt"][:, 128:256]), start=False, stop=True)
            if gi == G - 1:
                g_start = st["g_start"]
                gs = slice(g_start, min(g_start + d * 128 * G, S), d)
                if bi == 0:
                    P.op("act", [C.psb[gbank]], [accb], "copy", out=acc[:, gs], in_=C.ps[gbank][0:65, 0:128 * G])
                else:
                    P.op("dve", [C.psb[gbank], accb], [accb], "tensor_tensor", out=acc[:, gs], in0=acc[:, gs],
                         in1=C.ps[gbank][0:65, 0:128 * G], op=ALU.add)

        for i in range(len(blocks) + 1):
            if i < len(blocks):
                stage_a(blocks[i])
            if i >= 1:
                stage_b(blocks[i - 1], blocks[i - 2] if i >= 2 else None)
        for cb in range(8):
            cs = slice(cb * 512, (cb + 1) * 512)
            rd, rdb = rds[nrm % 2], rdbs[nrm % 2]
            yo, yob = yos[nrm % 2], yobs[nrm % 2]
            nrm += 1
            P.op("pe", [accb, selb], [C.psb[7]], "matmul", C.ps[7][0:64, :], lhsT=sel, rhs=acc[:, cs],
                 start=True, stop=True)
            P.op("dve", [C.psb[7]], [rdb], "reciprocal", out=rd, in_=C.ps[7][0:64, :])
            P.op("dve", [rdb, accb], [yob], "tensor_tensor", out=yo, in0=acc[0:64, cs], in1=rd, op=ALU.mult)
            P.dma("pool", yT[256 + hd * 64:256 + (hd + 1) * 64, cs], yo, [yob], [yTb[4 + hd]])
    out_proj_norm(C, yT, yTb, prm["l0_mix_w_out"], prm["l0_ln2_g"], prm["l0_ln2_b"], x_in, x_in_bufs, x_out, x_out_bufs)


def odd_mixer(C, x_in, x_in_bufs, x_out, x_out_bufs, prm, scr):
    P = C.P
    A = C.A
    N = C.N
    w_in = prm["l1_mix_w_in"]
    projT, projb = scr["projT"], scr["projb"]
    yT, yTb = scr["yT"], scr["yTb"]
    ccols = [i * 128 for i in range(16)] + [2048] + [2056 + i * 128 for i in range(12)]
    proj_phase(C, x_in, x_in_bufs, w_in, ccols, 16, projT, projb)
    P.barrier()
    A.reset()
    N.reset()
    triu = N.alloc(128)
    ones = N.alloc(128)
    zer = N.alloc(130)
    cb_ = Buf("mconst")
    P.dma("sp", triu, C.consts["triu"], [], [cb_])
    P.op("dve", [], [cb_], "memset", ones, 1.0)
    P.op("dve", [], [cb_], "memset", zer, 0.0)
    ng = N.alloc(512)
    ngb = Buf("ng")
    P.dma("sp", ng, prm["l1_mlstm_norm_g"].partition_broadcast(128), [], [ngb])
    biasb = N.alloc(8)
    biasbb = Buf("biasb")
    P.dma("sp", biasb[:, 0:4], prm["l1_mlstm_b_i"].partition_broadcast(128), [], [biasbb])
    P.dma("sp", biasb[:, 4:8], prm["l1_mlstm_b_f"].partition_broadcast(128), [], [biasbb])
    grow = N.alloc(S, 8)
    growb = Buf("grow")
    P.dma("sp", grow, projT[2048:2056, :], projb[16], [growb])
    gcol = N.alloc(256)
    nlf = N.alloc(256)
    tmp = N.alloc(128)
    egs = N.alloc(128)
    ea = N.alloc(128)
    eb = N.alloc(128)
    gb_ = Buf("gcol")
    for b in range(32):
        P.op("pe", [growb, C.identb], [C.psb[0]], "transpose", out=C.ps[0][:, b * 8:(b + 1) * 8],
             in_=grow[:, b * 128:(b + 1) * 128], identity=C.ident[0:8, 0:8])
    for b in range(32):
        P.op("dve", [C.psb[0], biasbb], [gb_], "tensor_tensor", out=gcol[:, b * 8:(b + 1) * 8],
             in0=C.ps[0][:, b * 8:(b + 1) * 8], in1=biasb, op=ALU.add)
    P.op("act", [gb_], [gb_], "activation", out=nlf, in_=gcol, func=AF.Exp, scale=-1.0)
    P.op("act", [gb_], [gb_], "activation", out=nlf, in_=nlf, func=AF.Ln, bias=1.0)
    P.op("pe", [gb_, cb_], [C.psb[1]], "matmul", C.ps[1][:, 0:256], lhsT=triu, rhs=nlf, start=True, stop=True)
    P.op("pe", [gb_, cb_], [C.psb[2]], "matmul", C.ps[2][:, 0:256], lhsT=ones, rhs=nlf, start=True, stop=True)
    g3 = gcol.rearrange("p (b j) -> p b j", j=8)
    cs3 = C.ps[1][:, 0:256].rearrange("p (b j) -> p b j", j=8)
    tot3 = C.ps[2][:, 0:256].rearrange("p (b j) -> p b j", j=8)
    v3 = lambda ap: ap.rearrange("p (b j) -> p b j", j=4)
    P.op("dve", [gb_, C.psb[1]], [gb_], "tensor_tensor", out=v3(tmp), in0=g3[:, :, 0:4], in1=cs3[:, :, 4:8], op=ALU.add)
    P.op("act", [gb_], [gb_], "activation", out=tmp, in_=tmp, func=AF.Exp)
    P.op("dve", [gb_], [gb_], "tensor_scalar", out=egs, in0=tmp, scalar1=float(128 ** -0.5), scalar2=None, op0=ALU.mult)
    P.op("act", [C.psb[1]], [gb_], "activation", out=v3(ea), in_=cs3[:, :, 4:8], func=AF.Exp, scale=-1.0)
    P.op("act", [C.psb[2]], [gb_], "activation", out=v3(eb), in_=tot3[:, :, 4:8], func=AF.Exp, scale=-1.0)
    cext = [A.alloc(130) for h in range(4)]
    cextb = [Buf("cext%d" % h) for h in range(4)]
    for h in range(4):
        P.op("dve", [cb_], [cextb[h]], "tensor_copy", out=r32(cext[h]), in_=zer)
    vexts = [A.alloc(130) for i in range(2)]
    vextbs = [Buf("vext%d" % i) for i in range(2)]
    for i in range(2):
        P.op("dve", [C.oneb], [vextbs[i]], "tensor_copy", out=r32(vexts[i][:, 128:130]), in_=C.one[:, 0:2])
    kgs = [A.alloc(128) for i in range(2)]
    kgbs = [Buf("kg%d" % i) for i in range(2)]
    wts = [A.alloc(128) for i in range(2)]
    wtbs = [Buf("wt%d" % i) for i in range(2)]
    q4s = [A.alloc3(4, 512) for i in range(2)]
    k4s = [A.alloc3(4, 512) for i in range(2)]
    qkbs = [[Buf("q4%d" % i), Buf("k4%d" % i)] for i in range(2)]
    v4 = N.alloc3(4, 512)
    o4 = N.alloc3(4, 512)
    v4b, o4b = Buf("v4"), Buf("o4")
    ymT = N.alloc3(4, 512)
    ymTb = Buf("ymT")
    scs = [N.alloc(16) for i in range(2)]
    scbs = [Buf("sc%d" % i) for i in range(2)]
    hts = [N.alloc(128) for i in range(2)]
    htbs = [Buf("ht%d" % i) for i in range(2)]
    sgos = [N.alloc(128) for i in range(2)]
    sgobs = [Buf("sgo%d" % i) for i in range(2)]
    hv = lambda r0: projT[r0:r0 + 512, :].rearrange("(h p) n -> p h n", p=128)
    pj = lambda c0: [b for ci in range(c0, c0 + 4) for b in projb[ci]]
    for t in range(8):
        ts_ = slice(t * 512, (t + 1) * 512)
        q4, k4 = q4s[t % 2], k4s[t % 2]
        q4b, k4b = qkbs[t % 2]
        P.dma("sp", r32(q4), r32(hv(0)[:, :, ts_]), pj(0), [q4b])
        P.dma("sp", r32(k4), r32(hv(512)[:, :, ts_]), pj(4), [k4b])
        P.dma("sp", v4, hv(1024)[:, :, ts_], pj(8), [v4b])
        P.dma("sp", o4, hv(1536)[:, :, ts_], pj(12), [o4b])
        for cc in range(4):
            c = t * 4 + cc
            cl = slice(cc * 128, (cc + 1) * 128)
            for h in range(4):
                i2 = h % 2
                ch = c * 4 + h
                vext, vextb = vexts[i2], vextbs[i2]
                kg, kgb = kgs[i2], kgbs[i2]
                wt, wtb = wts[i2], wtbs[i2]
                sc, scb = scs[i2], scbs[i2]
                ht, htb = hts[i2], htbs[i2]
                sgo, sgob = sgos[i2], sgobs[i2]
                egs_c = egs[:, ch:ch + 1]
                ea_c = ea[:, ch:ch + 1]
                eb_c = eb[:, ch:ch + 1]
                P.op("pe", [v4b, C.identb], [C.psb[0]], "transpose", out=C.ps[0][:, 0:128], in_=v4[:, h, cl],
                     identity=C.ident[:, :])
                P.op("act", [C.psb[0]], [vextb], "copy", out=r32(vext[:, 0:128]), in_=C.ps[0][:, 0:128])
                P.op("pe", [k4b, C.identb], [C.psb[1]], "transpose", out=C.ps[1][:, 0:128], in_=k4[:, h, cl],
                     identity=C.ident[:, :])
                P.op("dve", [C.psb[1], gb_], [kgb], "tensor_scalar", out=r32(kg), in0=C.ps[1][:, 0:128], scalar1=egs_c,
                     scalar2=None, op0=ALU.mult)
                P.op("pe", [k4b, q4b], [C.psb[2]], "matmul", C.ps[2][:, 0:128], lhsT=r32(k4[:, h, cl]),
                     rhs=r32(q4[:, h, cl]), start=True, stop=True)
                P.op("dve", [C.psb[2], gb_, cb_], [wtb], "scalar_tensor_tensor", out=r32(wt), in0=C.ps[2][:, 0:128],
                     scalar=egs_c, in1=triu, op0=ALU.mult, op1=ALU.mult)
                P.op("pe", [q4b, cextb[h]], [C.psb[3]], "matmul", C.ps[3][:, 0:130], lhsT=r32(q4[:, h, cl]),
                     rhs=r32(cext[h][:, 0:130]), start=True, stop=False)
                P.op("pe", [wtb, vextb], [C.psb[3]], "matmul", C.ps[3][:, 0:130], lhsT=r32(wt), rhs=r32(vext),
                     start=False, stop=True)
                P.op("dve", [C.psb[3], gb_], [scb], "tensor_scalar", out=sc[:, 0:1], in0=C.ps[3][:, 128:129],
                     scalar1=ea_c, scalar2=None, op0=ALU.mult)
                P.op("dve", [scb], [scb], "tensor_scalar", out=sc[:, 3:4], in0=sc[:, 0:1], scalar1=-1.0, scalar2=1.0,
                     op0=ALU.mult, op1=ALU.max)
                P.op("dve", [scb], [scb], "tensor_scalar", out=sc[:, 0:1], in0=sc[:, 0:1], scalar1=1.0, scalar2=None,
                     op0=ALU.max)
                P.op("dve", [scb], [scb], "tensor_tensor", out=sc[:, 0:1], in0=sc[:, 0:1], in1=sc[:, 3:4], op=ALU.max)
                P.op("dve", [scb], [scb], "reciprocal", out=sc[:, 1:2], in_=sc[:, 0:1])
                P.op("dve", [scb, gb_], [scb], "tensor_tensor", out=sc[:, 2:3], in0=sc[:, 1:2], in1=ea_c, op=ALU.mult)
                P.op("dve", [C.psb[3], scb], [htb], "tensor_scalar", out=ht, in0=C.ps[3][:, 0:128], scalar1=sc[:, 2:3],
                     scalar2=None, op0=ALU.mult)
                P.op("pe", [o4b, C.identb], [C.psb[4]], "transpose", out=C.ps[4][:, 0:128], in_=o4[:, h, cl],
                     identity=C.ident[:, :])
                P.op("act", [C.psb[4]], [sgob], "activation", out=sgo, in_=C.ps[4][:, 0:128], func=AF.Sigmoid)
                P.op("dve", [htb, sgob], [htb], "tensor_tensor", out=ht, in0=ht, in1=sgo, op=ALU.mult)
                P.op("dve", [htb], [scb], "bn_stats", out=sc[:, 4:10], in_=ht)
                P.op("dve", [scb], [scb], "bn_aggr", out=sc[:, 10:12], in_=sc[:, 4:10])
                P.op("dve", [scb], [scb], "tensor_scalar", out=sc[:, 12:13], in0=sc[:, 11:12], scalar1=LN_EPS,
                     scalar2=None, op0=ALU.add)
                P.op("pool", [scb, C.mhalfb], [scb], "tensor_tensor", out=sc[:, 13:14], in0=sc[:, 12:13],
                     in1=C.mhalf[:, 0:1], op=ALU.pow)
                P.op("dve", [htb, scb], [htb], "tensor_scalar", out=ht, in0=ht, scalar1=sc[:, 10:11],
                     scalar2=sc[:, 13:14], op0=ALU.subtract, op1=ALU.mult)
                P.op("dve", [htb, ngb], [htb], "tensor_tensor", out=ht, in0=ht, in1=ng[:, h * 128:(h + 1) * 128],
                     op=ALU.mult)
                P.op("pe", [htb, C.identb], [C.psb[5]], "transpose", out=C.ps[5][:, 0:128], in_=ht,
                     identity=C.ident[:, :])
                P.op("act", [C.psb[5]], [ymTb], "copy", out=ymT[:, h, cl], in_=C.ps[5][:, 0:128])
                P.op("pe", [kgb, vextb], [C.psb[6]], "matmul", C.ps[6][:, 0:130], lhsT=r32(kg), rhs=r32(vext),
                     start=True, stop=True)
                P.op("dve", [cextb[h], gb_], [cextb[h]], "tensor_scalar", out=r32(cext[h]), in0=cext[h], scalar1=eb_c,
                     scalar2=None, op0=ALU.mult)
                P.op("dve", [C.psb[6], cextb[h], gb_], [cextb[h]], "scalar_tensor_tensor", out=r32(cext[h]),
                     in0=C.ps[6][:, 0:130], scalar=eb_c, in1=cext[h], op0=ALU.mult, op1=ALU.add)
        for h in range(4):
            for hh in range(2):
                P.dma("pool", yT[h * 128 + hh * 64:h * 128 + (hh + 1) * 64, ts_], ymT[hh * 64:(hh + 1) * 64, h, :],
                      [ymTb], [yTb[2 * h + hh]])
    P.barrier()
    A.reset()
    N.reset()
    ustr = A.alloc(128)
    onesr = A.alloc(128)
    identr = A.alloc(128)
    scb_ = Buf("sconst")
    P.dma("sp", r32(ustr), r32(C.consts["ustr"]), [], [scb_])
    P.dma("sp", r32(onesr), r32(C.consts["ones128"]), [], [scb_])
    P.dma("sp", r32(identr), r32(C.consts["ident"]), [], [scb_])
    maskd = N.alloc3(4, 512)
    maskdb = Buf("maskd")
    P.dma("sp", maskd, C.consts["maskd"].rearrange("p (i q) -> p i q", i=4), [], [maskdb])
    zer = N.alloc(128)
    zerb = Buf("zer")
    P.op("dve", [], [zerb], "memset", zer, 0.0)
    qTs = [A.alloc(S, 64) for i in range(2)]
    kTs = [A.alloc(S, 64) for i in range(2)]
    vTs = [N.alloc(S, 64) for i in range(2)]
    hbufs = [[Buf("sq%d" % i), Buf("sk%d" % i), Buf("sv%d" % i)] for i in range(2)]
    vtoks = [A.alloc3(32, 128) for i in range(2)]
    vtokbs = [Buf("vtok%d" % i) for i in range(2)]
    for j in range(2):
        for kb in range(32):
            P.op("dve", [zerb], [vtokbs[j]], "tensor_copy", out=r32(vtoks[j][:, kb, 64:128]), in_=zer[:, 0:64])
    e_ = N.alloc(1024)
    eb_ = Buf("e")
    tts = [N.alloc(1024) for i in range(2)]
    ttbs = [Buf("tt%d" % i) for i in range(2)]
    nLs = [A.alloc(1024) for i in range(2)]
    nLbs = [Buf("nL%d" % i) for i in range(2)]
    Ats = [A.alloc(1024) for i in range(2)]
    Atbs = [Buf("At%d" % i) for i in range(2)]
    Rs = [A.alloc(512) for i in range(2)]
    Rbs = [Buf("R%d" % i) for i in range(2)]
    yos = [N.alloc(512, 64) for i in range(2)]
    yobs = [Buf("syo%d" % i) for i in range(2)]
    z2 = [C.psall[:, 0:1024], C.psall[:, 1024:2048]]
    z2b = [[C.psb[0], C.psb[1]], [C.psb[2], C.psb[3]]]
    nc2 = C.psall[:, 2048:3072]
    nc2b = [C.psb[4], C.psb[5]]
    steps = []
    for h in range(8):
        for Q in range(8):
            for G2 in range(2 * Q + 1, -1, -1):
                steps.append({"h": h, "Q": Q, "G2": G2, "first": G2 == 2 * Q + 1, "last": G2 == 0})
    cnt = {"r": 0, "y": 0}

    def prologue(h):
        hi = h % 2
        qT, kT, vT = qTs[hi], kTs[hi], vTs[hi]
        qTb, kTb, vTb = hbufs[hi]
        vtok, vtokb = vtoks[hi], vtokbs[hi]
        rq, rk, rv = 2056 + h * 64, 2568 + h * 64, 3080 + h * 64
        cidx = lambda r0: projb[17 + (r0 - 2056) // 128]
        P.dma("sp", r32(qT), r32(projT[rq:rq + 64, :]), cidx(rq), [qTb])
        P.dma("sp", r32(kT), r32(projT[rk:rk + 64, :]), cidx(rk), [kTb])
        P.dma("sp", vT, projT[rv:rv + 64, :], cidx(rv), [vTb])
        for rd in range(4):
            for i in range(8):
                kb = rd * 8 + i
                P.op("pe", [vTb, C.identb], [C.psb[6]], "transpose", out=C.ps[6][:, i * 64:(i + 1) * 64],
                     in_=vT[:, kb * 128:(kb + 1) * 128], identity=C.ident[0:64, 0:64])
            P.op("dve", [C.psb[6]], [vtokb], "tensor_copy", out=r32(vtok[:, rd * 8:(rd + 1) * 8, 0:64]),
                 in_=C.ps[6][:, :].rearrange("p (i e) -> p i e", e=64))

    def stage_a(k):
        St = steps[k]
        h, Q, G2 = St["h"], St["Q"], St["G2"]
        if Q == 0 and G2 == 1:
            prologue(h)
        hi = h % 2
        qT, kT = qTs[hi], kTs[hi]
        qTb, kTb, _ = hbufs[hi]
        qs = qT[:, Q * 512:(Q + 1) * 512]
        kbs = [2 * G2 + 1, 2 * G2]
        zi = k % 2
        z, zb = z2[zi], z2b[zi]
        nL, nLb = nLs[zi], nLbs[zi]
        for i, kb in enumerate(kbs):
            P.op("pe", [kTb, qTb], [zb[i]], "matmul", z[:, i * 512:(i + 1) * 512],
                 lhsT=r32(kT[:, kb * 128:(kb + 1) * 128]), rhs=r32(qs), start=True, stop=True)
        P.op("act", zb, [eb_], "activation", out=e_, in_=z, func=AF.Exp, scale=0.125)
        P.op("act", [eb_], [nLb], "activation", out=r32(nL), in_=e_, func=AF.Ln, bias=1.0)
        if G2 >= 2 * Q:
            for i, kb in enumerate(kbs):
                ib = kb - 4 * Q
                P.op("dve", [nLb, maskdb], [nLb], "tensor_tensor", out=r32(nL[:, i * 512:(i + 1) * 512]),
                     in0=nL[:, i * 512:(i + 1) * 512], in1=maskd[:, ib, :], op=ALU.mult)

    def stage_b(k):
        St = steps[k]
        h, Q, G2, first, last = St["h"], St["Q"], St["G2"], St["first"], St["last"]
        kbs = [2 * G2 + 1, 2 * G2]
        zi = k % 2
        z, zb = z2[zi], z2b[zi]
        nL, nLb = nLs[zi], nLbs[zi]
        At, Atb = Ats[zi], Atbs[zi]
        tt, ttb = tts[zi], ttbs[zi]
        if first:
            Rcur, Rcurb = None, None
        else:
            Rcur, Rcurb = steps[k - 1]["Rn"]
        if not last:
            P.op("pe", [nLb, scb_], [C.psb[6]], "matmul", C.ps[6][:, :], lhsT=r32(onesr), rhs=r32(nL[:, 0:512]),
                 start=True, stop=False)
            P.op("pe", [nLb, scb_], [C.psb[6]], "matmul", C.ps[6][:, :], lhsT=r32(onesr),
                 rhs=r32(nL[:, 512:1024]), start=False, stop=first)
            if not first:
                P.op("pe", [Rcurb, scb_], [C.psb[6]], "matmul", C.ps[6][:, :], lhsT=r32(identr), rhs=r32(Rcur),
                     start=False, stop=True)
            Rn, Rnb = Rs[cnt["r"] % 2], Rbs[cnt["r"] % 2]
            cnt["r"] += 1
            P.op("act", [C.psb[6]], [Rnb], "copy", out=r32(Rn), in_=C.ps[6][:, :])
            St["Rn"] = (Rn, Rnb)
        P.op("pe", [nLb, scb_], [nc2b[0]], "matmul", nc2[:, 0:512], lhsT=r32(ustr), rhs=r32(nL[:, 0:512]),
             start=True, stop=first)
        if not first:
            P.op("pe", [Rcurb, scb_], [nc2b[0]], "matmul", nc2[:, 0:512], lhsT=r32(identr), rhs=r32(Rcur),
                 start=False, stop=True)
        P.op("pe", [nLb, scb_], [nc2b[1]], "matmul", nc2[:, 512:1024], lhsT=r32(ustr),
             rhs=r32(nL[:, 512:1024]), start=True, stop=False)
        P.op("pe", [nLb, scb_], [nc2b[1]], "matmul", nc2[:, 512:1024], lhsT=r32(onesr),
             rhs=r32(nL[:, 0:512]), start=False, stop=first)
        if not first:
            P.op("pe", [Rcurb, scb_], [nc2b[1]], "matmul", nc2[:, 512:1024], lhsT=r32(identr), rhs=r32(Rcur),
                 start=False, stop=True)
        P.op("dve", zb + [nLb], [ttb], "scalar_tensor_tensor", out=tt, in0=z, scalar=0.125, in1=nL,
             op0=ALU.mult, op1=ALU.subtract)
        P.op("dve", [ttb] + nc2b, [ttb], "tensor_tensor", out=tt, in0=tt, in1=nc2, op=ALU.subtract)
        P.op("act", [ttb], [Atb], "activation", out=r32(At), in_=tt, func=AF.Exp)
        if G2 >= 2 * Q:
            for i, kb in enumerate(kbs):
                ib = kb - 4 * Q
                P.op("dve", [Atb, maskdb], [Atb], "tensor_tensor", out=r32(At[:, i * 512:(i + 1) * 512]),
                     in0=At[:, i * 512:(i + 1) * 512], in1=maskd[:, ib, :], op=ALU.mult)

    def stage_c(k):
        St = steps[k]
        h, Q, G2, first, last = St["h"], St["Q"], St["G2"], St["first"], St["last"]
        kbs = [2 * G2 + 1, 2 * G2]
        zi = k % 2
        At, Atb = Ats[zi], Atbs[zi]
        vtok, vtokb = vtoks[h % 2], vtokbs[h % 2]
        for i, kb in enumerate(kbs):
            P.op("pe", [vtokb, Atb], [C.psb[7]], "matmul", C.ps[7][:, :], lhsT=r32(vtok[:, kb, :]),
                 rhs=r32(At[:, i * 512:(i + 1) * 512]), start=(first and i == 0), stop=(last and i == 1))
        if last:
            yo, yob = yos[cnt["y"] % 2], yobs[cnt["y"] % 2]
            cnt["y"] += 1
            P.op("act", [C.psb[7]], [yob], "copy", out=yo, in_=C.ps[7][0:64, :])
            P.dma("pool", yT[512 + h * 64:512 + (h + 1) * 64, Q * 512:(Q + 1) * 512], yo, [yob], [yTb[8 + h]])

    ns = len(steps)
    for it in range(ns + 2):
        if it < ns:
            stage_a(it)
        if 1 <= it <= ns:
            stage_b(it - 1)
        if it >= 2:
            stage_c(it - 2)
    out_proj_norm(C, yT, yTb, prm["l1_mix_w_out"], prm["l1_ln2_g"], prm["l1_ln2_b"], x_in, x_in_bufs, x_out, x_out_bufs)


PARAM_NAMES = [
    "l0_ffn1_w_in", "l0_ffn1_w_out", "l0_ln1_g", "l0_ln1_b",
    "l0_mix_w_in", "l0_conv_w", "l0_mix_w_out", "l0_ln2_g", "l0_ln2_b",
    "l0_ffn2_w_in", "l0_ffn2_w_out", "l0_ln3_g", "l0_ln3_b",
    "l1_ffn1_w_in", "l1_ffn1_w_out", "l1_ln1_g", "l1_ln1_b",
    "l1_mix_w_in", "l1_mlstm_b_i", "l1_mlstm_b_f", "l1_mlstm_norm_g", "l1_mix_w_out",
    "l1_ln2_g", "l1_ln2_b",
    "l1_ffn2_w_in", "l1_ffn2_w_out", "l1_ln3_g", "l1_ln3_b",
]
PARAM_SHAPES = {
    "ffn1_w_in": [D, 2 * DFF], "ffn2_w_in": [D, 2 * DFF], "ffn1_w_out": [DFF, D], "ffn2_w_out": [DFF, D],
    "l0_mix_w_in": [D, 3072], "l1_mix_w_in": [D, 3592], "mix_w_out": [D, D], "l0_conv_w": [3, 256],
    "l1_mlstm_b_i": [4], "l1_mlstm_b_f": [4], "l1_mlstm_norm_g": [512],
}


def pshape(n):
    if n in PARAM_SHAPES:
        return PARAM_SHAPES[n]
    k = n[3:]
    if k in PARAM_SHAPES:
        return PARAM_SHAPES[k]
    return [D]


def build(nsub=6):
    nc = bass.Bass("TRN2", target_bir_lowering=False)
    nc.dge_precook = False
    x_d = nc.dram_tensor("x", [S, D], F32, kind="ExternalInput").ap()
    prm = {n: nc.dram_tensor(n, pshape(n), F32, kind="ExternalInput").ap() for n in PARAM_NAMES}
    consts = {n: nc.dram_tensor(n, list(a.shape), F32, kind="ExternalInput").ap() for n, a in CONSTS.items()}
    consts["conv_wT"] = nc.dram_tensor("conv_wT", [256, 3], F32, kind="ExternalInput").ap()
    out_d = nc.dram_tensor("out", [S, D], F32, kind="ExternalOutput").ap()
    skind = "ExternalOutput" if DEBUG_OUT else "Internal"
    xa = nc.dram_tensor("xa", [S, D], F32, kind=skind).ap()
    xb = nc.dram_tensor("xb", [S, D], F32, kind=skind).ap()
    projT = nc.dram_tensor("projT", [3712, S], F32, kind="Internal").ap()
    yT = nc.dram_tensor("yT", [D, S], F32, kind=skind).ap()
    with ExitStack() as stack:
        P = Prog(nc, stack)
        C = setup_common(P, nc, consts)

        def mkbufs(name, n):
            sem = P.new_sem("d_" + name, True)
            return [Buf("%s%d" % (name, i), sem) for i in range(n)]
        bufs = {"x": mkbufs("x", 32), "xa": mkbufs("xa", 32), "xb": mkbufs("xb", 32), "out": mkbufs("out", 32)}
        aps = {"x": x_d, "xa": xa, "xb": xb, "out": out_d}
        pb = mkbufs("projT", 29 * 8)
        scr = {"projT": projT, "projb": [[pb[c * 8 + t] for t in range(8)] for c in range(29)],
               "yT": yT, "yTb": mkbufs("yT", 16)}
        plan = ["ffn1", "mix", "ffn2"] * 2
        cur = "x"
        for si in range(nsub):
            layer = si // 3
            kind = plan[si]
            nxt = "out" if si == nsub - 1 else ("xa" if cur != "xa" else "xb")
            p = "l%d_" % layer
            if kind in ("ffn1", "ffn2"):
                lnn = "ln1" if kind == "ffn1" else "ln3"
                ffn_sublayer(C, aps[cur], bufs[cur], aps[nxt], bufs[nxt],
                             prm[p + kind + "_w_in"], prm[p + kind + "_w_out"], prm[p + lnn + "_g"], prm[p + lnn + "_b"])
            elif layer == 0:
                even_mixer(C, aps[cur], bufs[cur], aps[nxt], bufs[nxt], prm, scr)
            else:
                odd_mixer(C, aps[cur], bufs[cur], aps[nxt], bufs[nxt], prm, scr)
            cur = nxt
        P.final_wait("pool", bufs["out"])
        P.emit()
    return nc


def _mk_consts():
    c = {}
    c["ident"] = np.eye(128, dtype=np.float32)
    k = np.arange(128)[:, None]
    q = np.arange(128)[None, :]
    m = np.zeros((128, 256), np.float32)
    m[:, 0:128] = np.where(k <= q, 0.0, NEGM)
    m[:, 128:256] = np.where(k >= q, 0.0, NEGM)
    c["maskT"] = m
    sel = np.zeros((65, 64), np.float32)
    sel[64, :] = 1.0
    c["sel"] = sel
    a = np.arange(128)
    c["triu"] = (a[:, None] <= a[None, :]).astype(np.float32)
    c["ustr"] = (a[:, None] > a[None, :]).astype(np.float32)
    c["ones128"] = np.ones((128, 128), np.float32)
    kk = (np.arange(4)[None, :, None] * 128 + a[:, None, None])
    qq = np.arange(512)[None, None, :]
    c["maskd"] = (kk < qq).astype(np.float32).reshape(128, 2048)
    return c


CONSTS = _mk_consts()


def run(inputs, nsub=6, trace=False):
    nc = build(nsub)
    x = np.ascontiguousarray(inputs["x"], dtype=np.float32)
    in_maps = []
    for c in range(NCORES):
        m = {"x": x[c]}
        m.update(CONSTS)
        m["conv_wT"] = np.ascontiguousarray(np.asarray(inputs["l0_conv_w"], dtype=np.float32).T)
        for n in PARAM_NAMES:
            m[n] = np.ascontiguousarray(inputs[n], dtype=np.float32)
        in_maps.append(m)
    res = run_bass_kernel_spmd(nc, in_maps, core_ids=list(range(NCORES)), trace=trace)
    out = np.stack([res.results[c]["out"] for c in range(NCORES)], axis=0)
    if DEBUG_OUT:
        global LAST_DEBUG
        LAST_DEBUG = {n: res.results[0][n] for n in ("xa", "xb", "yT") if n in res.results[0]}
    return out, res


def kernel(**inputs):
    out, _ = run(inputs, 6)
    return out.astype(np.float32)
```

```python
import numpy as np
from contextlib import ExitStack
import concourse.bass as bass
import concourse.mybir as mybir
from concourse.bass_utils import run_bass_kernel_spmd

F32 = mybir.dt.float32
F32R = mybir.dt.float32r
AF = mybir.ActivationFunctionType
ALU = mybir.AluOpType
AX = mybir.AxisListType

D = 1024
S = 4096
DFF = 2816
NCH = DFF // 128
ALPHA = 4 ** 0.25
LN_EPS = 1e-5
NCORES = 8
DEBUG_OUT = False


class Buf:
    __slots__ = ("name", "w", "r", "dsem")

    def __init__(self, name, dsem=None):
        self.name = name
        self.w = {}
        self.r = {}
        self.dsem = dsem


class Eng:
    def __init__(self, name, sem):
        self.name = name
        self.sem = sem
        self.waited = {}
        self.insts = []


class SemC:
    def __init__(self, handle, is_dma):
        self.h = handle
        self.count = 0
        self.is_dma = is_dma


class Prog:
    def __init__(self, nc, stack):
        self.nc = nc
        self.stack = stack
        self.nsem = 0
        self.dma_sems = []
        self.free_sems = []
        self.phase_sems = []
        self.eng = {}
        for n in ("pe", "act", "dve", "pool", "sp"):
            self.eng[n] = Eng(n, self.new_sem(n, False))

    def new_sem(self, name, is_dma):
        self.nsem += 1
        h = self.stack.enter_context(self.nc.semaphore("s_%s_%d" % (name, self.nsem)))
        sc = SemC(h, is_dma)
        if is_dma:
            self.dma_sems.append(sc)
        return sc

    def sbuf(self, name, shape, dtype=F32):
        return self.stack.enter_context(self.nc.sbuf_tensor("sb_" + name, shape, dtype))

    def psum(self, name, shape, dtype=F32):
        return self.stack.enter_context(self.nc.psum_tensor("pt_" + name, shape, dtype))

    def _deps(self, e, reads, writes):
        need = {}

        def add(d):
            for s, v in d.items():
                if s.is_dma:
                    v = s.count
                if need.get(s, 0) < v:
                    need[s] = v
        for b in reads:
            add(b.w)
        for b in writes:
            add(b.w)
            add(b.r)
        waits = []
        for s, v in need.items():
            if s is e.sem and e.name == "pe":
                continue
            if e.waited.get(s, 0) >= v:
                continue
            e.waited[s] = v
            waits.append((s.h, v))
        return waits

    def op(self, en, reads, writes, meth, *args, **kwargs):
        e = self.eng[en]
        fn = (lambda h, meth=meth, args=args, kwargs=kwargs: getattr(h, meth)(*args, **kwargs))
        waits = self._deps(e, reads, writes)
        e.sem.count += 1
        v = e.sem.count
        e.insts.append((waits, fn, e.sem.h, 1))
        for b in reads:
            b.r[e.sem] = v
        for b in writes:
            b.w[e.sem] = v

    def dma(self, en, out, in_, reads, writes):
        e = self.eng[en]
        assert len(writes) == 1
        dst = writes[0]
        if dst.dsem is None:
            if self.free_sems:
                dst.dsem = self.free_sems.pop()
            else:
                dst.dsem = self.new_sem("d_" + dst.name, True)
            self.phase_sems.append(dst.dsem)
        waits = self._deps(e, reads, writes)
        dst.dsem.count += 16
        v = dst.dsem.count
        e.insts.append((waits, lambda h: h.dma_start(out=out, in_=in_), dst.dsem.h, 16))
        for b in reads:
            b.r[dst.dsem] = v
        dst.w[dst.dsem] = v

    def barrier(self):
        allsems = [e.sem for e in self.eng.values()] + self.dma_sems
        for e in self.eng.values():
            waits = []
            for sc in allsems:
                if sc is e.sem or sc.count == 0:
                    continue
                if e.waited.get(sc, 0) >= sc.count:
                    continue
                e.waited[sc] = sc.count
                waits.append((sc.h, sc.count))
            if waits:
                e.insts.append((waits, None, None, 0))
        self.free_sems.extend(self.phase_sems)
        self.phase_sems = []

    def final_wait(self, en, bufs):
        e = self.eng[en]
        waits = self._deps(e, bufs, ())
        e.insts.append((waits, None, None, 0))

    def emit(self):
        nc = self.nc
        with nc.Block() as block:
            def mk(e):
                def body(h):
                    for waits, fn, sem, inc in e.insts:
                        for sh, v in waits:
                            h.wait_ge(sh, v)
                        if fn is not None:
                            fn(h).then_inc(sem, inc)
                return body
            block.tensor(mk(self.eng["pe"]))
            block.scalar(mk(self.eng["act"]))
            block.vector(mk(self.eng["dve"]))
            block.gpsimd(mk(self.eng["pool"]))
            block.sync(mk(self.eng["sp"]))


def r32(ap):
    return ap.bitcast(F32R)


class Ctx:
    pass


class Arena:
    def __init__(self, P, nelem, name="arena"):
        self.t = P.sbuf(name, [128, nelem])
        self.n = nelem
        self.off = 0

    def reset(self):
        self.off = 0

    def alloc(self, n, parts=128):
        n += n % 2
        assert self.off + n <= self.n, (self.off, n, self.n)
        ap = self.t[0:parts, self.off:self.off + n]
        self.off += n
        return ap

    def alloc3(self, k, n, parts=128):
        return self.alloc(k * n, parts).rearrange("p (k n) -> p k n", k=k)


def setup_common(P, nc, consts):
    C = Ctx()
    C.P = P
    C.nc = nc
    C.psall = P.psum("psall", [128, 4096])
    C.ps = [C.psall[:, i * 512:(i + 1) * 512] for i in range(8)]
    C.psb = [Buf("ps%d" % i) for i in range(8)]
    C.ident = P.sbuf("ident", [128, 128])
    C.identb = Buf("ident")
    P.dma("sp", C.ident[:, :], consts["ident"], [], [C.identb])
    C.mhalf = P.sbuf("mhalf", [128, 1])
    C.mhalfb = Buf("mhalf")
    P.op("dve", [], [C.mhalfb], "memset", C.mhalf[:, :], -0.5)
    C.one = P.sbuf("one", [128, 64])
    C.oneb = Buf("one")
    P.op("dve", [], [C.oneb], "memset", C.one[:, :], 1.0)
    C.A = Arena(P, 30720, "arenaR")
    C.N = Arena(P, 15600, "arenaN")
    C.consts = consts
    return C


def load_gb(C, g_d, b_d, gt, bt, gb_b):
    P = C.P
    P.dma("sp", gt, g_d.partition_broadcast(128), [], [gb_b[0]])
    P.dma("sp", bt, b_d.partition_broadcast(128), [], [gb_b[1]])


class Work:
    pass


def alloc_norm(C, W):
    A = C.N
    W.z = [A.alloc(D) for i in range(4)]
    W.zb = [Buf("z%d" % i) for i in range(4)]
    W.st = [A.alloc(16) for i in range(4)]
    W.stb = [Buf("st%d" % i) for i in range(4)]
    W.gt = A.alloc(D)
    W.bt = A.alloc(D)
    W.gbb = [Buf("gt"), Buf("bt")]
    W.zcnt = 0


def post_norm_evac(C, W, y_banks, xs, xsb):
    P = C.P
    zi = W.zcnt
    W.zcnt += 1
    z = W.z[zi % 4]
    zb = W.zb[zi % 4]
    for hh in range(2):
        yb = y_banks[hh]
        P.op("dve", [xsb, C.psb[yb]], [zb], "scalar_tensor_tensor",
             out=z[:, hh * 512:(hh + 1) * 512], in0=xs[:, hh * 512:(hh + 1) * 512], scalar=ALPHA,
             in1=C.ps[yb][:, :], op0=ALU.mult, op1=ALU.add)
    return zi


def post_norm_finish(C, W, zi, x_out_rows, x_out_buf):
    post_norm_finish_a(C, W, zi)
    post_norm_finish_b(C, W, zi, x_out_rows, x_out_buf)


def post_norm_finish_a(C, W, zi):
    P = C.P
    z = W.z[zi % 4]
    zb = W.zb[zi % 4]
    st = W.st[zi % 4]
    stb = W.stb[zi % 4]
    for hh in range(2):
        P.op("dve", [zb], [stb], "bn_stats", out=st[:, hh * 6:(hh + 1) * 6], in_=z[:, hh * 512:(hh + 1) * 512])
    P.op("dve", [stb], [stb], "bn_aggr", out=st[:, 12:14], in_=st[:, 0:12])
    P.op("dve", [stb], [stb], "tensor_scalar", out=st[:, 14:15], in0=st[:, 13:14], scalar1=LN_EPS, scalar2=None,
         op0=ALU.add)
    P.op("pool", [stb, C.mhalfb], [stb], "tensor_tensor", out=st[:, 15:16], in0=st[:, 14:15], in1=C.mhalf[:, 0:1],
         op=ALU.pow)


def post_norm_finish_b(C, W, zi, x_out_rows, x_out_buf):
    P = C.P
    z = W.z[zi % 4]
    zb = W.zb[zi % 4]
    st = W.st[zi % 4]
    stb = W.stb[zi % 4]
    gt, bt, gb_b = W.gt, W.bt, W.gbb
    P.op("dve", [zb, stb], [zb], "tensor_scalar", out=z, in0=z, scalar1=st[:, 12:13],
         scalar2=st[:, 15:16], op0=ALU.subtract, op1=ALU.mult)
    P.op("dve", [zb, gb_b[0]], [zb], "tensor_tensor", out=z, in0=z, in1=gt, op=ALU.mult)
    P.op("dve", [zb, gb_b[1]], [zb], "tensor_tensor", out=z, in0=z, in1=bt, op=ALU.add)
    P.dma("pool", x_out_rows, z, [zb], [x_out_buf])


def post_norm_tile(C, W, y_banks, xs, xsb, x_out_rows, x_out_buf):
    zi = post_norm_evac(C, W, y_banks, xs, xsb)
    post_norm_finish(C, W, zi, x_out_rows, x_out_buf)


def alloc_xT(C, W, nbuf=2):
    A = C.A
    W.xs = [C.N.alloc(D) for i in range(4)]
    W.xsb = [Buf("xs%d" % i) for i in range(4)]
    W.nxT = nbuf
    W.xT = [A.alloc3(8, 512) for i in range(nbuf)]
    W.xTb = [[Buf("xT%d_%d" % (i, k)) for k in range(8)] for i in range(nbuf)]
    W.xscnt = 0
    W.xTcnt = 0


def load_xT(C, W, x_in, x_in_bufs, t):
    P = C.P
    xTi = W.xTcnt % W.nxT
    W.xTcnt += 1
    xT = W.xT[xTi]
    xTb = W.xTb[xTi]
    xsl = []
    for s in range(4):
        i = W.xscnt % 4
        W.xscnt += 1
        r0 = t * 512 + s * 128
        P.dma("sp", W.xs[i], x_in[r0:r0 + 128, :], [x_in_bufs[t * 4 + s]], [W.xsb[i]])
        xsl.append(i)
    for kc in range(8):
        bank = kc % 2
        for s in range(4):
            i = xsl[s]
            P.op("pe", [W.xsb[i], C.identb], [C.psb[bank]], "transpose",
                 out=C.ps[bank][:, s * 128:(s + 1) * 128], in_=W.xs[i][:, kc * 128:(kc + 1) * 128],
                 identity=C.ident[:, :])
        if kc % 2 == 0:
            P.op("act", [C.psb[bank]], [xTb[kc]], "copy", out=r32(xT[:, kc, :]), in_=C.ps[bank][:, :])
        else:
            P.op("dve", [C.psb[bank]], [xTb[kc]], "tensor_copy", out=r32(xT[:, kc, :]), in_=C.ps[bank][:, :])
    return xT, xTb


def reload_xs(C, W, x_in, x_in_bufs, row_tile):
    P = C.P
    i = W.xscnt % 4
    W.xscnt += 1
    r0 = row_tile * 128
    P.dma("sp", W.xs[i], x_in[r0:r0 + 128, :], [x_in_bufs[row_tile]], [W.xsb[i]])
    return W.xs[i], W.xsb[i]


def ffn_sublayer(C, x_in, x_in_bufs, x_out, x_out_bufs, w_in, w_out, g_d, b_d):
    P = C.P
    A = C.A
    P.barrier()
    A.reset()
    C.N.reset()
    W = Work()
    alloc_norm(C, W)
    xT = A.alloc3(8, 512)
    xTb = [Buf("xT_%d" % k) for k in range(8)]
    xss = [[C.N.alloc(D) for i in range(4)] for j in range(2)]
    xssb = [[Buf("xs%d_%d" % (j, i)) for i in range(4)] for j in range(2)]

    def issue_loads(t):
        for s_ in range(4):
            r0 = t * 512 + s_ * 128
            P.dma("sp", xss[t % 2][s_], x_in[r0:r0 + 128, :], [x_in_bufs[t * 4 + s_]], [xssb[t % 2][s_]])
    aT = A.alloc3(NCH, 512)
    aTb = [Buf("aT%d" % j) for j in range(NCH)]
    wins = [A.alloc(8 * 2 * 256).rearrange("p (k h n) -> p k h n", k=8, h=2) for i in range(3)]
    winbs = [Buf("win%d" % i) for i in range(3)]
    wouts = [A.alloc(D) for i in range(3)]
    woutbs = [Buf("wout%d" % i) for i in range(3)]
    sgs = [C.N.alloc(512) for i in range(2)]
    sgbs = [Buf("sg%d" % i) for i in range(2)]
    cnt = {"win": 0, "wout": 0, "sg": 0}
    load_gb(C, g_d, b_d, W.gt, W.bt, W.gbb)
    w_in_v = w_in.rearrange("(kc p) n -> p kc n", p=128)
    NT = S // 512
    issue_loads(0)
    pending = []
    for t in range(NT):
        for kc in range(8):
            bank = kc % 2
            for s_ in range(4):
                P.op("pe", [xssb[t % 2][s_], C.identb], [C.psb[bank]], "transpose",
                     out=C.ps[bank][:, s_ * 128:(s_ + 1) * 128], in_=xss[t % 2][s_][:, kc * 128:(kc + 1) * 128],
                     identity=C.ident[:, :])
            P.op("act", [C.psb[bank]], [xTb[kc]], "copy", out=r32(xT[:, kc, :]), in_=C.ps[bank][:, :])
        for grp in range(NCH // 2):
            if pending and grp >= 1:
                if grp in (1, 3, 5, 7):
                    post_norm_finish_a(C, W, pending[(grp - 1) // 2][0])
                if grp in (2, 4, 6, 8):
                    pz, prt = pending[(grp - 2) // 2]
                    post_norm_finish_b(C, W, pz, x_out[prt * 128:(prt + 1) * 128, :], x_out_bufs[prt])
                if grp == 8:
                    pending = []
            wi = cnt["win"] % 3
            cnt["win"] += 1
            win = wins[wi]
            winb = winbs[wi]
            P.dma("sp", r32(win[:, :, 0, :]), r32(w_in_v[:, :, grp * 256:(grp + 1) * 256]), [], [winb])
            P.dma("sp", r32(win[:, :, 1, :]), r32(w_in_v[:, :, DFF + grp * 256:DFF + (grp + 1) * 256]), [], [winb])
            for jj in range(2):
                j = grp * 2 + jj
                gb_ = 2 + 2 * (j % 2)
                ub_ = 3 + 2 * (j % 2)
                for half, bank in ((0, gb_), (1, ub_)):
                    for kc in range(8):
                        P.op("pe", [winb, xTb[kc]], [C.psb[bank]], "matmul",
                             C.ps[bank][:, :], lhsT=r32(win[:, kc, half, jj * 128:(jj + 1) * 128]),
                             rhs=r32(xT[:, kc, :]), start=(kc == 0), stop=(kc == 7))
                si = cnt["sg"] % 2
                cnt["sg"] += 1
                sg = sgs[si]
                P.op("act", [C.psb[gb_]], [sgbs[si]], "activation", out=sg, in_=C.ps[gb_][:, :], func=AF.Silu)
                P.op("dve", [sgbs[si], C.psb[ub_]], [aTb[j]], "scalar_tensor_tensor",
                     out=r32(aT[:, j, :]), in0=sg, scalar=0.5, in1=C.ps[ub_][:, :], op0=ALU.mult, op1=ALU.mult)
        if t + 1 < NT:
            issue_loads(t + 1)
        for j in range(NCH):
            wi = cnt["wout"] % 3
            cnt["wout"] += 1
            wout = wouts[wi]
            woutb = woutbs[wi]
            P.dma("sp", r32(wout), r32(w_out[j * 128:(j + 1) * 128, :]), [], [woutb])
            for s in range(4):
                for hh in range(2):
                    bank = 2 * s + hh
                    P.op("pe", [aTb[j], woutb], [C.psb[bank]], "matmul",
                         C.ps[bank][:, :], lhsT=r32(aT[:, j, s * 128:(s + 1) * 128]),
                         rhs=r32(wout[:, hh * 512:(hh + 1) * 512]), start=(j == 0), stop=(j == NCH - 1))
        zis = []
        for s in range(4):
            rt = t * 4 + s
            zis.append(post_norm_evac(C, W, (2 * s, 2 * s + 1), xss[t % 2][s], xssb[t % 2][s]))
        pending = [(zis[s], t * 4 + s) for s in range(4)]
    for pz, prt in pending:
        post_norm_finish(C, W, pz, x_out[prt * 128:(prt + 1) * 128, :], x_out_bufs[prt])


def out_proj_norm(C, yT_d, yT_bufs, w_out, g_d, b_d, x_in, x_in_bufs, x_out, x_out_bufs):
    P = C.P
    A = C.A
    P.barrier()
    A.reset()
    C.N.reset()
    W = Work()
    alloc_norm(C, W)
    W.xs = [C.N.alloc(D) for i in range(4)]
    W.xsb = [Buf("xs%d" % i) for i in range(4)]
    W.xscnt = 0
    wo = A.alloc3(8, D)
    wob = Buf("wo")
    P.dma("sp", r32(wo), r32(w_out.rearrange("(kc p) n -> p kc n", p=128)), [], [wob])
    yts = [A.alloc3(8, 512) for i in range(2)]
    ytbs = [Buf("yt%d" % i) for i in range(2)]
    load_gb(C, g_d, b_d, W.gt, W.bt, W.gbb)
    yT_v = yT_d.rearrange("(kc p) n -> p kc n", p=128)
    for t in range(S // 512):
        yt = yts[t % 2]
        ytb = ytbs[t % 2]
        P.dma("sp", r32(yt), r32(yT_v[:, :, t * 512:(t + 1) * 512]), yT_bufs, [ytb])
        for s in range(4):
            rt = t * 4 + s
            banks = (2 * (rt % 4), 2 * (rt % 4) + 1)
            for hh in range(2):
                for kc in range(8):
                    P.op("pe", [ytb, wob], [C.psb[banks[hh]]], "matmul", C.ps[banks[hh]][:, :],
                         lhsT=r32(yt[:, kc, s * 128:(s + 1) * 128]), rhs=r32(wo[:, kc, hh * 512:(hh + 1) * 512]),
                         start=(kc == 0), stop=(kc == 7))
            xs, xsb = reload_xs(C, W, x_in, x_in_bufs, rt)
            post_norm_tile(C, W, banks, xs, xsb, x_out[rt * 128:(rt + 1) * 128, :], x_out_bufs[rt])


def proj_phase(C, x_in, x_in_bufs, w_in, ccols, small_ci, projT, projb):
    P = C.P
    A = C.A
    N = C.N
    P.barrier()
    A.reset()
    N.reset()
    xs = [N.alloc(D) for i in range(4)]
    xsb = [Buf("xs%d" % i) for i in range(4)]
    evs = [N.alloc(512) for i in range(4)]
    evbs = [Buf("ev%d" % i) for i in range(4)]
    HT = 2048
    xT = A.alloc3(8, HT)
    xTb = [[Buf("xTb%d_%d" % (tt, kc)) for kc in range(8)] for tt in range(4)]
    wcs = [A.alloc3(8, 128) for i in range(3)]
    wcbs = [Buf("wc%d" % i) for i in range(3)]
    w_in_v = w_in.rearrange("(kc p) n -> p kc n", p=128)
    xc = 0
    k = 0
    wk = 0
    for half in range(2):
        for tt in range(4):
            t = half * 4 + tt
            xsl = []
            for s_ in range(4):
                i = xc % 4
                xc += 1
                r0 = t * 512 + s_ * 128
                P.dma("sp", xs[i], x_in[r0:r0 + 128, :], [x_in_bufs[t * 4 + s_]], [xsb[i]])
                xsl.append(i)
            for kc in range(8):
                bank = kc % 2
                for s_ in range(4):
                    i = xsl[s_]
                    P.op("pe", [xsb[i], C.identb], [C.psb[bank]], "transpose",
                         out=C.ps[bank][:, s_ * 128:(s_ + 1) * 128], in_=xs[i][:, kc * 128:(kc + 1) * 128],
                         identity=C.ident[:, :])
                dst = r32(xT[:, kc, tt * 512:(tt + 1) * 512])
                if kc % 2 == 0:
                    P.op("act", [C.psb[bank]], [xTb[tt][kc]], "copy", out=dst, in_=C.ps[bank][:, :])
                else:
                    P.op("dve", [C.psb[bank]], [xTb[tt][kc]], "tensor_copy", out=dst, in_=C.ps[bank][:, :])
        for ci, c0 in enumerate(ccols):
            nrow = 8 if ci == small_ci else 128
            wc = wcs[wk % 3]
            wcb = wcbs[wk % 3]
            wk += 1
            P.dma("sp", r32(wc), r32(w_in_v[:, :, c0:c0 + 128]), [], [wcb])
            for tt in range(4):
                t = half * 4 + tt
                ev = evs[k % 4]
                evb = evbs[k % 4]
                bank = 2 + (k % 4)
                for kc in range(8):
                    P.op("pe", [wcb, xTb[tt][kc]], [C.psb[bank]], "matmul", C.ps[bank][:, :],
                         lhsT=r32(wc[:, kc, :]), rhs=r32(xT[:, kc, tt * 512:(tt + 1) * 512]),
                         start=(kc == 0), stop=(kc == 7))
                if k % 2 == 0:
                    P.op("act", [C.psb[bank]], [evb], "copy", out=ev, in_=C.ps[bank][:, :])
                else:
                    P.op("dve", [C.psb[bank]], [evb], "tensor_copy", out=ev, in_=C.ps[bank][:, :])
                P.dma("pool", projT[c0:c0 + nrow, t * 512:(t + 1) * 512], ev[0:nrow, :], [evb], [projb[ci][t]])
                k += 1


NEGM = -240000.0


def even_mixer(C, x_in, x_in_bufs, x_out, x_out_bufs, prm, scr):
    P = C.P
    A = C.A
    w_in = prm["l0_mix_w_in"]
    projT, projb = scr["projT"], scr["projb"]
    yT, yTb = scr["yT"], scr["yTb"]
    proj_phase(C, x_in, x_in_bufs, w_in, [c * 128 for c in range(24)], -1, projT, projb)
    P.barrier()
    A.reset()
    C.N.reset()
    N = C.N
    HB = S // 2
    bg = N.alloc(HB + 2)
    cg = N.alloc(HB + 2)
    xh = N.alloc(HB + 2)
    acc = N.alloc(HB + 2)
    cw = N.alloc(4)
    bgb, cgb, xhb, accb, cwb = Buf("bg"), Buf("cg"), Buf("xh"), Buf("acc"), Buf("cw")
    for c in range(2):
        P.dma("sp", cw[:, 0:3], C.consts["conv_wT"][c * 128:(c + 1) * 128, :], [bgb, cgb, accb], [cwb])
        for hf in range(2):
            lo = max(0, hf * HB - 2)
            hi = hf * HB + HB
            n = hi - lo
            halo = hf * HB - lo
            P.dma("sp", bg[:, 0:n], projT[c * 128:(c + 1) * 128, lo:hi], projb[c], [bgb])
            P.dma("sp", cg[:, 0:n], projT[(2 + c) * 128:(3 + c) * 128, lo:hi], projb[2 + c], [cgb])
            P.dma("sp", xh[:, 0:n], projT[(4 + c) * 128:(5 + c) * 128, lo:hi], projb[4 + c], [xhb])
            P.op("dve", [cgb, xhb], [cgb], "tensor_tensor", out=cg[:, 0:n], in0=cg[:, 0:n], in1=xh[:, 0:n], op=ALU.mult)
            P.op("dve", [cgb, cwb], [accb], "tensor_scalar", out=acc[:, 0:n], in0=cg[:, 0:n], scalar1=cw[:, 2:3],
                 scalar2=None, op0=ALU.mult)
            P.op("dve", [cgb, cwb, accb], [accb], "scalar_tensor_tensor", out=acc[:, 1:n], in0=cg[:, 0:n - 1],
                 scalar=cw[:, 1:2], in1=acc[:, 1:n], op0=ALU.mult, op1=ALU.add)
            P.op("dve", [cgb, cwb, accb], [accb], "scalar_tensor_tensor", out=acc[:, 2:n], in0=cg[:, 0:n - 2],
                 scalar=cw[:, 0:1], in1=acc[:, 2:n], op0=ALU.mult, op1=ALU.add)
            P.op("dve", [bgb, accb], [accb], "tensor_tensor", out=acc[:, 0:n], in0=acc[:, 0:n], in1=bg[:, 0:n], op=ALU.mult)
            P.dma("pool", yT[c * 128:c * 128 + 64, hf * HB:hi], acc[0:64, halo:n], [accb], [yTb[2 * c]])
            P.dma("pool", yT[c * 128 + 64:c * 128 + 128, hf * HB:hi], acc[64:128, halo:n], [accb], [yTb[2 * c + 1]])
    P.barrier()
    A.reset()
    C.N.reset()
    qt = [A.alloc(S, 64) for i in range(2)]
    kt = [A.alloc(S, 64) for i in range(2)]
    N = C.N
    vt1 = N.alloc(S, 64)
    vt = [vt1, vt1]
    vtb = Buf("v")
    qkvb = [[Buf("q%d" % i), Buf("k%d" % i), vtb] for i in range(2)]
    acc = N.alloc(S, 65)
    accb = Buf("acc")
    mask = N.alloc(256)
    maskb = Buf("mask")
    sel = N.alloc(64, 65)
    selb = Buf("sel")
    P.dma("sp", mask, C.consts["maskT"], [], [maskb])
    P.dma("sp", sel, C.consts["sel"], [], [selb])
    sms = [N.alloc(256) for i in range(3)]
    smbs = [Buf("sm%d" % i) for i in range(3)]
    pts = [A.alloc(256) for i in range(3)]
    ptbs = [Buf("pt%d" % i) for i in range(3)]
    vxs = [A.alloc(65) for i in range(3)]
    vxbs = [Buf("vx%d" % i) for i in range(3)]
    for i in range(3):
        P.op("dve", [C.oneb], [vxbs[i]], "tensor_copy", out=r32(vxs[i][:, 64:65]), in_=C.one[:, 0:1])
    rds = [N.alloc(512, 64) for i in range(2)]
    rdbs = [Buf("rd%d" % i) for i in range(2)]
    yos = [N.alloc(512, 64) for i in range(2)]
    yobs = [Buf("yo%d" % i) for i in range(2)]
    st = {"blk": 0, "vxc": 0, "grp": 0}
    nrm = 0
    for hd in range(12):
        hi = hd % 2
        q, kk, v = qt[hi], kt[hi], vt[hi]
        qb, kb, vb = qkvb[hi]
        cq, rq = divmod(768 + hd * 64, 128)
        ck, rk = divmod(1536 + hd * 64, 128)
        cv, rv = divmod(2304 + hd * 64, 128)
        P.dma("sp", r32(q), r32(projT[768 + hd * 64:768 + (hd + 1) * 64, :]), projb[cq], [qb])
        P.dma("sp", r32(kk), r32(projT[1536 + hd * 64:1536 + (hd + 1) * 64, :]), projb[ck], [kb])
        P.dma("sp", v, projT[2304 + hd * 64:2304 + (hd + 1) * 64, :], projb[cv], [vb])
        blocks = []
        for bi, d in enumerate((1, 4, 16)):
            nb = 32 // d
            for r in range(d):
                for n in range(nb):
                    blocks.append({"bi": bi, "d": d, "r": r, "n": n, "nb": nb, "G": min(4, nb)})

        def stage_a(B):
            d, r, n = B["d"], B["r"], B["n"]
            st0 = r + d * 128 * n
            cur = slice(st0, min(st0 + d * 128, S), d)
            blk = st["blk"]
            st["blk"] += 1
            sbank = blk % 3
            sm, smb = sms[blk % 3], smbs[blk % 3]
            pt, ptb = pts[blk % 3], ptbs[blk % 3]
            ncol = 256 if n > 0 else 128
            P.op("pe", [kb, qb], [C.psb[sbank]], "matmul", C.ps[sbank][:, 0:128],
                 lhsT=r32(kk[:, cur]), rhs=r32(q[:, cur]), start=True, stop=True)
            if n > 0:
                sp0 = r + d * 128 * (n - 1)
                prv = slice(sp0, min(sp0 + d * 128, S), d)
                P.op("pe", [kb, qb], [C.psb[sbank]], "matmul", C.ps[sbank][:, 128:256],
                     lhsT=r32(kk[:, prv]), rhs=r32(q[:, cur]), start=True, stop=True)
            P.op("dve", [C.psb[sbank], maskb], [smb], "tensor_tensor", out=sm[:, 0:ncol],
                 in0=C.ps[sbank][:, 0:ncol], in1=mask[:, 0:ncol], op=ALU.add)
            P.op("act", [smb], [ptb], "activation", out=r32(pt[:, 0:ncol]), in_=sm[:, 0:ncol],
                 func=AF.Exp, scale=0.125)
            vxc = st["vxc"]
            st["vxc"] += 1
            vx, vxb = vxs[vxc % 3], vxbs[vxc % 3]
            tb = 3 + (vxc % 2)
            P.op("pe", [vb, C.identb], [C.psb[tb]], "transpose", out=C.ps[tb][:, 0:64],
                 in_=v[:, cur], identity=C.ident[0:64, 0:64])
            P.op("dve", [C.psb[tb]], [vxb], "tensor_copy", out=r32(vx[:, 0:64]), in_=C.ps[tb][:, 0:64])
            B.update(pt=pt, ptb=ptb, vx=vx, vxb=vxb, st0=st0)

        def stage_b(B, Bprev):
            d, n, G, bi = B["d"], B["n"], B["G"], B["bi"]
            gi = n % G
            if gi == 0:
                st["gbank"] = 5 + (st["grp"] % 2)
                st["grp"] += 1
                st["g_start"] = B["st0"]
            gbank = st["gbank"]
            osl = C.ps[gbank][0:65, gi * 128:(gi + 1) * 128]
            P.op("pe", [B["vxb"], B["ptb"]], [C.psb[gbank]], "matmul", osl, lhsT=r32(B["vx"][:, 0:65]),
                 rhs=r32(B["pt"][:, 0:128]), start=True, stop=(n == 0))
            if n > 0:
                P.op("pe", [Bprev["vxb"], B["ptb"]], [C.psb[gbank]], "matmul", osl, lhsT=r32(Bprev["vx"][:, 0:65]),
                     rhs=r32(B["pt"][:, 128:256]), start=False, stop=True)
            if gi == G - 1:
                g_start = st["g_start"]
                gs = slice(g_start, min(g_start + d * 128 * G, S), d)
                if bi == 0:
                    P.op("act", [C.psb[gbank]], [accb], "copy", out=acc[:, gs], in_=C.ps[gbank][0:65, 0:128 * G])
                else:
                    P.op("dve", [C.psb[gbank], accb], [accb], "tensor_tensor", out=acc[:, gs], in0=acc[:, gs],
                         in1=C.ps[gbank][0:65, 0:128 * G], op=ALU.add)

        for i in range(len(blocks) + 1):
            if i < len(blocks):
                stage_a(blocks[i])
            if i >= 1:
                stage_b(blocks[i - 1], blocks[i - 2] if i >= 2 else None)
        for cb in range(8):
            cs = slice(cb * 512, (cb + 1) * 512)
            rd, rdb = rds[nrm % 2], rdbs[nrm % 2]
            yo, yob = yos[nrm % 2], yobs[nrm % 2]
            nrm += 1
            P.op("pe", [accb, selb], [C.psb[7]], "matmul", C.ps[7][0:64, :], lhsT=sel, rhs=acc[:, cs],
                 start=True, stop=True)
            P.op("dve", [C.psb[7]], [rdb], "reciprocal", out=rd, in_=C.ps[7][0:64, :])
            P.op("dve", [rdb, accb], [yob], "tensor_tensor", out=yo, in0=acc[0:64, cs], in1=rd, op=ALU.mult)
            P.dma("pool", yT[256 + hd * 64:256 + (hd + 1) * 64, cs], yo, [yob], [yTb[4 + hd]])
    out_proj_norm(C, yT, yTb, prm["l0_mix_w_out"], prm["l0_ln2_g"], prm["l0_ln2_b"], x_in, x_in_bufs, x_out, x_out_bufs)


def odd_mixer(C, x_in, x_in_bufs, x_out, x_out_bufs, prm, scr):
    P = C.P
    A = C.A
    N = C.N
    w_in = prm["l1_mix_w_in"]
    projT, projb = scr["projT"], scr["projb"]
    yT, yTb = scr["yT"], scr["yTb"]
    ccols = [i * 128 for i in range(16)] + [2048] + [2056 + i * 128 for i in range(12)]
    proj_phase(C, x_in, x_in_bufs, w_in, ccols, 16, projT, projb)
    P.barrier()
    A.reset()
    N.reset()
    triu = N.alloc(128)
    ones = N.alloc(128)
    zer = N.alloc(130)
    cb_ = Buf("mconst")
    P.dma("sp", triu, C.consts["triu"], [], [cb_])
    P.op("dve", [], [cb_], "memset", ones, 1.0)
    P.op("dve", [], [cb_], "memset", zer, 0.0)
    ng = N.alloc(512)
    ngb = Buf("ng")
    P.dma("sp", ng, prm["l1_mlstm_norm_g"].partition_broadcast(128), [], [ngb])
    biasb = N.alloc(8)
    biasbb = Buf("biasb")
    P.dma("sp", biasb[:, 0:4], prm["l1_mlstm_b_i"].partition_broadcast(128), [], [biasbb])
    P.dma("sp", biasb[:, 4:8], prm["l1_mlstm_b_f"].partition_broadcast(128), [], [biasbb])
    grow = N.alloc(S, 8)
    growb = Buf("grow")
    P.dma("sp", grow, projT[2048:2056, :], projb[16], [growb])
    gcol = N.alloc(256)
    nlf = N.alloc(256)
    tmp = N.alloc(128)
    egs = N.alloc(128)
    ea = N.alloc(128)
    eb = N.alloc(128)
    gb_ = Buf("gcol")
    for b in range(32):
        P.op("pe", [growb, C.identb], [C.psb[0]], "transpose", out=C.ps[0][:, b * 8:(b + 1) * 8],
             in_=grow[:, b * 128:(b + 1) * 128], identity=C.ident[0:8, 0:8])
    for b in range(32):
        P.op("dve", [C.psb[0], biasbb], [gb_], "tensor_tensor", out=gcol[:, b * 8:(b + 1) * 8],
             in0=C.ps[0][:, b * 8:(b + 1) * 8], in1=biasb, op=ALU.add)
    P.op("act", [gb_], [gb_], "activation", out=nlf, in_=gcol, func=AF.Exp, scale=-1.0)
    P.op("act", [gb_], [gb_], "activation", out=nlf, in_=nlf, func=AF.Ln, bias=1.0)
    P.op("pe", [gb_, cb_], [C.psb[1]], "matmul", C.ps[1][:, 0:256], lhsT=triu, rhs=nlf, start=True, stop=True)
    P.op("pe", [gb_, cb_], [C.psb[2]], "matmul", C.ps[2][:, 0:256], lhsT=ones, rhs=nlf, start=True, stop=True)
    g3 = gcol.rearrange("p (b j) -> p b j", j=8)
    cs3 = C.ps[1][:, 0:256].rearrange("p (b j) -> p b j", j=8)
    tot3 = C.ps[2][:, 0:256].rearrange("p (b j) -> p b j", j=8)
    v3 = lambda ap: ap.rearrange("p (b j) -> p b j", j=4)
    P.op("dve", [gb_, C.psb[1]], [gb_], "tensor_tensor", out=v3(tmp), in0=g3[:, :, 0:4], in1=cs3[:, :, 4:8], op=ALU.add)
    P.op("act", [gb_], [gb_], "activation", out=tmp, in_=tmp, func=AF.Exp)
    P.op("dve", [gb_], [gb_], "tensor_scalar", out=egs, in0=tmp, scalar1=float(128 ** -0.5), scalar2=None, op0=ALU.mult)
    P.op("act", [C.psb[1]], [gb_], "activation", out=v3(ea), in_=cs3[:, :, 4:8], func=AF.Exp, scale=-1.0)
    P.op("act", [C.psb[2]], [gb_], "activation", out=v3(eb), in_=tot3[:, :, 4:8], func=AF.Exp, scale=-1.0)
    cext = [A.alloc(130) for h in range(4)]
    cextb = [Buf("cext%d" % h) for h in range(4)]
    for h in range(4):
        P.op("dve", [cb_], [cextb[h]], "tensor_copy", out=r32(cext[h]), in_=zer)
    vexts = [A.alloc(130) for i in range(2)]
    vextbs = [Buf("vext%d" % i) for i in range(2)]
    for i in range(2):
        P.op("dve", [C.oneb], [vextbs[i]], "tensor_copy", out=r32(vexts[i][:, 128:130]), in_=C.one[:, 0:2])
    kgs = [A.alloc(128) for i in range(2)]
    kgbs = [Buf("kg%d" % i) for i in range(2)]
    wts = [A.alloc(128) for i in range(2)]
    wtbs = [Buf("wt%d" % i) for i in range(2)]
    q4s = [A.alloc3(4, 512) for i in range(2)]
    k4s = [A.alloc3(4, 512) for i in range(2)]
    qkbs = [[Buf("q4%d" % i), Buf("k4%d" % i)] for i in range(2)]
    v4 = N.alloc3(4, 512)
    o4 = N.alloc3(4, 512)
    v4b, o4b = Buf("v4"), Buf("o4")
    ymT = N.alloc3(4, 512)
    ymTb = Buf("ymT")
    scs = [N.alloc(16) for i in range(2)]
    scbs = [Buf("sc%d" % i) for i in range(2)]
    hts = [N.alloc(128) for i in range(2)]
    htbs = [Buf("ht%d" % i) for i in range(2)]
    sgos = [N.alloc(128) for i in range(2)]
    sgobs = [Buf("sgo%d" % i) for i in range(2)]
    hv = lambda r0: projT[r0:r0 + 512, :].rearrange("(h p) n -> p h n", p=128)
    pj = lambda c0: [b for ci in range(c0, c0 + 4) for b in projb[ci]]
    for t in range(8):
        ts_ = slice(t * 512, (t + 1) * 512)
        q4, k4 = q4s[t % 2], k4s[t % 2]
        q4b, k4b = qkbs[t % 2]
        P.dma("sp", r32(q4), r32(hv(0)[:, :, ts_]), pj(0), [q4b])
        P.dma("sp", r32(k4), r32(hv(512)[:, :, ts_]), pj(4), [k4b])
        P.dma("sp", v4, hv(1024)[:, :, ts_], pj(8), [v4b])
        P.dma("sp", o4, hv(1536)[:, :, ts_], pj(12), [o4b])
        for cc in range(4):
            c = t * 4 + cc
            cl = slice(cc * 128, (cc + 1) * 128)
            for h in range(4):
                i2 = h % 2
                ch = c * 4 + h
                vext, vextb = vexts[i2], vextbs[i2]
                kg, kgb = kgs[i2], kgbs[i2]
                wt, wtb = wts[i2], wtbs[i2]
                sc, scb = scs[i2], scbs[i2]
                ht, htb = hts[i2], htbs[i2]
                sgo, sgob = sgos[i2], sgobs[i2]
                egs_c = egs[:, ch:ch + 1]
                ea_c = ea[:, ch:ch + 1]
                eb_c = eb[:, ch:ch + 1]
                P.op("pe", [v4b, C.identb], [C.psb[0]], "transpose", out=C.ps[0][:, 0:128], in_=v4[:, h, cl],
                     identity=C.ident[:, :])
                P.op("act", [C.psb[0]], [vextb], "copy", out=r32(vext[:, 0:128]), in_=C.ps[0][:, 0:128])
                P.op("pe", [k4b, C.identb], [C.psb[1]], "transpose", out=C.ps[1][:, 0:128], in_=k4[:, h, cl],
                     identity=C.ident[:, :])
                P.op("dve", [C.psb[1], gb_], [kgb], "tensor_scalar", out=r32(kg), in0=C.ps[1][:, 0:128], scalar1=egs_c,
                     scalar2=None, op0=ALU.mult)
                P.op("pe", [k4b, q4b], [C.psb[2]], "matmul", C.ps[2][:, 0:128], lhsT=r32(k4[:, h, cl]),
                     rhs=r32(q4[:, h, cl]), start=True, stop=True)
                P.op("dve", [C.psb[2], gb_, cb_], [wtb], "scalar_tensor_tensor", out=r32(wt), in0=C.ps[2][:, 0:128],
                     scalar=egs_c, in1=triu, op0=ALU.mult, op1=ALU.mult)
                P.op("pe", [q4b, cextb[h]], [C.psb[3]], "matmul", C.ps[3][:, 0:130], lhsT=r32(q4[:, h, cl]),
                     rhs=r32(cext[h][:, 0:130]), start=True, stop=False)
                P.op("pe", [wtb, vextb], [C.psb[3]], "matmul", C.ps[3][:, 0:130], lhsT=r32(wt), rhs=r32(vext),
                     start=False, stop=True)
                P.op("dve", [C.psb[3], gb_], [scb], "tensor_scalar", out=sc[:, 0:1], in0=C.ps[3][:, 128:129],
                     scalar1=ea_c, scalar2=None, op0=ALU.mult)
                P.op("dve", [scb], [scb], "tensor_scalar", out=sc[:, 3:4], in0=sc[:, 0:1], scalar1=-1.0, scalar2=1.0,
                     op0=ALU.mult, op1=ALU.max)
                P.op("dve", [scb], [scb], "tensor_scalar", out=sc[:, 0:1], in0=sc[:, 0:1], scalar1=1.0, scalar2=None,
                     op0=ALU.max)
                P.op("dve", [scb], [scb], "tensor_tensor", out=sc[:, 0:1], in0=sc[:, 0:1], in1=sc[:, 3:4], op=ALU.max)
                P.op("dve", [scb], [scb], "reciprocal", out=sc[:, 1:2], in_=sc[:, 0:1])
                P.op("dve", [scb, gb_], [scb], "tensor_tensor", out=sc[:, 2:3], in0=sc[:, 1:2], in1=ea_c, op=ALU.mult)
                P.op("dve", [C.psb[3], scb], [htb], "tensor_scalar", out=ht, in0=C.ps[3][:, 0:128], scalar1=sc[:, 2:3],
                     scalar2=None, op0=ALU.mult)
                P.op("pe", [o4b, C.identb], [C.psb[4]], "transpose", out=C.ps[4][:, 0:128], in_=o4[:, h, cl],
                     identity=C.ident[:, :])
                P.op("act", [C.psb[4]], [sgob], "activation", out=sgo, in_=C.ps[4][:, 0:128], func=AF.Sigmoid)
                P.op("dve", [htb, sgob], [htb], "tensor_tensor", out=ht, in0=ht, in1=sgo, op=ALU.mult)
                P.op("dve", [htb], [scb], "bn_stats", out=sc[:, 4:10], in_=ht)
                P.op("dve", [scb], [scb], "bn_aggr", out=sc[:, 10:12], in_=sc[:, 4:10])
                P.op("dve", [scb], [scb], "tensor_scalar", out=sc[:, 12:13], in0=sc[:, 11:12], scalar1=LN_EPS,
                     scalar2=None, op0=ALU.add)
                P.op("pool", [scb, C.mhalfb], [scb], "tensor_tensor", out=sc[:, 13:14], in0=sc[:, 12:13],
                     in1=C.mhalf[:, 0:1], op=ALU.pow)
                P.op("dve", [htb, scb], [htb], "tensor_scalar", out=ht, in0=ht, scalar1=sc[:, 10:11],
                     scalar2=sc[:, 13:14], op0=ALU.subtract, op1=ALU.mult)
                P.op("dve", [htb, ngb], [htb], "tensor_tensor", out=ht, in0=ht, in1=ng[:, h * 128:(h + 1) * 128],
                     op=ALU.mult)
                P.op("pe", [htb, C.identb], [C.psb[5]], "transpose", out=C.ps[5][:, 0:128], in_=ht,
                     identity=C.ident[:, :])
                P.op("act", [C.psb[5]], [ymTb], "copy", out=ymT[:, h, cl], in_=C.ps[5][:, 0:128])
                P.op("pe", [kgb, vextb], [C.psb[6]], "matmul", C.ps[6][:, 0:130], lhsT=r32(kg), rhs=r32(vext),
                     start=True, stop=True)
                P.op("dve", [cextb[h], gb_], [cextb[h]], "tensor_scalar", out=r32(cext[h]), in0=cext[h], scalar1=eb_c,
                     scalar2=None, op0=ALU.mult)
                P.op("dve", [C.psb[6], cextb[h], gb_], [cextb[h]], "scalar_tensor_tensor", out=r32(cext[h]),
                     in0=C.ps[6][:, 0:130], scalar=eb_c, in1=cext[h], op0=ALU.mult, op1=ALU.add)
        for h in range(4):
            for hh in range(2):
                P.dma("pool", yT[h * 128 + hh * 64:h * 128 + (hh + 1) * 64, ts_], ymT[hh * 64:(hh + 1) * 64, h, :],
                      [ymTb], [yTb[2 * h + hh]])
    P.barrier()
    A.reset()
    N.reset()
    ustr = A.alloc(128)
    onesr = A.alloc(128)
    identr = A.alloc(128)
    scb_ = Buf("sconst")
    P.dma("sp", r32(ustr), r32(C.consts["ustr"]), [], [scb_])
    P.dma("sp", r32(onesr), r32(C.consts["ones128"]), [], [scb_])
    P.dma("sp", r32(identr), r32(C.consts["ident"]), [], [scb_])
    maskd = N.alloc3(4, 512)
    maskdb = Buf("maskd")
    P.dma("sp", maskd, C.consts["maskd"].rearrange("p (i q) -> p i q", i=4), [], [maskdb])
    zer = N.alloc(128)
    zerb = Buf("zer")
    P.op("dve", [], [zerb], "memset", zer, 0.0)
    qTs = [A.alloc(S, 64) for i in range(2)]
    kTs = [A.alloc(S, 64) for i in range(2)]
    vTs = [N.alloc(S, 64) for i in range(2)]
    hbufs = [[Buf("sq%d" % i), Buf("sk%d" % i), Buf("sv%d" % i)] for i in range(2)]
    vtoks = [A.alloc3(32, 128) for i in range(2)]
    vtokbs = [Buf("vtok%d" % i) for i in range(2)]
    for j in range(2):
        for kb in range(32):
            P.op("dve", [zerb], [vtokbs[j]], "tensor_copy", out=r32(vtoks[j][:, kb, 64:128]), in_=zer[:, 0:64])
    e_ = N.alloc(1024)
    eb_ = Buf("e")
    tts = [N.alloc(1024) for i in range(2)]
    ttbs = [Buf("tt%d" % i) for i in range(2)]
    nLs = [A.alloc(1024) for i in range(2)]
    nLbs = [Buf("nL%d" % i) for i in range(2)]
    Ats = [A.alloc(1024) for i in range(2)]
    Atbs = [Buf("At%d" % i) for i in range(2)]
    Rs = [A.alloc(512) for i in range(2)]
    Rbs = [Buf("R%d" % i) for i in range(2)]
    yos = [N.alloc(512, 64) for i in range(2)]
    yobs = [Buf("syo%d" % i) for i in range(2)]
    z2 = [C.psall[:, 0:1024], C.psall[:, 1024:2048]]
    z2b = [[C.psb[0], C.psb[1]], [C.psb[2], C.psb[3]]]
    nc2 = C.psall[:, 2048:3072]
    nc2b = [C.psb[4], C.psb[5]]
    steps = []
    for h in range(8):
        for Q in range(8):
            for G2 in range(2 * Q + 1, -1, -1):
                steps.append({"h": h, "Q": Q, "G2": G2, "first": G2 == 2 * Q + 1, "last": G2 == 0})
    cnt = {"r": 0, "y": 0}

    def prologue(h):
        hi = h % 2
        qT, kT, vT = qTs[hi], kTs[hi], vTs[hi]
        qTb, kTb, vTb = hbufs[hi]
        vtok, vtokb = vtoks[hi], vtokbs[hi]
        rq, rk, rv = 2056 + h * 64, 2568 + h * 64, 3080 + h * 64
        cidx = lambda r0: projb[17 + (r0 - 2056) // 128]
        P.dma("sp", r32(qT), r32(projT[rq:rq + 64, :]), cidx(rq), [qTb])
        P.dma("sp", r32(kT), r32(projT[rk:rk + 64, :]), cidx(rk), [kTb])
        P.dma("sp", vT, projT[rv:rv + 64, :], cidx(rv), [vTb])
        for rd in range(4):
            for i in range(8):
                kb = rd * 8 + i
                P.op("pe", [vTb, C.identb], [C.psb[6]], "transpose", out=C.ps[6][:, i * 64:(i + 1) * 64],
                     in_=vT[:, kb * 128:(kb + 1) * 128], identity=C.ident[0:64, 0:64])
            P.op("dve", [C.psb[6]], [vtokb], "tensor_copy", out=r32(vtok[:, rd * 8:(rd + 1) * 8, 0:64]),
                 in_=C.ps[6][:, :].rearrange("p (i e) -> p i e", e=64))

    def stage_a(k):
        St = steps[k]
        h, Q, G2 = St["h"], St["Q"], St["G2"]
        if Q == 0 and G2 == 1:
            prologue(h)
        hi = h % 2
        qT, kT = qTs[hi], kTs[hi]
        qTb, kTb, _ = hbufs[hi]
        qs = qT[:, Q * 512:(Q + 1) * 512]
        kbs = [2 * G2 + 1, 2 * G2]
        zi = k % 2
        z, zb = z2[zi], z2b[zi]
        nL, nLb = nLs[zi], nLbs[zi]
        for i, kb in enumerate(kbs):
            P.op("pe", [kTb, qTb], [zb[i]], "matmul", z[:, i * 512:(i + 1) * 512],
                 lhsT=r32(kT[:, kb * 128:(kb + 1) * 128]), rhs=r32(qs), start=True, stop=True)
        P.op("act", zb, [eb_], "activation", out=e_, in_=z, func=AF.Exp, scale=0.125)
        P.op("act", [eb_], [nLb], "activation", out=r32(nL), in_=e_, func=AF.Ln, bias=1.0)
        if G2 >= 2 * Q:
            for i, kb in enumerate(kbs):
                ib = kb - 4 * Q
                P.op("dve", [nLb, maskdb], [nLb], "tensor_tensor", out=r32(nL[:, i * 512:(i + 1) * 512]),
                     in0=nL[:, i * 512:(i + 1) * 512], in1=maskd[:, ib, :], op=ALU.mult)

    def stage_b(k):
        St = steps[k]
        h, Q, G2, first, last = St["h"], St["Q"], St["G2"], St["first"], St["last"]
        kbs = [2 * G2 + 1, 2 * G2]
        zi = k % 2
        z, zb = z2[zi], z2b[zi]
        nL, nLb = nLs[zi], nLbs[zi]
        At, Atb = Ats[zi], Atbs[zi]
        tt, ttb = tts[zi], ttbs[zi]
        if first:
            Rcur, Rcurb = None, None
        else:
            Rcur, Rcurb = steps[k - 1]["Rn"]
        if not last:
            P.op("pe", [nLb, scb_], [C.psb[6]], "matmul", C.ps[6][:, :], lhsT=r32(onesr), rhs=r32(nL[:, 0:512]),
                 start=True, stop=False)
            P.op("pe", [nLb, scb_], [C.psb[6]], "matmul", C.ps[6][:, :], lhsT=r32(onesr),
                 rhs=r32(nL[:, 512:1024]), start=False, stop=first)
            if not first:
                P.op("pe", [Rcurb, scb_], [C.psb[6]], "matmul", C.ps[6][:, :], lhsT=r32(identr), rhs=r32(Rcur),
                     start=False, stop=True)
            Rn, Rnb = Rs[cnt["r"] % 2], Rbs[cnt["r"] % 2]
            cnt["r"] += 1
            P.op("act", [C.psb[6]], [Rnb], "copy", out=r32(Rn), in_=C.ps[6][:, :])
            St["Rn"] = (Rn, Rnb)
        P.op("pe", [nLb, scb_], [nc2b[0]], "matmul", nc2[:, 0:512], lhsT=r32(ustr), rhs=r32(nL[:, 0:512]),
             start=True, stop=first)
        if not first:
            P.op("pe", [Rcurb, scb_], [nc2b[0]], "matmul", nc2[:, 0:512], lhsT=r32(identr), rhs=r32(Rcur),
                 start=False, stop=True)
        P.op("pe", [nLb, scb_], [nc2b[1]], "matmul", nc2[:, 512:1024], lhsT=r32(ustr),
             rhs=r32(nL[:, 512:1024]), start=True, stop=False)
        P.op("pe", [nLb, scb_], [nc2b[1]], "matmul", nc2[:, 512:1024], lhsT=r32(onesr),
             rhs=r32(nL[:, 0:512]), start=False, stop=first)
        if not first:
            P.op("pe", [Rcurb, scb_], [nc2b[1]], "matmul", nc2[:, 512:1024], lhsT=r32(identr), rhs=r32(Rcur),
                 start=False, stop=True)
        P.op("dve", zb + [nLb], [ttb], "scalar_tensor_tensor", out=tt, in0=z, scalar=0.125, in1=nL,
             op0=ALU.mult, op1=ALU.subtract)
        P.op("dve", [ttb] + nc2b, [ttb], "tensor_tensor", out=tt, in0=tt, in1=nc2, op=ALU.subtract)
        P.op("act", [ttb], [Atb], "activation", out=r32(At), in_=tt, func=AF.Exp)
        if G2 >= 2 * Q:
            for i, kb in enumerate(kbs):
                ib = kb - 4 * Q
                P.op("dve", [Atb, maskdb], [Atb], "tensor_tensor", out=r32(At[:, i * 512:(i + 1) * 512]),
                     in0=At[:, i * 512:(i + 1) * 512], in1=maskd[:, ib, :], op=ALU.mult)

    def stage_c(k):
        St = steps[k]
        h, Q, G2, first, last = St["h"], St["Q"], St["G2"], St["first"], St["last"]
        kbs = [2 * G2 + 1, 2 * G2]
        zi = k % 2
        At, Atb = Ats[zi], Atbs[zi]
        vtok, vtokb = vtoks[h % 2], vtokbs[h % 2]
        for i, kb in enumerate(kbs):
            P.op("pe", [vtokb, Atb], [C.psb[7]], "matmul", C.ps[7][:, :], lhsT=r32(vtok[:, kb, :]),
                 rhs=r32(At[:, i * 512:(i + 1) * 512]), start=(first and i == 0), stop=(last and i == 1))
        if last:
            yo, yob = yos[cnt["y"] % 2], yobs[cnt["y"] % 2]
            cnt["y"] += 1
            P.op("act", [C.psb[7]], [yob], "copy", out=yo, in_=C.ps[7][0:64, :])
            P.dma("pool", yT[512 + h * 64:512 + (h + 1) * 64, Q * 512:(Q + 1) * 512], yo, [yob], [yTb[8 + h]])

    ns = len(steps)
    for it in range(ns + 2):
        if it < ns:
            stage_a(it)
        if 1 <= it <= ns:
            stage_b(it - 1)
        if it >= 2:
            stage_c(it - 2)
    out_proj_norm(C, yT, yTb, prm["l1_mix_w_out"], prm["l1_ln2_g"], prm["l1_ln2_b"], x_in, x_in_bufs, x_out, x_out_bufs)


PARAM_NAMES = [
    "l0_ffn1_w_in", "l0_ffn1_w_out", "l0_ln1_g", "l0_ln1_b",
    "l0_mix_w_in", "l0_conv_w", "l0_mix_w_out", "l0_ln2_g", "l0_ln2_b",
    "l0_ffn2_w_in", "l0_ffn2_w_out", "l0_ln3_g", "l0_ln3_b",
    "l1_ffn1_w_in", "l1_ffn1_w_out", "l1_ln1_g", "l1_ln1_b",
    "l1_mix_w_in", "l1_mlstm_b_i", "l1_mlstm_b_f", "l1_mlstm_norm_g", "l1_mix_w_out",
    "l1_ln2_g", "l1_ln2_b",
    "l1_ffn2_w_in", "l1_ffn2_w_out", "l1_ln3_g", "l1_ln3_b",
]
PARAM_SHAPES = {
    "ffn1_w_in": [D, 2 * DFF], "ffn2_w_in": [D, 2 * DFF], "ffn1_w_out": [DFF, D], "ffn2_w_out": [DFF, D],
    "l0_mix_w_in": [D, 3072], "l1_mix_w_in": [D, 3592], "mix_w_out": [D, D], "l0_conv_w": [3, 256],
    "l1_mlstm_b_i": [4], "l1_mlstm_b_f": [4], "l1_mlstm_norm_g": [512],
}


def pshape(n):
    if n in PARAM_SHAPES:
        return PARAM_SHAPES[n]
    k = n[3:]
    if k in PARAM_SHAPES:
        return PARAM_SHAPES[k]
    return [D]


def build(nsub=6):
    nc = bass.Bass("TRN2", target_bir_lowering=False)
    nc.dge_precook = False
    x_d = nc.dram_tensor("x", [S, D], F32, kind="ExternalInput").ap()
    prm = {n: nc.dram_tensor(n, pshape(n), F32, kind="ExternalInput").ap() for n in PARAM_NAMES}
    consts = {n: nc.dram_tensor(n, list(a.shape), F32, kind="ExternalInput").ap() for n, a in CONSTS.items()}
    consts["conv_wT"] = nc.dram_tensor("conv_wT", [256, 3], F32, kind="ExternalInput").ap()
    out_d = nc.dram_tensor("out", [S, D], F32, kind="ExternalOutput").ap()
    skind = "ExternalOutput" if DEBUG_OUT else "Internal"
    xa = nc.dram_tensor("xa", [S, D], F32, kind=skind).ap()
    xb = nc.dram_tensor("xb", [S, D], F32, kind=skind).ap()
    projT = nc.dram_tensor("projT", [3712, S], F32, kind="Internal").ap()
    yT = nc.dram_tensor("yT", [D, S], F32, kind=skind).ap()
    with ExitStack() as stack:
        P = Prog(nc, stack)
        C = setup_common(P, nc, consts)

        def mkbufs(name, n):
            sem = P.new_sem("d_" + name, True)
            return [Buf("%s%d" % (name, i), sem) for i in range(n)]
        bufs = {"x": mkbufs("x", 32), "xa": mkbufs("xa", 32), "xb": mkbufs("xb", 32), "out": mkbufs("out", 32)}
        aps = {"x": x_d, "xa": xa, "xb": xb, "out": out_d}
        pb = mkbufs("projT", 29 * 8)
        scr = {"projT": projT, "projb": [[pb[c * 8 + t] for t in range(8)] for c in range(29)],
               "yT": yT, "yTb": mkbufs("yT", 16)}
        plan = ["ffn1", "mix", "ffn2"] * 2
        cur = "x"
        for si in range(nsub):
            layer = si // 3
            kind = plan[si]
            nxt = "out" if si == nsub - 1 else ("xa" if cur != "xa" else "xb")
            p = "l%d_" % layer
            if kind in ("ffn1", "ffn2"):
                lnn = "ln1" if kind == "ffn1" else "ln3"
                ffn_sublayer(C, aps[cur], bufs[cur], aps[nxt], bufs[nxt],
                             prm[p + kind + "_w_in"], prm[p + kind + "_w_out"], prm[p + lnn + "_g"], prm[p + lnn + "_b"])
            elif layer == 0:
                even_mixer(C, aps[cur], bufs[cur], aps[nxt], bufs[nxt], prm, scr)
            else:
                odd_mixer(C, aps[cur], bufs[cur], aps[nxt], bufs[nxt], prm, scr)
            cur = nxt
        P.final_wait("pool", bufs["out"])
        P.emit()
    return nc


def _mk_consts():
    c = {}
    c["ident"] = np.eye(128, dtype=np.float32)
    k = np.arange(128)[:, None]
    q = np.arange(128)[None, :]
    m = np.zeros((128, 256), np.float32)
    m[:, 0:128] = np.where(k <= q, 0.0, NEGM)
    m[:, 128:256] = np.where(k >= q, 0.0, NEGM)
    c["maskT"] = m
    sel = np.zeros((65, 64), np.float32)
    sel[64, :] = 1.0
    c["sel"] = sel
    a = np.arange(128)
    c["triu"] = (a[:, None] <= a[None, :]).astype(np.float32)
    c["ustr"] = (a[:, None] > a[None, :]).astype(np.float32)
    c["ones128"] = np.ones((128, 128), np.float32)
    kk = (np.arange(4)[None, :, None] * 128 + a[:, None, None])
    qq = np.arange(512)[None, None, :]
    c["maskd"] = (kk < qq).astype(np.float32).reshape(128, 2048)
    return c


CONSTS = _mk_consts()


def run(inputs, nsub=6, trace=False):
    nc = build(nsub)
    x = np.ascontiguousarray(inputs["x"], dtype=np.float32)
    in_maps = []
    for c in range(NCORES):
        m = {"x": x[c]}
        m.update(CONSTS)
        m["conv_wT"] = np.ascontiguousarray(np.asarray(inputs["l0_conv_w"], dtype=np.float32).T)
        for n in PARAM_NAMES:
            m[n] = np.ascontiguousarray(inputs[n], dtype=np.float32)
        in_maps.append(m)
    res = run_bass_kernel_spmd(nc, in_maps, core_ids=list(range(NCORES)), trace=trace)
    out = np.stack([res.results[c]["out"] for c in range(NCORES)], axis=0)
    if DEBUG_OUT:
        global LAST_DEBUG
        LAST_DEBUG = {n: res.results[0][n] for n in ("xa", "xb", "yT") if n in res.results[0]}
    return out, res


def kernel(**inputs):
    out, _ = run(inputs, 6)
    return out.astype(np.float32)
```

```python
import numpy as np
from contextlib import ExitStack
import concourse.bass as bass
import concourse.mybir as mybir
from concourse.bass_utils import run_bass_kernel_spmd

F32 = mybir.dt.float32
F32R = mybir.dt.float32r
AF = mybir.ActivationFunctionType
ALU = mybir.AluOpType
AX = mybir.AxisListType

D = 1024
S = 4096
DFF = 2816
NCH = DFF // 128
ALPHA = 4 ** 0.25
LN_EPS = 1e-5
NCORES = 8
DEBUG_OUT = False


class Buf:
    __slots__ = ("name", "w", "r", "dsem")

    def __init__(self, name, dsem=None):
        self.name = name
        self.w = {}
        self.r = {}
        self.dsem = dsem


class Eng:
    def __init__(self, name, sem):
        self.name = name
        self.sem = sem
        self.waited = {}
        self.insts = []


class SemC:
    def __init__(self, handle, is_dma):
        self.h = handle
        self.count = 0
        self.is_dma = is_dma


class Prog:
    def __init__(self, nc, stack):
        self.nc = nc
        self.stack = stack
        self.nsem = 0
        self.dma_sems = []
        self.free_sems = []
        self.phase_sems = []
        self.eng = {}
        for n in ("pe", "act", "dve", "pool", "sp"):
            self.eng[n] = Eng(n, self.new_sem(n, False))

    def new_sem(self, name, is_dma):
        self.nsem += 1
        h = self.stack.enter_context(self.nc.semaphore("s_%s_%d" % (name, self.nsem)))
        sc = SemC(h, is_dma)
        if is_dma:
            self.dma_sems.append(sc)
        return sc

    def sbuf(self, name, shape, dtype=F32):
        return self.stack.enter_context(self.nc.sbuf_tensor("sb_" + name, shape, dtype))

    def psum(self, name, shape, dtype=F32):
        return self.stack.enter_context(self.nc.psum_tensor("pt_" + name, shape, dtype))

    def _deps(self, e, reads, writes):
        need = {}

        def add(d):
            for s, v in d.items():
                if s.is_dma:
                    v = s.count
                if need.get(s, 0) < v:
                    need[s] = v
        for b in reads:
            add(b.w)
        for b in writes:
            add(b.w)
            add(b.r)
        waits = []
        for s, v in need.items():
            if s is e.sem and e.name == "pe":
                continue
            if e.waited.get(s, 0) >= v:
                continue
            e.waited[s] = v
            waits.append((s.h, v))
        return waits

    def op(self, en, reads, writes, meth, *args, **kwargs):
        e = self.eng[en]
        fn = (lambda h, meth=meth, args=args, kwargs=kwargs: getattr(h, meth)(*args, **kwargs))
        waits = self._deps(e, reads, writes)
        e.sem.count += 1
        v = e.sem.count
        e.insts.append((waits, fn, e.sem.h, 1))
        for b in reads:
            b.r[e.sem] = v
        for b in writes:
            b.w[e.sem] = v

    def dma(self, en, out, in_, reads, writes):
        e = self.eng[en]
        assert len(writes) == 1
        dst = writes[0]
        if dst.dsem is None:
            if self.free_sems:
                dst.dsem = self.free_sems.pop()
            else:
                dst.dsem = self.new_sem("d_" + dst.name, True)
            self.phase_sems.append(dst.dsem)
        waits = self._deps(e, reads, writes)
        dst.dsem.count += 16
        v = dst.dsem.count
        e.insts.append((waits, lambda h: h.dma_start(out=out, in_=in_), dst.dsem.h, 16))
        for b in reads:
            b.r[dst.dsem] = v
        dst.w[dst.dsem] = v

    def barrier(self):
        allsems = [e.sem for e in self.eng.values()] + self.dma_sems
        for e in self.eng.values():
            waits = []
            for sc in allsems:
                if sc is e.sem or sc.count == 0:
                    continue
                if e.waited.get(sc, 0) >= sc.count:
                    continue
                e.waited[sc] = sc.count
                waits.append((sc.h, sc.count))
            if waits:
                e.insts.append((waits, None, None, 0))
        self.free_sems.extend(self.phase_sems)
        self.phase_sems = []

    def final_wait(self, en, bufs):
        e = self.eng[en]
        waits = self._deps(e, bufs, ())
        e.insts.append((waits, None, None, 0))

    def emit(self):
        nc = self.nc
        with nc.Block() as block:
            def mk(e):
                def body(h):
                    for waits, fn, sem, inc in e.insts:
                        for sh, v in waits:
                            h.wait_ge(sh, v)
                        if fn is not None:
                            fn(h).then_inc(sem, inc)
                return body
            block.tensor(mk(self.eng["pe"]))
            block.scalar(mk(self.eng["act"]))
            block.vector(mk(self.eng["dve"]))
            block.gpsimd(mk(self.eng["pool"]))
            block.sync(mk(self.eng["sp"]))


def r32(ap):
    return ap.bitcast(F32R)


class Ctx:
    pass


class Arena:
    def __init__(self, P, nelem, name="arena"):
        self.t = P.sbuf(name, [128, nelem])
        self.n = nelem
        self.off = 0

    def reset(self):
        self.off = 0

    def alloc(self, n, parts=128):
        n += n % 2
        assert self.off + n <= self.n, (self.off, n, self.n)
        ap = self.t[0:parts, self.off:self.off + n]
        self.off += n
        return ap

    def alloc3(self, k, n, parts=128):
        return self.alloc(k * n, parts).rearrange("p (k n) -> p k n", k=k)


def setup_common(P, nc, consts):
    C = Ctx()
    C.P = P
    C.nc = nc
    C.psall = P.psum("psall", [128, 4096])
    C.ps = [C.psall[:, i * 512:(i + 1) * 512] for i in range(8)]
    C.psb = [Buf("ps%d" % i) for i in range(8)]
    C.ident = P.sbuf("ident", [128, 128])
    C.identb = Buf("ident")
    P.dma("sp", C.ident[:, :], consts["ident"], [], [C.identb])
    C.mhalf = P.sbuf("mhalf", [128, 1])
    C.mhalfb = Buf("mhalf")
    P.op("dve", [], [C.mhalfb], "memset", C.mhalf[:, :], -0.5)
    C.one = P.sbuf("one", [128, 64])
    C.oneb = Buf("one")
    P.op("dve", [], [C.oneb], "memset", C.one[:, :], 1.0)
    C.A = Arena(P, 30720, "arenaR")
    C.N = Arena(P, 15600, "arenaN")
    C.consts = consts
    return C


def load_gb(C, g_d, b_d, gt, bt, gb_b):
    P = C.P
    P.dma("sp", gt, g_d.partition_broadcast(128), [], [gb_b[0]])
    P.dma("sp", bt, b_d.partition_broadcast(128), [], [gb_b[1]])


class Work:
    pass


def alloc_norm(C, W):
    A = C.N
    W.z = [A.alloc(D) for i in range(4)]
    W.zb = [Buf("z%d" % i) for i in range(4)]
    W.st = [A.alloc(16) for i in range(4)]
    W.stb = [Buf("st%d" % i) for i in range(4)]
    W.gt = A.alloc(D)
    W.bt = A.alloc(D)
    W.gbb = [Buf("gt"), Buf("bt")]
    W.zcnt = 0


def post_norm_evac(C, W, y_banks, xs, xsb):
    P = C.P
    zi = W.zcnt
    W.zcnt += 1
    z = W.z[zi % 4]
    zb = W.zb[zi % 4]
    for hh in range(2):
        yb = y_banks[hh]
        P.op("dve", [xsb, C.psb[yb]], [zb], "scalar_tensor_tensor",
             out=z[:, hh * 512:(hh + 1) * 512], in0=xs[:, hh * 512:(hh + 1) * 512], scalar=ALPHA,
             in1=C.ps[yb][:, :], op0=ALU.mult, op1=ALU.add)
    return zi


def post_norm_finish(C, W, zi, x_out_rows, x_out_buf):
    post_norm_finish_a(C, W, zi)
    post_norm_finish_b(C, W, zi, x_out_rows, x_out_buf)


def post_norm_finish_a(C, W, zi):
    P = C.P
    z = W.z[zi % 4]
    zb = W.zb[zi % 4]
    st = W.st[zi % 4]
    stb = W.stb[zi % 4]
    for hh in range(2):
        P.op("dve", [zb], [stb], "bn_stats", out=st[:, hh * 6:(hh + 1) * 6], in_=z[:, hh * 512:(hh + 1) * 512])
    P.op("dve", [stb], [stb], "bn_aggr", out=st[:, 12:14], in_=st[:, 0:12])
    P.op("dve", [stb], [stb], "tensor_scalar", out=st[:, 14:15], in0=st[:, 13:14], scalar1=LN_EPS, scalar2=None,
         op0=ALU.add)
    P.op("pool", [stb, C.mhalfb], [stb], "tensor_tensor", out=st[:, 15:16], in0=st[:, 14:15], in1=C.mhalf[:, 0:1],
         op=ALU.pow)


def post_norm_finish_b(C, W, zi, x_out_rows, x_out_buf):
    P = C.P
    z = W.z[zi % 4]
    zb = W.zb[zi % 4]
    st = W.st[zi % 4]
    stb = W.stb[zi % 4]
    gt, bt, gb_b = W.gt, W.bt, W.gbb
    P.op("dve", [zb, stb], [zb], "tensor_scalar", out=z, in0=z, scalar1=st[:, 12:13],
         scalar2=st[:, 15:16], op0=ALU.subtract, op1=ALU.mult)
    P.op("dve", [zb, gb_b[0]], [zb], "tensor_tensor", out=z, in0=z, in1=gt, op=ALU.mult)
    P.op("dve", [zb, gb_b[1]], [zb], "tensor_tensor", out=z, in0=z, in1=bt, op=ALU.add)
    P.dma("pool", x_out_rows, z, [zb], [x_out_buf])


def post_norm_tile(C, W, y_banks, xs, xsb, x_out_rows, x_out_buf):
    zi = post_norm_evac(C, W, y_banks, xs, xsb)
    post_norm_finish(C, W, zi, x_out_rows, x_out_buf)


def alloc_xT(C, W, nbuf=2):
    A = C.A
    W.xs = [C.N.alloc(D) for i in range(4)]
    W.xsb = [Buf("xs%d" % i) for i in range(4)]
    W.nxT = nbuf
    W.xT = [A.alloc3(8, 512) for i in range(nbuf)]
    W.xTb = [[Buf("xT%d_%d" % (i, k)) for k in range(8)] for i in range(nbuf)]
    W.xscnt = 0
    W.xTcnt = 0


def load_xT(C, W, x_in, x_in_bufs, t):
    P = C.P
    xTi = W.xTcnt % W.nxT
    W.xTcnt += 1
    xT = W.xT[xTi]
    xTb = W.xTb[xTi]
    xsl = []
    for s in range(4):
        i = W.xscnt % 4
        W.xscnt += 1
        r0 = t * 512 + s * 128
        P.dma("sp", W.xs[i], x_in[r0:r0 + 128, :], [x_in_bufs[t * 4 + s]], [W.xsb[i]])
        xsl.append(i)
    for kc in range(8):
        bank = kc % 2
        for s in range(4):
            i = xsl[s]
            P.op("pe", [W.xsb[i], C.identb], [C.psb[bank]], "transpose",
                 out=C.ps[bank][:, s * 128:(s + 1) * 128], in_=W.xs[i][:, kc * 128:(kc + 1) * 128],
                 identity=C.ident[:, :])
        if kc % 2 == 0:
            P.op("act", [C.psb[bank]], [xTb[kc]], "copy", out=r32(xT[:, kc, :]), in_=C.ps[bank][:, :])
        else:
            P.op("dve", [C.psb[bank]], [xTb[kc]], "tensor_copy", out=r32(xT[:, kc, :]), in_=C.ps[bank][:, :])
    return xT, xTb


def reload_xs(C, W, x_in, x_in_bufs, row_tile):
    P = C.P
    i = W.xscnt % 4
    W.xscnt += 1
    r0 = row_tile * 128
    P.dma("sp", W.xs[i], x_in[r0:r0 + 128, :], [x_in_bufs[row_tile]], [W.xsb[i]])
    return W.xs[i], W.xsb[i]


def ffn_sublayer(C, x_in, x_in_bufs, x_out, x_out_bufs, w_in, w_out, g_d, b_d):
    P = C.P
    A = C.A
    P.barrier()
    A.reset()
    C.N.reset()
    W = Work()
    alloc_norm(C, W)
    xT = A.alloc3(8, 512)
    xTb = [Buf("xT_%d" % k) for k in range(8)]
    xss = [[C.N.alloc(D) for i in range(4)] for j in range(2)]
    xssb = [[Buf("xs%d_%d" % (j, i)) for i in range(4)] for j in range(2)]

    def issue_loads(t):
        for s_ in range(4):
            r0 = t * 512 + s_ * 128
            P.dma("sp", xss[t % 2][s_], x_in[r0:r0 + 128, :], [x_in_bufs[t * 4 + s_]], [xssb[t % 2][s_]])
    aT = A.alloc3(NCH, 512)
    aTb = [Buf("aT%d" % j) for j in range(NCH)]
    wins = [A.alloc(8 * 2 * 256).rearrange("p (k h n) -> p k h n", k=8, h=2) for i in range(3)]
    winbs = [Buf("win%d" % i) for i in range(3)]
    wouts = [A.alloc(D) for i in range(3)]
    woutbs = [Buf("wout%d" % i) for i in range(3)]
    sgs = [C.N.alloc(512) for i in range(2)]
    sgbs = [Buf("sg%d" % i) for i in range(2)]
    cnt = {"win": 0, "wout": 0, "sg": 0}
    load_gb(C, g_d, b_d, W.gt, W.bt, W.gbb)
    w_in_v = w_in.rearrange("(kc p) n -> p kc n", p=128)
    NT = S // 512
    issue_loads(0)
    pending = []
    for t in range(NT):
        for kc in range(8):
            bank = kc % 2
            for s_ in range(4):
                P.op("pe", [xssb[t % 2][s_], C.identb], [C.psb[bank]], "transpose",
                     out=C.ps[bank][:, s_ * 128:(s_ + 1) * 128], in_=xss[t % 2][s_][:, kc * 128:(kc + 1) * 128],
                     identity=C.ident[:, :])
            P.op("act", [C.psb[bank]], [xTb[kc]], "copy", out=r32(xT[:, kc, :]), in_=C.ps[bank][:, :])
        for grp in range(NCH // 2):
            if pending and grp >= 1:
                if grp in (1, 3, 5, 7):
                    post_norm_finish_a(C, W, pending[(grp - 1) // 2][0])
                if grp in (2, 4, 6, 8):
                    pz, prt = pending[(grp - 2) // 2]
                    post_norm_finish_b(C, W, pz, x_out[prt * 128:(prt + 1) * 128, :], x_out_bufs[prt])
                if grp == 8:
                    pending = []
            wi = cnt["win"] % 3
            cnt["win"] += 1
            win = wins[wi]
            winb = winbs[wi]
            P.dma("sp", r32(win[:, :, 0, :]), r32(w_in_v[:, :, grp * 256:(grp + 1) * 256]), [], [winb])
            P.dma("sp", r32(win[:, :, 1, :]), r32(w_in_v[:, :, DFF + grp * 256:DFF + (grp + 1) * 256]), [], [winb])
            for jj in range(2):
                j = grp * 2 + jj
                gb_ = 2 + 2 * (j % 2)
                ub_ = 3 + 2 * (j % 2)
                for half, bank in ((0, gb_), (1, ub_)):
                    for kc in range(8):
                        P.op("pe", [winb, xTb[kc]], [C.psb[bank]], "matmul",
                             C.ps[bank][:, :], lhsT=r32(win[:, kc, half, jj * 128:(jj + 1) * 128]),
                             rhs=r32(xT[:, kc, :]), start=(kc == 0), stop=(kc == 7))
                si = cnt["sg"] % 2
                cnt["sg"] += 1
                sg = sgs[si]
                P.op("act", [C.psb[gb_]], [sgbs[si]], "activation", out=sg, in_=C.ps[gb_][:, :], func=AF.Silu)
                P.op("dve", [sgbs[si], C.psb[ub_]], [aTb[j]], "scalar_tensor_tensor",
                     out=r32(aT[:, j, :]), in0=sg, scalar=0.5, in1=C.ps[ub_][:, :], op0=ALU.mult, op1=ALU.mult)
        if t + 1 < NT:
            issue_loads(t + 1)
        for j in range(NCH):
            wi = cnt["wout"] % 3
            cnt["wout"] += 1
            wout = wouts[wi]
            woutb = woutbs[wi]
            P.dma("sp", r32(wout), r32(w_out[j * 128:(j + 1) * 128, :]), [], [woutb])
            for s in range(4):
                for hh in range(2):
                    bank = 2 * s + hh
                    P.op("pe", [aTb[j], woutb], [C.psb[bank]], "matmul",
                         C.ps[bank][:, :], lhsT=r32(aT[:, j, s * 128:(s + 1) * 128]),
                         rhs=r32(wout[:, hh * 512:(hh + 1) * 512]), start=(j == 0), stop=(j == NCH - 1))
        zis = []
        for s in range(4):
            rt = t * 4 + s
            zis.append(post_norm_evac(C, W, (2 * s, 2 * s + 1), xss[t % 2][s], xssb[t % 2][s]))
        pending = [(zis[s], t * 4 + s) for s in range(4)]
    for pz, prt in pending:
        post_norm_finish(C, W, pz, x_out[prt * 128:(prt + 1) * 128, :], x_out_bufs[prt])


def out_proj_norm(C, yT_d, yT_bufs, w_out, g_d, b_d, x_in, x_in_bufs, x_out, x_out_bufs):
    P = C.P
    A = C.A
    P.barrier()
    A.reset()
    C.N.reset()
    W = Work()
    alloc_norm(C, W)
    W.xs = [C.N.alloc(D) for i in range(4)]
    W.xsb = [Buf("xs%d" % i) for i in range(4)]
    W.xscnt = 0
    wo = A.alloc3(8, D)
    wob = Buf("wo")
    P.dma("sp", r32(wo), r32(w_out.rearrange("(kc p) n -> p kc n", p=128)), [], [wob])
    yts = [A.alloc3(8, 512) for i in range(2)]
    ytbs = [Buf("yt%d" % i) for i in range(2)]
    load_gb(C, g_d, b_d, W.gt, W.bt, W.gbb)
    yT_v = yT_d.rearrange("(kc p) n -> p kc n", p=128)
    pend = None
    for t in range(S // 512):
        yt = yts[t % 2]
        ytb = ytbs[t % 2]
        P.dma("sp", r32(yt), r32(yT_v[:, :, t * 512:(t + 1) * 512]), yT_bufs, [ytb])
        for s in range(4):
            rt = t * 4 + s
            banks = (2 * (rt % 4), 2 * (rt % 4) + 1)
            for hh in range(2):
                for kc in range(8):
                    P.op("pe", [ytb, wob], [C.psb[banks[hh]]], "matmul", C.ps[banks[hh]][:, :],
                         lhsT=r32(yt[:, kc, s * 128:(s + 1) * 128]), rhs=r32(wo[:, kc, hh * 512:(hh + 1) * 512]),
                         start=(kc == 0), stop=(kc == 7))
            xs, xsb = reload_xs(C, W, x_in, x_in_bufs, rt)
            zi = post_norm_evac(C, W, banks, xs, xsb)
            post_norm_finish_a(C, W, zi)
            if pend is not None:
                post_norm_finish_b(C, W, pend[0], x_out[pend[1] * 128:(pend[1] + 1) * 128, :], x_out_bufs[pend[1]])
            pend = (zi, rt)
    post_norm_finish_b(C, W, pend[0], x_out[pend[1] * 128:(pend[1] + 1) * 128, :], x_out_bufs[pend[1]])


def proj_phase(C, x_in, x_in_bufs, w_in, ccols, small_ci, projT, projb):
    P = C.P
    A = C.A
    N = C.N
    P.barrier()
    A.reset()
    N.reset()
    xs = [N.alloc(D) for i in range(4)]
    xsb = [Buf("xs%d" % i) for i in range(4)]
    evs = [N.alloc(512) for i in range(4)]
    evbs = [Buf("ev%d" % i) for i in range(4)]
    HT = 2048
    xT = A.alloc3(8, HT)
    xTb = [[Buf("xTb%d_%d" % (tt, kc)) for kc in range(8)] for tt in range(4)]
    wcs = [A.alloc3(8, 128) for i in range(3)]
    wcbs = [Buf("wc%d" % i) for i in range(3)]
    w_in_v = w_in.rearrange("(kc p) n -> p kc n", p=128)
    xc = 0
    k = 0
    wk = 0
    for half in range(2):
        for tt in range(4):
            t = half * 4 + tt
            xsl = []
            for s_ in range(4):
                i = xc % 4
                xc += 1
                r0 = t * 512 + s_ * 128
                P.dma("sp", xs[i], x_in[r0:r0 + 128, :], [x_in_bufs[t * 4 + s_]], [xsb[i]])
                xsl.append(i)
            for kc in range(8):
                bank = kc % 2
                for s_ in range(4):
                    i = xsl[s_]
                    P.op("pe", [xsb[i], C.identb], [C.psb[bank]], "transpose",
                         out=C.ps[bank][:, s_ * 128:(s_ + 1) * 128], in_=xs[i][:, kc * 128:(kc + 1) * 128],
                         identity=C.ident[:, :])
                dst = r32(xT[:, kc, tt * 512:(tt + 1) * 512])
                if kc % 2 == 0:
                    P.op("act", [C.psb[bank]], [xTb[tt][kc]], "copy", out=dst, in_=C.ps[bank][:, :])
                else:
                    P.op("dve", [C.psb[bank]], [xTb[tt][kc]], "tensor_copy", out=dst, in_=C.ps[bank][:, :])
        for ci, c0 in enumerate(ccols):
            nrow = 8 if ci == small_ci else 128
            wc = wcs[wk % 3]
            wcb = wcbs[wk % 3]
            wk += 1
            P.dma("sp", r32(wc), r32(w_in_v[:, :, c0:c0 + 128]), [], [wcb])
            for tt in range(4):
                t = half * 4 + tt
                ev = evs[k % 4]
                evb = evbs[k % 4]
                bank = 2 + (k % 4)
                for kc in range(8):
                    P.op("pe", [wcb, xTb[tt][kc]], [C.psb[bank]], "matmul", C.ps[bank][:, :],
                         lhsT=r32(wc[:, kc, :]), rhs=r32(xT[:, kc, tt * 512:(tt + 1) * 512]),
                         start=(kc == 0), stop=(kc == 7))
                if k % 2 == 0:
                    P.op("act", [C.psb[bank]], [evb], "copy", out=ev, in_=C.ps[bank][:, :])
                else:
                    P.op("dve", [C.psb[bank]], [evb], "tensor_copy", out=ev, in_=C.ps[bank][:, :])
                P.dma("pool", projT[c0:c0 + nrow, t * 512:(t + 1) * 512], ev[0:nrow, :], [evb], [projb[ci][t]])
                k += 1


NEGM = -240000.0


def even_mixer(C, x_in, x_in_bufs, x_out, x_out_bufs, prm, scr):
    P = C.P
    A = C.A
    w_in = prm["l0_mix_w_in"]
    projT, projb = scr["projT"], scr["projb"]
    yT, yTb = scr["yT"], scr["yTb"]
    proj_phase(C, x_in, x_in_bufs, w_in, [c * 128 for c in range(24)], -1, projT, projb)
    P.barrier()
    A.reset()
    C.N.reset()
    N = C.N
    HB = S // 2
    bg = N.alloc(HB + 2)
    cg = N.alloc(HB + 2)
    xh = N.alloc(HB + 2)
    acc = N.alloc(HB + 2)
    cw = N.alloc(4)
    bgb, cgb, xhb, accb, cwb = Buf("bg"), Buf("cg"), Buf("xh"), Buf("acc"), Buf("cw")
    for c in range(2):
        P.dma("sp", cw[:, 0:3], C.consts["conv_wT"][c * 128:(c + 1) * 128, :], [bgb, cgb, accb], [cwb])
        for hf in range(2):
            lo = max(0, hf * HB - 2)
            hi = hf * HB + HB
            n = hi - lo
            halo = hf * HB - lo
            P.dma("sp", bg[:, 0:n], projT[c * 128:(c + 1) * 128, lo:hi], projb[c], [bgb])
            P.dma("sp", cg[:, 0:n], projT[(2 + c) * 128:(3 + c) * 128, lo:hi], projb[2 + c], [cgb])
            P.dma("sp", xh[:, 0:n], projT[(4 + c) * 128:(5 + c) * 128, lo:hi], projb[4 + c], [xhb])
            P.op("dve", [cgb, xhb], [cgb], "tensor_tensor", out=cg[:, 0:n], in0=cg[:, 0:n], in1=xh[:, 0:n], op=ALU.mult)
            P.op("dve", [cgb, cwb], [accb], "tensor_scalar", out=acc[:, 0:n], in0=cg[:, 0:n], scalar1=cw[:, 2:3],
                 scalar2=None, op0=ALU.mult)
            P.op("dve", [cgb, cwb, accb], [accb], "scalar_tensor_tensor", out=acc[:, 1:n], in0=cg[:, 0:n - 1],
                 scalar=cw[:, 1:2], in1=acc[:, 1:n], op0=ALU.mult, op1=ALU.add)
            P.op("dve", [cgb, cwb, accb], [accb], "scalar_tensor_tensor", out=acc[:, 2:n], in0=cg[:, 0:n - 2],
                 scalar=cw[:, 0:1], in1=acc[:, 2:n], op0=ALU.mult, op1=ALU.add)
            P.op("dve", [bgb, accb], [accb], "tensor_tensor", out=acc[:, 0:n], in0=acc[:, 0:n], in1=bg[:, 0:n], op=ALU.mult)
            P.dma("pool", yT[c * 128:c * 128 + 64, hf * HB:hi], acc[0:64, halo:n], [accb], [yTb[2 * c]])
            P.dma("pool", yT[c * 128 + 64:c * 128 + 128, hf * HB:hi], acc[64:128, halo:n], [accb], [yTb[2 * c + 1]])
    P.barrier()
    A.reset()
    C.N.reset()
    qt = [A.alloc(S, 64) for i in range(2)]
    kt = [A.alloc(S, 64) for i in range(2)]
    N = C.N
    vt1 = N.alloc(S, 64)
    vt = [vt1, vt1]
    vtb = Buf("v")
    qkvb = [[Buf("q%d" % i), Buf("k%d" % i), vtb] for i in range(2)]
    acc = N.alloc(S, 65)
    accb = Buf("acc")
    mask = N.alloc(256)
    maskb = Buf("mask")
    sel = N.alloc(64, 65)
    selb = Buf("sel")
    P.dma("sp", mask, C.consts["maskT"], [], [maskb])
    P.dma("sp", sel, C.consts["sel"], [], [selb])
    sms = [N.alloc(256) for i in range(3)]
    smbs = [Buf("sm%d" % i) for i in range(3)]
    pts = [A.alloc(256) for i in range(3)]
    ptbs = [Buf("pt%d" % i) for i in range(3)]
    vxs = [A.alloc(65) for i in range(3)]
    vxbs = [Buf("vx%d" % i) for i in range(3)]
    for i in range(3):
        P.op("dve", [C.oneb], [vxbs[i]], "tensor_copy", out=r32(vxs[i][:, 64:65]), in_=C.one[:, 0:1])
    rds = [N.alloc(512, 64) for i in range(2)]
    rdbs = [Buf("rd%d" % i) for i in range(2)]
    yos = [N.alloc(512, 64) for i in range(2)]
    yobs = [Buf("yo%d" % i) for i in range(2)]
    st = {"blk": 0, "vxc": 0, "grp": 0}
    nrm = 0
    for hd in range(12):
        hi = hd % 2
        q, kk, v = qt[hi], kt[hi], vt[hi]
        qb, kb, vb = qkvb[hi]
        cq, rq = divmod(768 + hd * 64, 128)
        ck, rk = divmod(1536 + hd * 64, 128)
        cv, rv = divmod(2304 + hd * 64, 128)
        P.dma("sp", r32(q), r32(projT[768 + hd * 64:768 + (hd + 1) * 64, :]), projb[cq], [qb])
        P.dma("sp", r32(kk), r32(projT[1536 + hd * 64:1536 + (hd + 1) * 64, :]), projb[ck], [kb])
        P.dma("sp", v, projT[2304 + hd * 64:2304 + (hd + 1) * 64, :], projb[cv], [vb])
        blocks = []
        for bi, d in enumerate((1, 4, 16)):
            nb = 32 // d
            for r in range(d):
                for n in range(nb):
                    blocks.append({"bi": bi, "d": d, "r": r, "n": n, "nb": nb, "G": min(4, nb)})

        def stage_a(B):
            d, r, n = B["d"], B["r"], B["n"]
            st0 = r + d * 128 * n
            cur = slice(st0, min(st0 + d * 128, S), d)
            blk = st["blk"]
            st["blk"] += 1
            sbank = blk % 3
            sm, smb = sms[blk % 3], smbs[blk % 3]
            pt, ptb = pts[blk % 3], ptbs[blk % 3]
            ncol = 256 if n > 0 else 128
            P.op("pe", [kb, qb], [C.psb[sbank]], "matmul", C.ps[sbank][:, 0:128],
                 lhsT=r32(kk[:, cur]), rhs=r32(q[:, cur]), start=True, stop=True)
            if n > 0:
                sp0 = r + d * 128 * (n - 1)
                prv = slice(sp0, min(sp0 + d * 128, S), d)
                P.op("pe", [kb, qb], [C.psb[sbank]], "matmul", C.ps[sbank][:, 128:256],
                     lhsT=r32(kk[:, prv]), rhs=r32(q[:, cur]), start=True, stop=True)
            P.op("dve", [C.psb[sbank], maskb], [smb], "tensor_tensor", out=sm[:, 0:ncol],
                 in0=C.ps[sbank][:, 0:ncol], in1=mask[:, 0:ncol], op=ALU.add)
            P.op("act", [smb], [ptb], "activation", out=r32(pt[:, 0:ncol]), in_=sm[:, 0:ncol],
                 func=AF.Exp, scale=0.125)
            vxc = st["vxc"]
            st["vxc"] += 1
            vx, vxb = vxs[vxc % 3], vxbs[vxc % 3]
            tb = 3 + (vxc % 2)
            P.op("pe", [vb, C.identb], [C.psb[tb]], "transpose", out=C.ps[tb][:, 0:64],
                 in_=v[:, cur], identity=C.ident[0:64, 0:64])
            P.op("dve", [C.psb[tb]], [vxb], "tensor_copy", out=r32(vx[:, 0:64]), in_=C.ps[tb][:, 0:64])
            B.update(pt=pt, ptb=ptb, vx=vx, vxb=vxb, st0=st0)

        def stage_b(B, Bprev):
            d, n, G, bi = B["d"], B["n"], B["G"], B["bi"]
            gi = n % G
            if gi == 0:
                st["gbank"] = 5 + (st["grp"] % 2)
                st["grp"] += 1
                st["g_start"] = B["st0"]
            gbank = st["gbank"]
            osl = C.ps[gbank][0:65, gi * 128:(gi + 1) * 128]
            P.op("pe", [B["vxb"], B["ptb"]], [C.psb[gbank]], "matmul", osl, lhsT=r32(B["vx"][:, 0:65]),
                 rhs=r32(B["pt"][:, 0:128]), start=True, stop=(n == 0))
            if n > 0:
                P.op("pe", [Bprev["vxb"], B["ptb"]], [C.psb[gbank]], "matmul", osl, lhsT=r32(Bprev["vx"][:, 0:65]),
                     rhs=r32(B["pt"][:, 128:256]), start=False, stop=True)
            if gi == G - 1:
                g_start = st["g_start"]
                gs = slice(g_start, min(g_start + d * 128 * G, S), d)
                if bi == 0:
                    P.op("act", [C.psb[gbank]], [accb], "copy", out=acc[:, gs], in_=C.ps[gbank][0:65, 0:128 * G])
                else:
                    P.op("dve", [C.psb[gbank], accb], [accb], "tensor_tensor", out=acc[:, gs], in0=acc[:, gs],
                         in1=C.ps[gbank][0:65, 0:128 * G], op=ALU.add)

        for i in range(len(blocks) + 1):
            if i < len(blocks):
                stage_a(blocks[i])
            if i >= 1:
                stage_b(blocks[i - 1], blocks[i - 2] if i >= 2 else None)
        for cb in range(8):
            cs = slice(cb * 512, (cb + 1) * 512)
            rd, rdb = rds[nrm % 2], rdbs[nrm % 2]
            yo, yob = yos[nrm % 2], yobs[nrm % 2]
            nrm += 1
            P.op("pe", [accb, selb], [C.psb[7]], "matmul", C.ps[7][0:64, :], lhsT=sel, rhs=acc[:, cs],
                 start=True, stop=True)
            P.op("dve", [C.psb[7]], [rdb], "reciprocal", out=rd, in_=C.ps[7][0:64, :])
            P.op("dve", [rdb, accb], [yob], "tensor_tensor", out=yo, in0=acc[0:64, cs], in1=rd, op=ALU.mult)
            P.dma("pool", yT[256 + hd * 64:256 + (hd + 1) * 64, cs], yo, [yob], [yTb[4 + hd]])
    out_proj_norm(C, yT, yTb, prm["l0_mix_w_out"], prm["l0_ln2_g"], prm["l0_ln2_b"], x_in, x_in_bufs, x_out, x_out_bufs)


def odd_mixer(C, x_in, x_in_bufs, x_out, x_out_bufs, prm, scr):
    P = C.P
    A = C.A
    N = C.N
    w_in = prm["l1_mix_w_in"]
    projT, projb = scr["projT"], scr["projb"]
    yT, yTb = scr["yT"], scr["yTb"]
    ccols = [i * 128 for i in range(16)] + [2048] + [2056 + i * 128 for i in range(12)]
    proj_phase(C, x_in, x_in_bufs, w_in, ccols, 16, projT, projb)
    P.barrier()
    A.reset()
    N.reset()
    triu = N.alloc(128)
    ones = N.alloc(128)
    zer = N.alloc(130)
    cb_ = Buf("mconst")
    P.dma("sp", triu, C.consts["triu"], [], [cb_])
    P.op("dve", [], [cb_], "memset", ones, 1.0)
    P.op("dve", [], [cb_], "memset", zer, 0.0)
    ng = N.alloc(512)
    ngb = Buf("ng")
    P.dma("sp", ng, prm["l1_mlstm_norm_g"].partition_broadcast(128), [], [ngb])
    biasb = N.alloc(8)
    biasbb = Buf("biasb")
    P.dma("sp", biasb[:, 0:4], prm["l1_mlstm_b_i"].partition_broadcast(128), [], [biasbb])
    P.dma("sp", biasb[:, 4:8], prm["l1_mlstm_b_f"].partition_broadcast(128), [], [biasbb])
    grow = N.alloc(S, 8)
    growb = Buf("grow")
    P.dma("sp", grow, projT[2048:2056, :], projb[16], [growb])
    gcol = N.alloc(256)
    nlf = N.alloc(256)
    tmp = N.alloc(128)
    egs = N.alloc(128)
    ea = N.alloc(128)
    eb = N.alloc(128)
    gb_ = Buf("gcol")
    for b in range(32):
        P.op("pe", [growb, C.identb], [C.psb[0]], "transpose", out=C.ps[0][:, b * 8:(b + 1) * 8],
             in_=grow[:, b * 128:(b + 1) * 128], identity=C.ident[0:8, 0:8])
    for b in range(32):
        P.op("dve", [C.psb[0], biasbb], [gb_], "tensor_tensor", out=gcol[:, b * 8:(b + 1) * 8],
             in0=C.ps[0][:, b * 8:(b + 1) * 8], in1=biasb, op=ALU.add)
    P.op("act", [gb_], [gb_], "activation", out=nlf, in_=gcol, func=AF.Exp, scale=-1.0)
    P.op("act", [gb_], [gb_], "activation", out=nlf, in_=nlf, func=AF.Ln, bias=1.0)
    P.op("pe", [gb_, cb_], [C.psb[1]], "matmul", C.ps[1][:, 0:256], lhsT=triu, rhs=nlf, start=True, stop=True)
    P.op("pe", [gb_, cb_], [C.psb[2]], "matmul", C.ps[2][:, 0:256], lhsT=ones, rhs=nlf, start=True, stop=True)
    g3 = gcol.rearrange("p (b j) -> p b j", j=8)
    cs3 = C.ps[1][:, 0:256].rearrange("p (b j) -> p b j", j=8)
    tot3 = C.ps[2][:, 0:256].rearrange("p (b j) -> p b j", j=8)
    v3 = lambda ap: ap.rearrange("p (b j) -> p b j", j=4)
    P.op("dve", [gb_, C.psb[1]], [gb_], "tensor_tensor", out=v3(tmp), in0=g3[:, :, 0:4], in1=cs3[:, :, 4:8], op=ALU.add)
    P.op("act", [gb_], [gb_], "activation", out=tmp, in_=tmp, func=AF.Exp)
    P.op("dve", [gb_], [gb_], "tensor_scalar", out=egs, in0=tmp, scalar1=float(128 ** -0.5), scalar2=None, op0=ALU.mult)
    P.op("act", [C.psb[1]], [gb_], "activation", out=v3(ea), in_=cs3[:, :, 4:8], func=AF.Exp, scale=-1.0)
    P.op("act", [C.psb[2]], [gb_], "activation", out=v3(eb), in_=tot3[:, :, 4:8], func=AF.Exp, scale=-1.0)
    cext = [A.alloc(130) for h in range(4)]
    cextb = [Buf("cext%d" % h) for h in range(4)]
    for h in range(4):
        P.op("dve", [cb_], [cextb[h]], "tensor_copy", out=r32(cext[h]), in_=zer)
    vexts = [A.alloc(130) for i in range(2)]
    vextbs = [Buf("vext%d" % i) for i in range(2)]
    for i in range(2):
        P.op("dve", [C.oneb], [vextbs[i]], "tensor_copy", out=r32(vexts[i][:, 128:130]), in_=C.one[:, 0:2])
    kgs = [A.alloc(128) for i in range(2)]
    kgbs = [Buf("kg%d" % i) for i in range(2)]
    wts = [A.alloc(128) for i in range(2)]
    wtbs = [Buf("wt%d" % i) for i in range(2)]
    q4s = [A.alloc3(4, 512) for i in range(2)]
    k4s = [A.alloc3(4, 512) for i in range(2)]
    qkbs = [[Buf("q4%d" % i), Buf("k4%d" % i)] for i in range(2)]
    v4 = N.alloc3(4, 512)
    o4 = N.alloc3(4, 512)
    v4b, o4b = Buf("v4"), Buf("o4")
    ymT = N.alloc3(4, 512)
    ymTb = Buf("ymT")
    scs = [N.alloc(16) for i in range(2)]
    scbs = [Buf("sc%d" % i) for i in range(2)]
    hts = [N.alloc(128) for i in range(2)]
    htbs = [Buf("ht%d" % i) for i in range(2)]
    sgos = [N.alloc(128) for i in range(2)]
    sgobs = [Buf("sgo%d" % i) for i in range(2)]
    hv = lambda r0: projT[r0:r0 + 512, :].rearrange("(h p) n -> p h n", p=128)
    pj = lambda c0: [b for ci in range(c0, c0 + 4) for b in projb[ci]]
    mtail = [None]
    for t in range(8):
        ts_ = slice(t * 512, (t + 1) * 512)
        q4, k4 = q4s[t % 2], k4s[t % 2]
        q4b, k4b = qkbs[t % 2]
        P.dma("sp", r32(q4), r32(hv(0)[:, :, ts_]), pj(0), [q4b])
        P.dma("sp", r32(k4), r32(hv(512)[:, :, ts_]), pj(4), [k4b])
        P.dma("sp", v4, hv(1024)[:, :, ts_], pj(8), [v4b])
        P.dma("sp", o4, hv(1536)[:, :, ts_], pj(12), [o4b])
        for cc in range(4):
            c = t * 4 + cc
            cl = slice(cc * 128, (cc + 1) * 128)
            for h in range(4):
                i2 = h % 2
                ch = c * 4 + h
                vext, vextb = vexts[i2], vextbs[i2]
                kg, kgb = kgs[i2], kgbs[i2]
                wt, wtb = wts[i2], wtbs[i2]
                sc, scb = scs[i2], scbs[i2]
                ht, htb = hts[i2], htbs[i2]
                sgo, sgob = sgos[i2], sgobs[i2]
                egs_c = egs[:, ch:ch + 1]
                ea_c = ea[:, ch:ch + 1]
                eb_c = eb[:, ch:ch + 1]
                P.op("pe", [v4b, C.identb], [C.psb[0]], "transpose", out=C.ps[0][:, 0:128], in_=v4[:, h, cl],
                     identity=C.ident[:, :])
                P.op("act", [C.psb[0]], [vextb], "copy", out=r32(vext[:, 0:128]), in_=C.ps[0][:, 0:128])
                P.op("pe", [k4b, C.identb], [C.psb[1]], "transpose", out=C.ps[1][:, 0:128], in_=k4[:, h, cl],
                     identity=C.ident[:, :])
                P.op("dve", [C.psb[1], gb_], [kgb], "tensor_scalar", out=r32(kg), in0=C.ps[1][:, 0:128], scalar1=egs_c,
                     scalar2=None, op0=ALU.mult)
                P.op("pe", [k4b, q4b], [C.psb[2]], "matmul", C.ps[2][:, 0:128], lhsT=r32(k4[:, h, cl]),
                     rhs=r32(q4[:, h, cl]), start=True, stop=True)
                P.op("dve", [C.psb[2], gb_, cb_], [wtb], "scalar_tensor_tensor", out=r32(wt), in0=C.ps[2][:, 0:128],
                     scalar=egs_c, in1=triu, op0=ALU.mult, op1=ALU.mult)
                P.op("pe", [q4b, cextb[h]], [C.psb[3]], "matmul", C.ps[3][:, 0:130], lhsT=r32(q4[:, h, cl]),
                     rhs=r32(cext[h][:, 0:130]), start=True, stop=False)
                P.op("pe", [wtb, vextb], [C.psb[3]], "matmul", C.ps[3][:, 0:130], lhsT=r32(wt), rhs=r32(vext),
                     start=False, stop=True)
                P.op("dve", [C.psb[3], gb_], [scb], "tensor_scalar", out=sc[:, 0:1], in0=C.ps[3][:, 128:129],
                     scalar1=ea_c, scalar2=None, op0=ALU.mult)
                P.op("dve", [scb], [scb], "tensor_scalar", out=sc[:, 3:4], in0=sc[:, 0:1], scalar1=-1.0, scalar2=1.0,
                     op0=ALU.mult, op1=ALU.max)
                P.op("dve", [scb], [scb], "tensor_scalar", out=sc[:, 0:1], in0=sc[:, 0:1], scalar1=1.0, scalar2=None,
                     op0=ALU.max)
                P.op("dve", [scb], [scb], "tensor_tensor", out=sc[:, 0:1], in0=sc[:, 0:1], in1=sc[:, 3:4], op=ALU.max)
                P.op("dve", [scb], [scb], "reciprocal", out=sc[:, 1:2], in_=sc[:, 0:1])
                P.op("dve", [scb, gb_], [scb], "tensor_tensor", out=sc[:, 2:3], in0=sc[:, 1:2], in1=ea_c, op=ALU.mult)
                P.op("dve", [C.psb[3], scb], [htb], "tensor_scalar", out=ht, in0=C.ps[3][:, 0:128], scalar1=sc[:, 2:3],
                     scalar2=None, op0=ALU.mult)
                P.op("pe", [o4b, C.identb], [C.psb[4]], "transpose", out=C.ps[4][:, 0:128], in_=o4[:, h, cl],
                     identity=C.ident[:, :])
                P.op("act", [C.psb[4]], [sgob], "activation", out=sgo, in_=C.ps[4][:, 0:128], func=AF.Sigmoid)
                P.op("dve", [htb, sgob], [htb], "tensor_tensor", out=ht, in0=ht, in1=sgo, op=ALU.mult)
                P.op("dve", [htb], [scb], "bn_stats", out=sc[:, 4:10], in_=ht)
                P.op("dve", [scb], [scb], "bn_aggr", out=sc[:, 10:12], in_=sc[:, 4:10])
                P.op("dve", [scb], [scb], "tensor_scalar", out=sc[:, 12:13], in0=sc[:, 11:12], scalar1=LN_EPS,
                     scalar2=None, op0=ALU.add)
                P.op("pool", [scb, C.mhalfb], [scb], "tensor_tensor", out=sc[:, 13:14], in0=sc[:, 12:13],
                     in1=C.mhalf[:, 0:1], op=ALU.pow)
                P.op("pe", [kgb, vextb], [C.psb[6]], "matmul", C.ps[6][:, 0:130], lhsT=r32(kg), rhs=r32(vext),
                     start=True, stop=True)
                P.op("dve", [cextb[h], gb_], [cextb[h]], "tensor_scalar", out=r32(cext[h]), in0=cext[h], scalar1=eb_c,
                     scalar2=None, op0=ALU.mult)
                P.op("dve", [C.psb[6], cextb[h], gb_], [cextb[h]], "scalar_tensor_tensor", out=r32(cext[h]),
                     in0=C.ps[6][:, 0:130], scalar=eb_c, in1=cext[h], op0=ALU.mult, op1=ALU.add)
                if mtail[0] is not None:
                    mtail[0]()

                def tail(ht=ht, htb=htb, sc=sc, scb=scb, h=h, cl=cl):
                    P.op("dve", [htb, scb], [htb], "tensor_scalar", out=ht, in0=ht, scalar1=sc[:, 10:11],
                         scalar2=sc[:, 13:14], op0=ALU.subtract, op1=ALU.mult)
                    P.op("dve", [htb, ngb], [htb], "tensor_tensor", out=ht, in0=ht, in1=ng[:, h * 128:(h + 1) * 128],
                         op=ALU.mult)
                    P.op("pe", [htb, C.identb], [C.psb[5]], "transpose", out=C.ps[5][:, 0:128], in_=ht,
                         identity=C.ident[:, :])
                    P.op("act", [C.psb[5]], [ymTb], "copy", out=ymT[:, h, cl], in_=C.ps[5][:, 0:128])
                mtail[0] = tail
        if mtail[0] is not None:
            mtail[0]()
            mtail[0] = None
        for h in range(4):
            for hh in range(2):
                P.dma("pool", yT[h * 128 + hh * 64:h * 128 + (hh + 1) * 64, ts_], ymT[hh * 64:(hh + 1) * 64, h, :],
                      [ymTb], [yTb[2 * h + hh]])
    P.barrier()
    A.reset()
    N.reset()
    ustr = A.alloc(128)
    onesr = A.alloc(128)
    identr = A.alloc(128)
    scb_ = Buf("sconst")
    P.dma("sp", r32(ustr), r32(C.consts["ustr"]), [], [scb_])
    P.dma("sp", r32(onesr), r32(C.consts["ones128"]), [], [scb_])
    P.dma("sp", r32(identr), r32(C.consts["ident"]), [], [scb_])
    maskd = N.alloc3(4, 512)
    maskdb = Buf("maskd")
    P.dma("sp", maskd, C.consts["maskd"].rearrange("p (i q) -> p i q", i=4), [], [maskdb])
    zer = N.alloc(128)
    zerb = Buf("zer")
    P.op("dve", [], [zerb], "memset", zer, 0.0)
    qTs = [A.alloc(S, 64) for i in range(2)]
    kTs = [A.alloc(S, 64) for i in range(2)]
    vTs = [N.alloc(S, 64) for i in range(2)]
    hbufs = [[Buf("sq%d" % i), Buf("sk%d" % i), Buf("sv%d" % i)] for i in range(2)]
    vtoks = [A.alloc3(32, 128) for i in range(2)]
    vtokbs = [Buf("vtok%d" % i) for i in range(2)]
    for j in range(2):
        for kb in range(32):
            P.op("dve", [zerb], [vtokbs[j]], "tensor_copy", out=r32(vtoks[j][:, kb, 64:128]), in_=zer[:, 0:64])
    e_ = N.alloc(1024)
    eb_ = Buf("e")
    tts = [N.alloc(1024) for i in range(2)]
    ttbs = [Buf("tt%d" % i) for i in range(2)]
    nLs = [A.alloc(1024) for i in range(2)]
    nLbs = [Buf("nL%d" % i) for i in range(2)]
    Ats = [A.alloc(1024) for i in range(2)]
    Atbs = [Buf("At%d" % i) for i in range(2)]
    Rs = [A.alloc(512) for i in range(2)]
    Rbs = [Buf("R%d" % i) for i in range(2)]
    yos = [N.alloc(512, 64) for i in range(2)]
    yobs = [Buf("syo%d" % i) for i in range(2)]
    z2 = [C.psall[:, 0:1024], C.psall[:, 1024:2048]]
    z2b = [[C.psb[0], C.psb[1]], [C.psb[2], C.psb[3]]]
    nc2 = C.psall[:, 2048:3072]
    nc2b = [C.psb[4], C.psb[5]]
    steps = []
    for h in range(8):
        for Q in range(8):
            for G2 in range(2 * Q + 1, -1, -1):
                steps.append({"h": h, "Q": Q, "G2": G2, "first": G2 == 2 * Q + 1, "last": G2 == 0})
    cnt = {"r": 0, "y": 0}

    def prologue(h):
        hi = h % 2
        qT, kT, vT = qTs[hi], kTs[hi], vTs[hi]
        qTb, kTb, vTb = hbufs[hi]
        vtok, vtokb = vtoks[hi], vtokbs[hi]
        rq, rk, rv = 2056 + h * 64, 2568 + h * 64, 3080 + h * 64
        cidx = lambda r0: projb[17 + (r0 - 2056) // 128]
        P.dma("sp", r32(qT), r32(projT[rq:rq + 64, :]), cidx(rq), [qTb])
        P.dma("sp", r32(kT), r32(projT[rk:rk + 64, :]), cidx(rk), [kTb])
        P.dma("sp", vT, projT[rv:rv + 64, :], cidx(rv), [vTb])
        for rd in range(4):
            for i in range(8):
                kb = rd * 8 + i
                P.op("pe", [vTb, C.identb], [C.psb[6]], "transpose", out=C.ps[6][:, i * 64:(i + 1) * 64],
                     in_=vT[:, kb * 128:(kb + 1) * 128], identity=C.ident[0:64, 0:64])
            P.op("dve", [C.psb[6]], [vtokb], "tensor_copy", out=r32(vtok[:, rd * 8:(rd + 1) * 8, 0:64]),
                 in_=C.ps[6][:, :].rearrange("p (i e) -> p i e", e=64))

    def stage_a(k):
        St = steps[k]
        h, Q, G2 = St["h"], St["Q"], St["G2"]
        if Q == 0 and G2 == 1:
            prologue(h)
        hi = h % 2
        qT, kT = qTs[hi], kTs[hi]
        qTb, kTb, _ = hbufs[hi]
        qs = qT[:, Q * 512:(Q + 1) * 512]
        kbs = [2 * G2 + 1, 2 * G2]
        zi = k % 2
        z, zb = z2[zi], z2b[zi]
        nL, nLb = nLs[zi], nLbs[zi]
        for i, kb in enumerate(kbs):
            P.op("pe", [kTb, qTb], [zb[i]], "matmul", z[:, i * 512:(i + 1) * 512],
                 lhsT=r32(kT[:, kb * 128:(kb + 1) * 128]), rhs=r32(qs), start=True, stop=True)
        P.op("act", zb, [eb_], "activation", out=e_, in_=z, func=AF.Exp, scale=0.125)
        P.op("act", [eb_], [nLb], "activation", out=r32(nL), in_=e_, func=AF.Ln, bias=1.0)
        if G2 >= 2 * Q:
            for i, kb in enumerate(kbs):
                ib = kb - 4 * Q
                P.op("dve", [nLb, maskdb], [nLb], "tensor_tensor", out=r32(nL[:, i * 512:(i + 1) * 512]),
                     in0=nL[:, i * 512:(i + 1) * 512], in1=maskd[:, ib, :], op=ALU.mult)

    def stage_b(k):
        St = steps[k]
        h, Q, G2, first, last = St["h"], St["Q"], St["G2"], St["first"], St["last"]
        kbs = [2 * G2 + 1, 2 * G2]
        zi = k % 2
        z, zb = z2[zi], z2b[zi]
        nL, nLb = nLs[zi], nLbs[zi]
        At, Atb = Ats[zi], Atbs[zi]
        tt, ttb = tts[zi], ttbs[zi]
        if first:
            Rcur, Rcurb = None, None
        else:
            Rcur, Rcurb = steps[k - 1]["Rn"]
        if not last:
            P.op("pe", [nLb, scb_], [C.psb[6]], "matmul", C.ps[6][:, :], lhsT=r32(onesr), rhs=r32(nL[:, 0:512]),
                 start=True, stop=False)
            P.op("pe", [nLb, scb_], [C.psb[6]], "matmul", C.ps[6][:, :], lhsT=r32(onesr),
                 rhs=r32(nL[:, 512:1024]), start=False, stop=first)
            if not first:
                P.op("pe", [Rcurb, scb_], [C.psb[6]], "matmul", C.ps[6][:, :], lhsT=r32(identr), rhs=r32(Rcur),
                     start=False, stop=True)
            Rn, Rnb = Rs[cnt["r"] % 2], Rbs[cnt["r"] % 2]
            cnt["r"] += 1
            P.op("act", [C.psb[6]], [Rnb], "copy", out=r32(Rn), in_=C.ps[6][:, :])
            St["Rn"] = (Rn, Rnb)
        P.op("pe", [nLb, scb_], [nc2b[0]], "matmul", nc2[:, 0:512], lhsT=r32(ustr), rhs=r32(nL[:, 0:512]),
             start=True, stop=first)
        if not first:
            P.op("pe", [Rcurb, scb_], [nc2b[0]], "matmul", nc2[:, 0:512], lhsT=r32(identr), rhs=r32(Rcur),
                 start=False, stop=True)
        P.op("pe", [nLb, scb_], [nc2b[1]], "matmul", nc2[:, 512:1024], lhsT=r32(ustr),
             rhs=r32(nL[:, 512:1024]), start=True, stop=False)
        P.op("pe", [nLb, scb_], [nc2b[1]], "matmul", nc2[:, 512:1024], lhsT=r32(onesr),
             rhs=r32(nL[:, 0:512]), start=False, stop=first)
        if not first:
            P.op("pe", [Rcurb, scb_], [nc2b[1]], "matmul", nc2[:, 512:1024], lhsT=r32(identr), rhs=r32(Rcur),
                 start=False, stop=True)
        P.op("dve", zb + [nLb], [ttb], "scalar_tensor_tensor", out=tt, in0=z, scalar=0.125, in1=nL,
             op0=ALU.mult, op1=ALU.subtract)
        P.op("dve", [ttb] + nc2b, [ttb], "tensor_tensor", out=tt, in0=tt, in1=nc2, op=ALU.subtract)
        P.op("act", [ttb], [Atb], "activation", out=r32(At), in_=tt, func=AF.Exp)
        if G2 >= 2 * Q:
            for i, kb in enumerate(kbs):
                ib = kb - 4 * Q
                P.op("dve", [Atb, maskdb], [Atb], "tensor_tensor", out=r32(At[:, i * 512:(i + 1) * 512]),
                     in0=At[:, i * 512:(i + 1) * 512], in1=maskd[:, ib, :], op=ALU.mult)

    def stage_c(k):
        St = steps[k]
        h, Q, G2, first, last = St["h"], St["Q"], St["G2"], St["first"], St["last"]
        kbs = [2 * G2 + 1, 2 * G2]
        zi = k % 2
        At, Atb = Ats[zi], Atbs[zi]
        vtok, vtokb = vtoks[h % 2], vtokbs[h % 2]
        for i, kb in enumerate(kbs):
            P.op("pe", [vtokb, Atb], [C.psb[7]], "matmul", C.ps[7][:, :], lhsT=r32(vtok[:, kb, :]),
                 rhs=r32(At[:, i * 512:(i + 1) * 512]), start=(first and i == 0), stop=(last and i == 1))
        if last:
            yo, yob = yos[cnt["y"] % 2], yobs[cnt["y"] % 2]
            cnt["y"] += 1
            P.op("act", [C.psb[7]], [yob], "copy", out=yo, in_=C.ps[7][0:64, :])
            P.dma("pool", yT[512 + h * 64:512 + (h + 1) * 64, Q * 512:(Q + 1) * 512], yo, [yob], [yTb[8 + h]])

    ns = len(steps)
    for it in range(ns + 2):
        if it < ns:
            stage_a(it)
        if 1 <= it <= ns:
            stage_b(it - 1)
        if it >= 2:
            stage_c(it - 2)
    out_proj_norm(C, yT, yTb, prm["l1_mix_w_out"], prm["l1_ln2_g"], prm["l1_ln2_b"], x_in, x_in_bufs, x_out, x_out_bufs)


PARAM_NAMES = [
    "l0_ffn1_w_in", "l0_ffn1_w_out", "l0_ln1_g", "l0_ln1_b",
    "l0_mix_w_in", "l0_conv_w", "l0_mix_w_out", "l0_ln2_g", "l0_ln2_b",
    "l0_ffn2_w_in", "l0_ffn2_w_out", "l0_ln3_g", "l0_ln3_b",
    "l1_ffn1_w_in", "l1_ffn1_w_out", "l1_ln1_g", "l1_ln1_b",
    "l1_mix_w_in", "l1_mlstm_b_i", "l1_mlstm_b_f", "l1_mlstm_norm_g", "l1_mix_w_out",
    "l1_ln2_g", "l1_ln2_b",
    "l1_ffn2_w_in", "l1_ffn2_w_out", "l1_ln3_g", "l1_ln3_b",
]
PARAM_SHAPES = {
    "ffn1_w_in": [D, 2 * DFF], "ffn2_w_in": [D, 2 * DFF], "ffn1_w_out": [DFF, D], "ffn2_w_out": [DFF, D],
    "l0_mix_w_in": [D, 3072], "l1_mix_w_in": [D, 3592], "mix_w_out": [D, D], "l0_conv_w": [3, 256],
    "l1_mlstm_b_i": [4], "l1_mlstm_b_f": [4], "l1_mlstm_norm_g": [512],
}


def pshape(n):
    if n in PARAM_SHAPES:
        return PARAM_SHAPES[n]
    k = n[3:]
    if k in PARAM_SHAPES:
        return PARAM_SHAPES[k]
    return [D]


def build(nsub=6):
    nc = bass.Bass("TRN2", target_bir_lowering=False)
    nc.dge_precook = False
    x_d = nc.dram_tensor("x", [S, D], F32, kind="ExternalInput").ap()
    prm = {n: nc.dram_tensor(n, pshape(n), F32, kind="ExternalInput").ap() for n in PARAM_NAMES}
    consts = {n: nc.dram_tensor(n, list(a.shape), F32, kind="ExternalInput").ap() for n, a in CONSTS.items()}
    consts["conv_wT"] = nc.dram_tensor("conv_wT", [256, 3], F32, kind="ExternalInput").ap()
    out_d = nc.dram_tensor("out", [S, D], F32, kind="ExternalOutput").ap()
    skind = "ExternalOutput" if DEBUG_OUT else "Internal"
    xa = nc.dram_tensor("xa", [S, D], F32, kind=skind).ap()
    xb = nc.dram_tensor("xb", [S, D], F32, kind=skind).ap()
    projT = nc.dram_tensor("projT", [3712, S], F32, kind="Internal").ap()
    yT = nc.dram_tensor("yT", [D, S], F32, kind=skind).ap()
    with ExitStack() as stack:
        P = Prog(nc, stack)
        C = setup_common(P, nc, consts)

        def mkbufs(name, n):
            sem = P.new_sem("d_" + name, True)
            return [Buf("%s%d" % (name, i), sem) for i in range(n)]
        bufs = {"x": mkbufs("x", 32), "xa": mkbufs("xa", 32), "xb": mkbufs("xb", 32), "out": mkbufs("out", 32)}
        aps = {"x": x_d, "xa": xa, "xb": xb, "out": out_d}
        pb = mkbufs("projT", 29 * 8)
        scr = {"projT": projT, "projb": [[pb[c * 8 + t] for t in range(8)] for c in range(29)],
               "yT": yT, "yTb": mkbufs("yT", 16)}
        plan = ["ffn1", "mix", "ffn2"] * 2
        cur = "x"
        for si in range(nsub):
            layer = si // 3
            kind = plan[si]
            nxt = "out" if si == nsub - 1 else ("xa" if cur != "xa" else "xb")
            p = "l%d_" % layer
            if kind in ("ffn1", "ffn2"):
                lnn = "ln1" if kind == "ffn1" else "ln3"
                ffn_sublayer(C, aps[cur], bufs[cur], aps[nxt], bufs[nxt],
                             prm[p + kind + "_w_in"], prm[p + kind + "_w_out"], prm[p + lnn + "_g"], prm[p + lnn + "_b"])
            elif layer == 0:
                even_mixer(C, aps[cur], bufs[cur], aps[nxt], bufs[nxt], prm, scr)
            else:
                odd_mixer(C, aps[cur], bufs[cur], aps[nxt], bufs[nxt], prm, scr)
            cur = nxt
        P.final_wait("pool", bufs["out"])
        P.emit()
    return nc


def _mk_consts():
    c = {}
    c["ident"] = np.eye(128, dtype=np.float32)
    k = np.arange(128)[:, None]
    q = np.arange(128)[None, :]
    m = np.zeros((128, 256), np.float32)
    m[:, 0:128] = np.where(k <= q, 0.0, NEGM)
    m[:, 128:256] = np.where(k >= q, 0.0, NEGM)
    c["maskT"] = m
    sel = np.zeros((65, 64), np.float32)
    sel[64, :] = 1.0
    c["sel"] = sel
    a = np.arange(128)
    c["triu"] = (a[:, None] <= a[None, :]).astype(np.float32)
    c["ustr"] = (a[:, None] > a[None, :]).astype(np.float32)
    c["ones128"] = np.ones((128, 128), np.float32)
    kk = (np.arange(4)[None, :, None] * 128 + a[:, None, None])
    qq = np.arange(512)[None, None, :]
    c["maskd"] = (kk < qq).astype(np.float32).reshape(128, 2048)
    return c


CONSTS = _mk_consts()


def run(inputs, nsub=6, trace=False):
    nc = build(nsub)
    x = np.ascontiguousarray(inputs["x"], dtype=np.float32)
    in_maps = []
    for c in range(NCORES):
        m = {"x": x[c]}
        m.update(CONSTS)
        m["conv_wT"] = np.ascontiguousarray(np.asarray(inputs["l0_conv_w"], dtype=np.float32).T)
        for n in PARAM_NAMES:
            m[n] = np.ascontiguousarray(inputs[n], dtype=np.float32)
        in_maps.append(m)
    res = run_bass_kernel_spmd(nc, in_maps, core_ids=list(range(NCORES)), trace=trace)
    out = np.stack([res.results[c]["out"] for c in range(NCORES)], axis=0)
    if DEBUG_OUT:
        global LAST_DEBUG
        LAST_DEBUG = {n: res.results[0][n] for n in ("xa", "xb", "yT") if n in res.results[0]}
    return out, res


def kernel(**inputs):
    out, _ = run(inputs, 6)
    return out.astype(np.float32)
```
